# Optimizing a Trainium2 kernel written in Bass

```python
import math
import jax, jax.numpy as jnp
from jax import lax
import numpy as np

D_MODEL = 1024
BATCH = 32
SEQ = 2048
DEPTH = 1

CHUNK = 64
Q_BLOCK = 128
A_HEADS = 8
A_HEAD_DIM = 64
A_WIDTH = A_HEADS * A_HEAD_DIM
IDX_HEADS = 4
IDX_DIM = 64
TOPK_MAX = 256
REL_BUCKETS = 32
REL_MAX_DIST = 128
SSM_GROUPS = 16
SSM_GROUP_CH = 16
SSM_WIDTH = SSM_GROUPS * SSM_GROUP_CH
SSM_STATE = 64
DT_MIN = 0.001
DT_MAX = 0.1
MEM_TOKENS = 256
MEM_HEADS = 4
MEM_HEAD_DIM = 64
MEM_WIDTH = MEM_HEADS * MEM_HEAD_DIM
N_BRANCHES = 3
FFN_DIM = 2816
NORM_EPS = 1e-6
IN_SPLITS = (A_WIDTH, A_HEAD_DIM, A_HEAD_DIM, IDX_HEADS * IDX_DIM, IDX_DIM, IDX_HEADS,
             SSM_WIDTH, MEM_WIDTH, N_BRANCHES * D_MODEL)
W_IN_COLS = (A_WIDTH + 2 * A_HEAD_DIM + IDX_HEADS * IDX_DIM + IDX_DIM + IDX_HEADS
             + SSM_WIDTH + MEM_WIDTH + N_BRANCHES * D_MODEL)

kernel_name = 'hybrid_dsa_s5_memory_macaron_block'


def rms_norm(x, g):
    xf = x.astype(jnp.float32)
    y = xf * lax.rsqrt(jnp.mean(xf * xf, axis=-1, keepdims=True) + NORM_EPS)
    return (y * g.astype(jnp.float32)).astype(x.dtype)


def swiglu_ffn(h, w_in, w_out):
    gate, up = jnp.split(h @ w_in, 2, axis=-1)
    return (jax.nn.silu(gate) * up) @ w_out


def t5_bucket(rel):
    half = REL_BUCKETS // 2
    max_exact = half // 2
    n = jnp.abs(rel)
    nf = jnp.maximum(n, 1).astype(jnp.float32)
    large = max_exact + (jnp.log(nf / max_exact) / math.log(REL_MAX_DIST / max_exact)
                         * (half - max_exact)).astype(jnp.int32)
    large = jnp.minimum(large, half - 1)
    return jnp.where(rel > 0, half, 0) + jnp.where(n < max_exact, n, large)


def indexed_sparse_attention(q, k, v, q_idx, k_idx, w_idx, rel_bias):
    b_, s_ = q.shape[0], q.shape[1]
    topk = min(TOPK_MAX, s_ // 4)
    nblk = s_ // Q_BLOCK
    key_pos = jnp.arange(s_, dtype=jnp.int32)
    scale = A_HEAD_DIM ** -0.5
    idx_scale = IDX_DIM ** -0.5
    head_w_scale = IDX_HEADS ** -0.5

    def to_blocks(a):
        return a.reshape((b_, nblk, Q_BLOCK) + a.shape[2:]).swapaxes(0, 1)

    def block(args):
        qb, qib, wb, start = args
        t = start + jnp.arange(Q_BLOCK, dtype=jnp.int32)
        limit = (t // CHUNK + 1) * CHUNK
        rel = jax.nn.relu(jnp.einsum('bqhd,bsd->bqhs', qib, k_idx).astype(jnp.float32) * idx_scale)
        score = jnp.einsum('bqhs,bqh->bqs', rel, wb.astype(jnp.float32) * head_w_scale)
        score = jnp.where(key_pos[None, None, :] < limit[None, :, None], score, -jnp.inf)
        _, sel = lax.top_k(score, topk)
        valid = sel < limit[None, :, None]
        flat = sel.reshape(b_, Q_BLOCK * topk, 1)
        k_sel = jnp.take_along_axis(k, flat, axis=1).reshape(b_, Q_BLOCK, topk, A_HEAD_DIM)
        v_sel = jnp.take_along_axis(v, flat, axis=1).reshape(b_, Q_BLOCK, topk, A_HEAD_DIM)
        bias = rel_bias[t5_bucket(sel - t[None, :, None])]
        logits = (jnp.einsum('bqhd,bqkd->bqhk', qb, k_sel).astype(jnp.float32) * scale
                  + jnp.transpose(bias, (0, 1, 3, 2)).astype(jnp.float32))
        logits = jnp.where(valid[:, :, None, :], logits, -jnp.inf)
        p = jax.nn.softmax(logits, axis=-1).astype(v_sel.dtype)
        return jnp.einsum('bqhk,bqkd->bqhd', p, v_sel)

    starts = jnp.arange(nblk, dtype=jnp.int32) * Q_BLOCK
    out = lax.map(block, (to_blocks(q), to_blocks(q_idx), to_blocks(w_idx), starts))
    return out.swapaxes(0, 1).reshape(b_, s_, A_WIDTH)


def _complex_affine_combine(e1, e2):
    a1r, a1i, b1r, b1i = e1
    a2r, a2i, b2r, b2i = e2
    return (a2r * a1r - a2i * a1i,
            a2r * a1i + a2i * a1r,
            a2r * b1r - a2i * b1i + b2r,
            a2r * b1i + a2i * b1r + b2i)


def s5_layer(u, lam_re, lam_im, log_dt, b_re, b_im, c_re, c_im, d):
    b_, s_ = u.shape[0], u.shape[1]
    f32 = jnp.float32
    uf = u.astype(f32).reshape(b_, s_, SSM_GROUPS, SSM_GROUP_CH)
    lr, li = lam_re.astype(f32), lam_im.astype(f32)
    dt = jnp.exp(log_dt.astype(f32))[:, None]
    mag = jnp.exp(lr * dt)
    ar, ai = mag * jnp.cos(li * dt), mag * jnp.sin(li * dt)
    den = lr * lr + li * li
    fr = ((ar - 1.0) * lr + ai * li) / den
    fi = (ai * lr - (ar - 1.0) * li) / den
    br, bi = b_re.astype(f32), b_im.astype(f32)
    bbr = fr[..., None] * br - fi[..., None] * bi
    bbi = fr[..., None] * bi + fi[..., None] * br
    bur = jnp.einsum('bsgc,gpc->bsgp', uf, bbr)
    bui = jnp.einsum('bsgc,gpc->bsgp', uf, bbi)
    elems = (jnp.broadcast_to(ar, bur.shape), jnp.broadcast_to(ai, bur.shape), bur, bui)
    _, _, xr, xi = lax.associative_scan(_complex_affine_combine, elems, axis=1)
    y = (jnp.einsum('bsgp,gcp->bsgc', xr, c_re.astype(f32))
         - jnp.einsum('bsgp,gcp->bsgc', xi, c_im.astype(f32))
         + d.astype(f32).reshape(SSM_GROUPS, SSM_GROUP_CH) * uf)
    return y.reshape(b_, s_, SSM_WIDTH).astype(u.dtype)


def memory_attention(q, k, v):
    logits = jnp.einsum('bshd,bmhd->bhsm', q, k).astype(jnp.float32) * (MEM_HEAD_DIM ** -0.5)
    p = jax.nn.softmax(logits, axis=-1).astype(v.dtype)
    out = jnp.einsum('bhsm,bmhd->bshd', p, v)
    return out.reshape(q.shape[0], q.shape[1], MEM_WIDTH)


def setup_inputs(seed: int = 0) -> dict:
    key = jax.random.key(seed)
    ks = jax.random.split(key, 32)
    f32 = jnp.float32
    L = DEPTH

    def nrm(k, shape, scale):
        return jax.random.normal(k, shape, f32) * scale

    def gain(k, shape):
        return 1.0 + 0.02 * jax.random.normal(k, shape, f32)

    n = jnp.arange(SSM_STATE, dtype=f32)
    return {
        'x': nrm(ks[0], (BATCH, SEQ, D_MODEL), 1.0),
        'mem': nrm(ks[1], (BATCH, MEM_TOKENS, D_MODEL), 1.0),
        'ffn1_norm': gain(ks[2], (L, D_MODEL)),
        'ffn1_w_in': nrm(ks[3], (L, D_MODEL, 2 * FFN_DIM), D_MODEL ** -0.5),
        'ffn1_w_out': nrm(ks[4], (L, FFN_DIM, D_MODEL), FFN_DIM ** -0.5),
        'mix_norm': gain(ks[5], (L, D_MODEL)),
        'w_in': nrm(ks[6], (L, D_MODEL, W_IN_COLS), D_MODEL ** -0.5),
        'a_q_gain': gain(ks[7], (L, A_HEAD_DIM)),
        'a_k_gain': gain(ks[8], (L, A_HEAD_DIM)),
        'rel_bias': nrm(ks[9], (REL_BUCKETS, A_HEADS), 0.5),
        'w_o_a': nrm(ks[10], (L, A_WIDTH, D_MODEL), A_WIDTH ** -0.5),
        'ssm_lambda_re': -0.5 + nrm(ks[11], (L, SSM_GROUPS, SSM_STATE), 0.01),
        'ssm_lambda_im': math.pi * n + nrm(ks[12], (L, SSM_GROUPS, SSM_STATE), 0.01),
        'ssm_log_dt': jax.random.uniform(ks[13], (L, SSM_GROUPS), f32,
                                         minval=math.log(DT_MIN), maxval=math.log(DT_MAX)),
        'ssm_b_re': nrm(ks[14], (L, SSM_GROUPS, SSM_STATE, SSM_GROUP_CH), (2 * SSM_GROUP_CH) ** -0.5),
        'ssm_b_im': nrm(ks[15], (L, SSM_GROUPS, SSM_STATE, SSM_GROUP_CH), (2 * SSM_GROUP_CH) ** -0.5),
        'ssm_c_re': nrm(ks[16], (L, SSM_GROUPS, SSM_GROUP_CH, SSM_STATE), SSM_STATE ** -0.5),
        'ssm_c_im': nrm(ks[17], (L, SSM_GROUPS, SSM_GROUP_CH, SSM_STATE), SSM_STATE ** -0.5),
        'ssm_d': nrm(ks[18], (L, SSM_WIDTH), 1.0),
        'w_glu': nrm(ks[19], (L, SSM_WIDTH, 2 * D_MODEL), SSM_WIDTH ** -0.5),
        'mem_norm': gain(ks[20], (L, D_MODEL)),
        'w_mem_kv': nrm(ks[21], (L, D_MODEL, 2 * MEM_WIDTH), D_MODEL ** -0.5),
        'm_q_gain': gain(ks[22], (L, MEM_HEAD_DIM)),
        'm_k_gain': gain(ks[23], (L, MEM_HEAD_DIM)),
        'w_o_m': nrm(ks[24], (L, MEM_WIDTH, D_MODEL), MEM_WIDTH ** -0.5),
        'w_out': nrm(ks[25], (L, D_MODEL, D_MODEL), D_MODEL ** -0.5),
        'ffn2_norm': gain(ks[26], (L, D_MODEL)),
        'ffn2_w_in': nrm(ks[27], (L, D_MODEL, 2 * FFN_DIM), D_MODEL ** -0.5),
        'ffn2_w_out': nrm(ks[28], (L, FFN_DIM, D_MODEL), FFN_DIM ** -0.5),
        'final_norm': gain(ks[29], (L, D_MODEL)),
    }


def reference(x, mem, ffn1_norm, ffn1_w_in, ffn1_w_out, mix_norm, w_in, a_q_gain, a_k_gain,
              rel_bias, w_o_a, ssm_lambda_re, ssm_lambda_im, ssm_log_dt, ssm_b_re, ssm_b_im,
              ssm_c_re, ssm_c_im, ssm_d, w_glu, mem_norm, w_mem_kv, m_q_gain, m_k_gain, w_o_m,
              w_out, ffn2_norm, ffn2_w_in, ffn2_w_out, final_norm):
    b_, s_ = x.shape[0], x.shape[1]
    offsets = [sum(IN_SPLITS[:i + 1]) for i in range(len(IN_SPLITS) - 1)]
    for l in range(DEPTH):
        x = x + 0.5 * swiglu_ffn(rms_norm(x, ffn1_norm[l]), ffn1_w_in[l], ffn1_w_out[l])
        h = rms_norm(x, mix_norm[l])
        q_a, k_a, v_a, q_i, k_i, w_i, u_s, q_m, gates = jnp.split(h @ w_in[l], offsets, axis=-1)
        q_a = rms_norm(q_a.reshape(b_, s_, A_HEADS, A_HEAD_DIM), a_q_gain[l])
        k_a = rms_norm(k_a, a_k_gain[l])
        o_a = indexed_sparse_attention(q_a, k_a, v_a, q_i.reshape(b_, s_, IDX_HEADS, IDX_DIM),
                                       k_i, w_i, rel_bias) @ w_o_a[l]
        y_s = jax.nn.gelu(s5_layer(u_s, ssm_lambda_re[l], ssm_lambda_im[l], ssm_log_dt[l],
                                   ssm_b_re[l], ssm_b_im[l], ssm_c_re[l], ssm_c_im[l], ssm_d[l]))
        glu_a, glu_b = jnp.split(y_s @ w_glu[l], 2, axis=-1)
        o_b = glu_a * jax.nn.sigmoid(glu_b)
        mem_kv = rms_norm(mem, mem_norm[l]) @ w_mem_kv[l]
        m_k, m_v = jnp.split(mem_kv, 2, axis=-1)
        m_k = rms_norm(m_k.reshape(b_, MEM_TOKENS, MEM_HEADS, MEM_HEAD_DIM), m_k_gain[l])
        m_v = m_v.reshape(b_, MEM_TOKENS, MEM_HEADS, MEM_HEAD_DIM)
        q_m = rms_norm(q_m.reshape(b_, s_, MEM_HEADS, MEM_HEAD_DIM), m_q_gain[l])
        o_m = memory_attention(q_m, m_k, m_v) @ w_o_m[l]
        g = jax.nn.sigmoid(gates.reshape(b_, s_, N_BRANCHES, D_MODEL))
        merged = g[:, :, 0] * o_a + g[:, :, 1] * o_b + g[:, :, 2] * o_m
        x = x + merged @ w_out[l]
        x = x + 0.5 * swiglu_ffn(rms_norm(x, ffn2_norm[l]), ffn2_w_in[l], ffn2_w_out[l])
        x = rms_norm(x, final_norm[l])
    return x
```

```python
import math
from contextlib import ExitStack
import numpy as np
import concourse.bass as bass
import concourse.mybir as mybir
from concourse.bass_utils import run_bass_kernel_spmd

F32 = mybir.dt.float32
BF16 = mybir.dt.bfloat16
I32 = mybir.dt.int32
AF = mybir.ActivationFunctionType
ALU = mybir.AluOpType

D = 1024
KC = 8
FFN = 2816
FC = 22
WIN = 4548
EPS = 1e-6
NEG = 30000.0
BIG = 1.0e30
SLOT = 6144
NITER = 22


class Tok:
    __slots__ = ("sem", "val")

    def __init__(self, sem, val):
        self.sem = sem
        self.val = val


class Sem:
    def __init__(self, h):
        self.h = h
        self.n = 0


class Buf:
    def __init__(self, name):
        self.name = name
        self.w = None
        self.r = {}


class Eng:
    def __init__(self, eng, sem):
        self.eng = eng
        self.sem = sem
        self.waited = {}

    def wait(self, tok):
        if tok is None:
            return
        k = id(tok.sem)
        if self.waited.get(k, 0) >= tok.val:
            return
        self.eng.wait_ge(tok.sem.h, tok.val)
        self.waited[k] = tok.val


class Ctx:
    def __init__(self, nc, es):
        self.nc = nc
        self.es = es
        self.PE = Eng(nc.tensor, self.sem("pe"))
        self.ACT = Eng(nc.scalar, self.sem("act"))
        self.DVE = Eng(nc.vector, self.sem("dve"))
        self.POOL = Eng(nc.gpsimd, self.sem("pool"))
        self.SP = Eng(nc.sync, self.sem("sp"))

    def sem(self, name):
        return Sem(self.es.enter_context(self.nc.semaphore(name)))

    def _deps(self, E, reads, writes):
        for b in reads:
            E.wait(b.w)
        for b in writes:
            if b.w is not None and b.w.sem is not E.sem:
                E.wait(b.w)
            for t in b.r.values():
                if t.sem is not E.sem:
                    E.wait(t)

    def _commit(self, tok, reads, writes):
        for b in reads:
            b.r[id(tok.sem)] = tok
        for b in writes:
            b.w = tok
            b.r = {}

    def op(self, E, fn, reads=(), writes=()):
        self._deps(E, reads, writes)
        ins = fn()
        E.sem.n += 1
        ins.then_inc(E.sem.h, 1)
        tok = Tok(E.sem, E.sem.n)
        self._commit(tok, reads, writes)
        return tok

    def dma(self, Q, out, in_, sem, reads=(), writes=(), **kw):
        self._deps(Q, reads, writes)
        ins = Q.eng.dma_start(out=out, in_=in_, **kw)
        sem.n += 16
        ins.then_inc(sem.h, 16)
        tok = Tok(sem, sem.n)
        self._commit(tok, reads, writes)
        return tok

    def barrier(self, bufs):
        for E in (self.PE, self.ACT, self.DVE, self.POOL, self.SP):
            for b in bufs:
                E.wait(b.w)
                for t in b.r.values():
                    E.wait(t)


def _t5_bucket_np(rel):
    half = 16
    max_exact = 8
    n = np.abs(rel)
    nf = np.maximum(n, 1).astype(np.float32)
    large = max_exact + (np.log(nf / np.float32(max_exact)) / np.float32(math.log(128 / 8))
                         * np.float32(half - max_exact)).astype(np.int32)
    large = np.minimum(large, half - 1)
    return np.where(rel > 0, half, 0) + np.where(n < max_exact, n, large)


def _onehot_table():
    rel = np.arange(-255, 129, dtype=np.int32)
    bk = _t5_bucket_np(rel)
    oh = np.zeros((32, 384), np.float32)
    oh[bk, np.arange(384)] = 1.0
    return oh


WNAMES = ["ffn1_norm", "ffn1_w_in", "ffn1_w_out", "mix_norm", "w_in", "a_q_gain", "a_k_gain", "rel_bias",
          "w_o_a", "ssm_lambda_re", "ssm_lambda_im", "ssm_log_dt", "ssm_b_re", "ssm_b_im", "ssm_c_re",
          "ssm_c_im", "ssm_d", "w_glu", "mem_norm", "w_mem_kv", "m_q_gain", "m_k_gain", "w_o_m", "w_out",
          "ffn2_norm", "ffn2_w_in", "ffn2_w_out", "final_norm"]
WSHAPES = {"ffn1_norm": [1, D], "ffn1_w_in": [D, 2 * FFN], "ffn1_w_out": [FFN, D], "mix_norm": [1, D],
           "w_in": [D, WIN], "a_q_gain": [64, 1], "a_k_gain": [64, 1], "rel_bias": [32, 8],
           "w_o_a": [512, D], "ssm_lambda_re": [16, 64], "ssm_lambda_im": [16, 64], "ssm_log_dt": [1, 16],
           "ssm_b_re": [16, 64, 16], "ssm_b_im": [16, 64, 16], "ssm_c_re": [16, 16, 64],
           "ssm_c_im": [16, 16, 64], "ssm_d": [256, 1], "w_glu": [256, 2 * D], "mem_norm": [1, D],
           "w_mem_kv": [D, 512], "m_q_gain": [64, 1], "m_k_gain": [64, 1], "w_o_m": [256, D],
           "w_out": [D, D], "ffn2_norm": [1, D], "ffn2_w_in": [D, 2 * FFN], "ffn2_w_out": [FFN, D],
           "final_norm": [1, D]}


def build(NB, S, dbg=()):
    NT = S // 512
    TOPK = min(256, S // 4)
    nc = bass.Bass("TRN2", target_bir_lowering=False)
    x_d = nc.dram_tensor("x", [NB * S, D], F32, kind="ExternalInput").ap()
    mem_d = nc.dram_tensor("mem", [NB * 256, D], F32, kind="ExternalInput").ap()
    W = {n: nc.dram_tensor(n, WSHAPES[n], F32, kind="ExternalInput").ap() for n in WNAMES}
    oh_d = nc.dram_tensor("onehot", [32, 384], F32, kind="ExternalInput").ap()
    out_d = nc.dram_tensor("out", [NB * S, D], F32, kind="ExternalOutput").ap()
    dbg_d = {n: nc.dram_tensor("dbg_" + n, shp, F32, kind="ExternalOutput").ap() for n, shp in dbg}

    pieces = []

    def ffn_pieces(tag, win, wout):
        wi4 = win.rearrange("(kc p) (gu f) -> p kc gu f", p=128, gu=2)
        for i in range(11):
            srcs = [(0, 128, gu * 2048, [8, 256], wi4[:, :, gu, 256 * i:256 * i + 256]) for gu in range(2)]
            pieces.append(((tag, "in", i), srcs))
        wo3 = wout.rearrange("(f p) d -> p f d", p=128)
        for hh in range(2):
            for pp in range(2):
                pieces.append(((tag, "out", hh, pp),
                               [(0, 128, 0, [11, 512], wo3[:, 11 * pp:11 * pp + 11, 512 * hh:512 * hh + 512])]))

    win3 = W["w_in"].rearrange("(kc p) c -> p kc c", p=128)
    ffn_pieces("f1", W["ffn1_w_in"], W["ffn1_w_out"])
    pieces.append((("p1",), [(0, 128, 0, [8, 512], win3[:, :, 0:512])]))
    pieces.append((("p2",), [(0, 128, 0, [8, 452], win3[:, :, 512:964])]))
    pieces.append((("p3",), [(0, 128, 0, [8, 512], win3[:, :, 964:1476])]))
    woa3 = W["w_o_a"].rearrange("(h p) d -> p h d", p=64)
    wglu3 = W["w_glu"].rearrange("(kc p) d -> p kc d", p=128)
    wom3 = W["w_o_m"].rearrange("(h p) d -> p h d", p=64)
    for f in range(8):
        srcs = []
        for br in range(3):
            c0 = 1476 + br * 1024 + f * 128
            srcs.append((0, 128, br * 1024, [8, 128], win3[:, :, c0:c0 + 128]))
        srcs.append((0, 64, 3072, [8, 128], woa3[:, :, f * 128:f * 128 + 128]))
        for ab in range(2):
            srcs.append((0, 128, 4096 + ab * 256, [2, 128], wglu3[:, :, ab * 1024 + f * 128:ab * 1024 + f * 128 + 128]))
        srcs.append((0, 64, 4608, [4, 128], wom3[:, :, f * 128:f * 128 + 128]))
        pieces.append((("mg", f), srcs))
    wout3 = W["w_out"].rearrange("(kc p) d -> p kc d", p=128)
    for hh in range(2):
        pieces.append((("wo", hh), [(0, 128, 0, [8, 512], wout3[:, :, 512 * hh:512 * hh + 512])]))
    ffn_pieces("f2", W["ffn2_w_in"], W["ffn2_w_out"])
    wkv3 = W["w_mem_kv"].rearrange("(kc p) d -> p kc d", p=128)
    pieces.append((("kv",), [(0, 128, 0, [8, 512], wkv3[:, :, :])]))
    pidx = {k: i for i, (k, _) in enumerate(pieces)}
    NP = len(pieces)
    wscr = nc.dram_tensor("wscr", [NP, 128, SLOT], BF16, kind="Internal").ap()
    fd_d = nc.dram_tensor("fdscr", [8, 384], F32, kind="Internal").ap()

    with ExitStack() as es:
        c = Ctx(nc, es)
        PE, ACT, DVE, POOL, SP = c.PE, c.ACT, c.DVE, c.POOL, c.SP
        V, A, G, T = nc.vector, nc.scalar, nc.gpsimd, nc.tensor

        def sb(name, shape, dt):
            return es.enter_context(nc.sbuf_tensor(name, shape, dt))

        wscr_b = Buf("wscr")
        sem_pro = c.sem("pro")
        sem_const = c.sem("const")
        sem_out = c.sem("outst")
        sem_x = c.sem("xld")
        sem_misc = c.sem("misc")

        with ExitStack() as es2:
            stg32 = [es2.enter_context(nc.sbuf_tensor("stg32_%d" % i, [128, SLOT], F32)) for i in range(2)]
            stg16 = [es2.enter_context(nc.sbuf_tensor("stg16_%d" % i, [128, SLOT], BF16)) for i in range(2)]
            b32 = [Buf("s32a"), Buf("s32b")]
            b16 = [Buf("s16a"), Buf("s16b")]
            s32sem = [c.sem("s32a"), c.sem("s32b")]
            for i in range(2):
                c.op(POOL, lambda i=i: G.memset(stg32[i][:], 0.0), writes=[b32[i]])
            for k, (key, srcs) in enumerate(pieces):
                j = k % 2
                n = 0
                for (p0, p1, off, shp, src) in srcs:
                    sz = shp[0] * shp[1]
                    dst = stg32[j][p0:p1, off:off + sz].rearrange("p (a b) -> p a b", a=shp[0])
                    c.dma(SP, dst, src, s32sem[j], writes=[b32[j]])
                    n = max(n, off + sz)
                E = (ACT, DVE, POOL)[k % 3]
                if E is ACT:
                    c.op(ACT, lambda j=j, n=n: A.copy(out=stg16[j][:, 0:n], in_=stg32[j][:, 0:n]),
                         reads=[b32[j]], writes=[b16[j]])
                elif E is DVE:
                    c.op(DVE, lambda j=j, n=n: V.tensor_copy(stg16[j][:, 0:n], stg32[j][:, 0:n]),
                         reads=[b32[j]], writes=[b16[j]])
                else:
                    c.op(POOL, lambda j=j, n=n: G.tensor_copy(stg16[j][:, 0:n], stg32[j][:, 0:n]),
                         reads=[b32[j]], writes=[b16[j]])
                c.dma(SP, wscr[k, :, 0:n], stg16[j][:, 0:n], sem_pro, reads=[b16[j]], writes=[wscr_b])
            c.barrier(b32 + b16)

        ident_f = sb("ident_f", [128, 128], F32)
        ident = sb("ident", [128, 128], BF16)
        antiJ = sb("antiJ", [128, 128], BF16)
        I8big = sb("I8big", [128, 4, 128], BF16)
        ones = sb("ones", [128, 128], BF16)
        epsc = sb("epsc", [128, 1], F32)
        gcol = sb("gcol", [128, 4, KC], F32)
        gfin = sb("gfin", [128, D], F32)
        hg = sb("hg", [64, 4], F32)
        biasT = sb("biasT", [128, 2, 8, 128], BF16)
        cst_b = Buf("const")
        cosT = sb("cosT", [128, 8, 128], F32); sinT = sb("sinT", [128, 8, 128], F32)
        decT = sb("decT", [128, 8, 128], F32)
        BpT = sb("BpT", [128, 2, 8, 128], BF16)
        Cp = sb("Cp", [128, 2, 8, 128], BF16)
        ssmc = sb("ssmc", [128, 12, 8], F32)
        dcol = sb("dcol", [128, 2], F32)

        banks = [es.enter_context(nc.psum_tensor("bank%d" % i, [128, 512], F32)) for i in range(7)]
        bank_b = [Buf("bank%d" % i) for i in range(7)]
        psT = es.enter_context(nc.psum_tensor("psT", [128, 1024], BF16)); psT_b = Buf("psT")
        pinned = set()
        rr = [0]

        def nb():
            while True:
                i = rr[0] % 7
                rr[0] += 1
                if i not in pinned:
                    return i

        def pin():
            i = nb()
            pinned.add(i)
            return i

        def unpin(i):
            pinned.discard(i)

        def mm_group(out_ap, pairs, reads, writes, first=True, last=True):
            def fn():
                ins = None
                n = len(pairs)
                for i, (l, r) in enumerate(pairs):
                    ins = T.matmul(out_ap, l, r, start=(first and i == 0), stop=(last and i == n - 1))
                return ins
            return c.op(PE, fn, reads=reads, writes=writes)

        def dump(name, ap_sb, buf, dst):
            if name in dbg_d:
                c.dma(POOL, dst, ap_sb, sem_misc, reads=[buf])

        def cdma(out, in_, **kw):
            c.dma(SP, out, in_, sem_const, writes=[cst_b], **kw)

        for gi, nm in enumerate(["ffn1_norm", "mix_norm", "ffn2_norm", "mem_norm"]):
            cdma(gcol[:, gi, :], W[nm].rearrange("o (c p) -> p (o c)", p=128), allow_slow_non_contiguous=True)
        cdma(gfin[:], W["final_norm"][0:1, :].partition_broadcast(128))
        for gi, nm in enumerate(["a_q_gain", "a_k_gain", "m_q_gain", "m_k_gain"]):
            cdma(hg[:, gi:gi + 1], W[nm][:, :])
        es3 = ExitStack()
        sb_main = sb

        def sb(name, shape, dt):
            return es3.enter_context(nc.sbuf_tensor(name, shape, dt))
        rb_sb = sb("rb_sb", [32, 8], F32); oh_sb = sb("oh_sb", [32, 384], F32); rb15 = sb("rb15", [8, 1], F32)
        fsb = sb("fsb", [8, 384], F32); hank = sb("hank", [128, 2, 8, 128], F32); hankb = sb("hankb", [128, 2, 8, 128], BF16)
        cdma(rb_sb[:], W["rel_bias"][:, :])
        cdma(oh_sb[:], oh_d[:, :])
        cdma(rb15[:], W["rel_bias"][15:16, :].rearrange("o h -> h o"), allow_slow_non_contiguous=True)
        for t in range(2):
            cdma(ssmc[t * 64:(t + 1) * 64, 0, :], W["ssm_lambda_re"].rearrange("(c t) s -> t s c", t=2)[t], allow_slow_non_contiguous=True)
            cdma(ssmc[t * 64:(t + 1) * 64, 1, :], W["ssm_lambda_im"].rearrange("(c t) s -> t s c", t=2)[t], allow_slow_non_contiguous=True)
            cdma(ssmc[t * 64:(t + 1) * 64, 2, :], W["ssm_log_dt"].rearrange("o (c t) -> t o c", t=2)[t].partition_broadcast(64), allow_slow_non_contiguous=True)
        braw = sb("braw", [128, 2, 8, 16], F32); craw = sb("craw", [128, 2, 8, 16], F32)
        for t in range(2):
            cdma(braw[t * 64:(t + 1) * 64, 0, :, :], W["ssm_b_re"].rearrange("(c t) s h -> t s c h", t=2)[t])
            cdma(braw[t * 64:(t + 1) * 64, 1, :, :], W["ssm_b_im"].rearrange("(c t) s h -> t s c h", t=2)[t])
            for cc in range(8):
                cdma(craw[t * 64:(t + 1) * 64, 0, cc, :], W["ssm_c_re"].rearrange("(c t) o s -> t c s o", t=2)[t, cc], allow_slow_non_contiguous=True)
                cdma(craw[t * 64:(t + 1) * 64, 1, cc, :], W["ssm_c_im"].rearrange("(c t) o s -> t c s o", t=2)[t, cc], allow_slow_non_contiguous=True)
        cdma(dcol[:, :], W["ssm_d"].rearrange("(m p) o -> p (m o)", p=128), allow_slow_non_contiguous=True)

        def cop(E, fn):
            return c.op(E, fn, reads=[cst_b], writes=[cst_b])

        cop(POOL, lambda: G.memset(ident_f[:], 1.0))
        cop(POOL, lambda: G.affine_select(out=ident_f[:], in_=ident_f[:], pattern=[[-1, 128]], compare_op=ALU.is_equal,
                                          fill=0.0, base=0, channel_multiplier=1))
        cop(DVE, lambda: V.tensor_copy(ident[:], ident_f[:]))
        for i in range(4):
            cop(DVE, lambda i=i: V.tensor_scalar(out=I8big[:, i, :], in0=ident_f[:], scalar1=NEG, scalar2=None, op0=ALU.mult))
        cop(POOL, lambda: G.memset(ident_f[:], 1.0))
        cop(POOL, lambda: G.affine_select(out=ident_f[:], in_=ident_f[:], pattern=[[1, 128]], compare_op=ALU.is_equal,
                                          fill=0.0, base=-127, channel_multiplier=1))
        cop(DVE, lambda: V.tensor_copy(antiJ[:], ident_f[:]))
        cop(DVE, lambda: V.memset(ones[:], 1.0))
        cop(DVE, lambda: V.memset(epsc[:], EPS))
        cop(DVE, lambda: V.tensor_scalar(out=hg[:, 0:1], in0=hg[:, 0:1], scalar1=0.125, scalar2=None, op0=ALU.mult))
        cop(DVE, lambda: V.tensor_scalar(out=hg[:, 2:3], in0=hg[:, 2:3], scalar1=0.125, scalar2=None, op0=ALU.mult))
        b0 = 0
        c.op(PE, lambda: T.matmul(banks[b0][0:8, 0:384], rb_sb[:, :], oh_sb[:, :], start=True, stop=True),
             reads=[cst_b], writes=[bank_b[b0]])
        c.op(DVE, lambda: V.tensor_scalar(out=fsb[:], in0=banks[b0][0:8, 0:384], scalar1=rb15[:, 0:1], scalar2=None,
                                          op0=ALU.subtract), reads=[bank_b[b0], cst_b], writes=[cst_b])
        fd_b = Buf("fd")
        c.dma(SP, fd_d[:, :], fsb[:], sem_const, reads=[cst_b], writes=[fd_b])
        for di, delta in enumerate((0, 128)):
            src = bass.AP(tensor=fd_d.tensor, offset=128 - delta, ap=[[1, 128], [384, 8], [1, 128]])
            c.dma(SP, hank[:, di, :, :], src, sem_const, reads=[fd_b], writes=[cst_b])
        cop(DVE, lambda: V.tensor_copy(hankb[:], hank[:]))
        for di in range(2):
            for h in range(8):
                bi = nb()
                c.op(PE, lambda di=di, h=h, bi=bi: T.matmul(banks[bi][:, 0:128], hankb[:, di, h, :], antiJ[:], start=True, stop=True),
                     reads=[cst_b], writes=[bank_b[bi]])
                c.op(DVE, lambda di=di, h=h, bi=bi: V.tensor_copy(biasT[:, di, h, :], banks[bi][:, 0:128]),
                     reads=[bank_b[bi], cst_b], writes=[cst_b])

        LR, LI, LDT, DT, MAG, TH, COS, SIN, FR, FI, T1, T2 = range(12)
        sc = lambda i: ssmc[:, i, :]
        cop(ACT, lambda: A.activation(out=sc(DT), in_=sc(LDT), func=AF.Exp))
        cop(DVE, lambda: V.tensor_tensor(out=sc(T1), in0=sc(LR), in1=sc(DT), op=ALU.mult))
        cop(ACT, lambda: A.activation(out=sc(MAG), in_=sc(T1), func=AF.Exp))
        cop(DVE, lambda: V.tensor_tensor(out=sc(TH), in0=sc(LI), in1=sc(DT), op=ALU.mult))
        MAGIC = 12582912.0
        TWO_PI = 2.0 * math.pi

        def sin_of(out_ap, in_ap, tmp_ap, shift):
            cop(DVE, lambda: V.tensor_scalar(out=tmp_ap, in0=in_ap, scalar1=shift, scalar2=1.0 / TWO_PI, op0=ALU.add, op1=ALU.mult))
            cop(DVE, lambda: V.tensor_scalar(out=tmp_ap, in0=tmp_ap, scalar1=MAGIC, scalar2=None, op0=ALU.add))
            cop(DVE, lambda: V.tensor_scalar(out=tmp_ap, in0=tmp_ap, scalar1=-MAGIC, scalar2=-TWO_PI, op0=ALU.add, op1=ALU.mult))
            cop(DVE, lambda: V.scalar_tensor_tensor(out=tmp_ap, in0=in_ap, scalar=shift, in1=tmp_ap, op0=ALU.add, op1=ALU.add))
            cop(DVE, lambda: V.tensor_scalar(out=tmp_ap, in0=tmp_ap, scalar1=math.pi, scalar2=-math.pi, op0=ALU.min, op1=ALU.max))
            cop(ACT, lambda: A.activation(out=out_ap, in_=tmp_ap, func=AF.Sin))

        sin_of(sc(SIN), sc(TH), sc(T1), 0.0)
        sin_of(sc(COS), sc(TH), sc(T1), math.pi / 2)
        arr = sb("arr", [128, 6, 8], F32)
        a_ = lambda i: arr[:, i, :]
        cop(DVE, lambda: V.tensor_tensor(out=a_(0), in0=sc(MAG), in1=sc(COS), op=ALU.mult))
        cop(DVE, lambda: V.tensor_scalar(out=a_(0), in0=a_(0), scalar1=-1.0, scalar2=None, op0=ALU.add))
        cop(DVE, lambda: V.tensor_tensor(out=a_(1), in0=sc(MAG), in1=sc(SIN), op=ALU.mult))
        cop(DVE, lambda: V.tensor_tensor(out=a_(2), in0=sc(LR), in1=sc(LR), op=ALU.mult))
        cop(DVE, lambda: V.tensor_tensor(out=a_(3), in0=sc(LI), in1=sc(LI), op=ALU.mult))
        cop(DVE, lambda: V.tensor_tensor(out=a_(2), in0=a_(2), in1=a_(3), op=ALU.add))
        cop(DVE, lambda: V.reciprocal(out=a_(5), in_=a_(2)))
        cop(DVE, lambda: V.tensor_tensor(out=a_(3), in0=a_(0), in1=sc(LR), op=ALU.mult))
        cop(DVE, lambda: V.tensor_tensor(out=a_(4), in0=a_(1), in1=sc(LI), op=ALU.mult))
        cop(DVE, lambda: V.tensor_tensor(out=a_(3), in0=a_(3), in1=a_(4), op=ALU.add))
        cop(DVE, lambda: V.tensor_tensor(out=sc(FR), in0=a_(3), in1=a_(5), op=ALU.mult))
        cop(DVE, lambda: V.tensor_tensor(out=a_(3), in0=a_(1), in1=sc(LR), op=ALU.mult))
        cop(DVE, lambda: V.tensor_tensor(out=a_(4), in0=a_(0), in1=sc(LI), op=ALU.mult))
        cop(DVE, lambda: V.tensor_tensor(out=a_(3), in0=a_(3), in1=a_(4), op=ALU.subtract))
        cop(DVE, lambda: V.tensor_tensor(out=sc(FI), in0=a_(3), in1=a_(5), op=ALU.mult))
        bbar = sb("bbar", [128, 2, 8, 16], F32); btmp = sb("btmp", [128, 8, 16], F32)
        frb = ssmc[:, FR, :].unsqueeze(2).to_broadcast([128, 8, 16])
        fib = ssmc[:, FI, :].unsqueeze(2).to_broadcast([128, 8, 16])
        cop(DVE, lambda: V.tensor_tensor(out=bbar[:, 0, :, :], in0=braw[:, 0, :, :], in1=frb, op=ALU.mult))
        cop(DVE, lambda: V.tensor_tensor(out=btmp[:], in0=braw[:, 1, :, :], in1=fib, op=ALU.mult))
        cop(DVE, lambda: V.tensor_tensor(out=bbar[:, 0, :, :], in0=bbar[:, 0, :, :], in1=btmp[:], op=ALU.subtract))
        cop(DVE, lambda: V.tensor_tensor(out=bbar[:, 1, :, :], in0=braw[:, 1, :, :], in1=frb, op=ALU.mult))
        cop(DVE, lambda: V.tensor_tensor(out=btmp[:], in0=braw[:, 0, :, :], in1=fib, op=ALU.mult))
        cop(DVE, lambda: V.tensor_tensor(out=bbar[:, 1, :, :], in0=bbar[:, 1, :, :], in1=btmp[:], op=ALU.add))
        xpad = sb("xpad", [128, 128], BF16)
        for ri in range(2):
            for cc in range(8):
                cop(DVE, lambda: V.memset(xpad[:], 0.0))
                for t in range(2):
                    col = 32 * (cc % 4) + 16 * t
                    cop(DVE, lambda ri=ri, cc=cc, t=t, col=col: V.tensor_copy(xpad[t * 64:(t + 1) * 64, col:col + 16],
                                                                               bbar[t * 64:(t + 1) * 64, ri, cc, :]))
                c.op(PE, lambda: T.transpose(psT[:, 0:128], xpad[:], ident[:]), reads=[cst_b], writes=[psT_b])
                c.op(DVE, lambda ri=ri, cc=cc: V.tensor_copy(BpT[:, ri, cc, :], psT[:, 0:128]), reads=[psT_b, cst_b], writes=[cst_b])
        cop(DVE, lambda: V.memset(Cp[:], 0.0))
        for ri in range(2):
            for cc in range(8):
                for t in range(2):
                    col = 16 * ((2 * cc + t) % 8)
                    cop(DVE, lambda ri=ri, cc=cc, t=t, col=col: V.tensor_scalar(
                        out=Cp[t * 64:(t + 1) * 64, ri, cc, col:col + 16], in0=craw[t * 64:(t + 1) * 64, ri, cc, :],
                        scalar1=(1.0 if ri == 0 else -1.0), scalar2=None, op0=ALU.mult))
        tau_i = sb("tau_i", [128, 128], I32); tau = sb("tau", [128, 128], F32); ang = sb("ang", [128, 8, 128], F32)
        atmp = sb("atmp", [128, 8, 128], F32)
        cop(POOL, lambda: G.iota(tau_i[:], pattern=[[1, 128]], base=0, channel_multiplier=0))
        cop(DVE, lambda: V.tensor_copy(tau[:], tau_i[:]))
        for cc in range(8):
            cop(DVE, lambda cc=cc: V.tensor_scalar(out=ang[:, cc, :], in0=tau[:], scalar1=ssmc[:, TH, cc:cc + 1], scalar2=None, op0=ALU.mult))
        sin_of(sinT[:], ang[:], atmp[:], 0.0)
        sin_of(cosT[:], ang[:], atmp[:], math.pi / 2)
        cop(DVE, lambda: V.tensor_copy(decT[:], ssmc[:, MAG, :].unsqueeze(2).to_broadcast([128, 8, 128])))
        cop(DVE, lambda: V.memset(decT[:, :, 0:1], 0.0))
        c.barrier([cst_b])
        es3.close()
        sb = sb_main
        ring = [sb("ring%d" % i, [128, SLOT], BF16) for i in range(3)]
        ring_b = [Buf("ring%d" % i) for i in range(3)]
        ring_s = [c.sem("ring%d" % i) for i in range(3)]
        ring_ctr = [0]

        def piece(key):
            k = pidx[key]
            j = ring_ctr[0] % 3
            ring_ctr[0] += 1
            n = max(off + shp[0] * shp[1] for (_, _, off, shp, _) in pieces[k][1])
            c.dma(SP, ring[j][:, 0:n], wscr[k, :, 0:n], ring_s[j], reads=[wscr_b], writes=[ring_b[j]])
            return ring[j], ring_b[j]

        xt = sb("xt", [128, 4, D], F32); xt_b = Buf("xt")
        hT = sb("hT", [128, KC, 512], BF16); hT_b = Buf("hT")
        xs = sb("xs", [128, D], BF16); xs_b = Buf("xs")
        ss = sb("ss", [128, 8], F32); ss_b = Buf("ss")
        arena = sb("arena", [128, max(4 * S, FC * 256)], F32)
        actT = arena[:, 0:FC * 256].bitcast(BF16).rearrange("p (f n) -> p f n", f=FC); actT_b = Buf("actT")
        kT = sb("kT", [64, S], BF16); kT_b = Buf("kT")
        kiT = sb("kiT", [64, S], BF16); kiT_b = Buf("kiT")
        Vaug = sb("Vaug", [128, S // 128, 128], BF16); Vaug_b = Buf("Vaug")
        qaT = sb("qaT", [64, 8, 512], BF16); qaT_b = Buf("qaT")
        qiT = sb("qiT", [64, 4, 512], BF16); qiT_b = Buf("qiT")
        qmT = sb("qmT", [64, 4, 512], BF16); qmT_b = Buf("qmT")
        uT = sb("uT", [128, 2, 512], BF16); uT_b = Buf("uT")
        wq = sb("wq", [128, 4, 4], F32); wq_b = Buf("wq")
        Isc = [arena[:, i * S:(i + 1) * S] for i in range(4)]; Isc_b = [Buf("Isc%d" % i) for i in range(4)]
        MB = sb("MB", [128, S], BF16); MB_b = Buf("MB")
        ET = [sb("ET%d" % i, [128, 512], BF16) for i in range(2)]; ET_b = [Buf("ET%d" % i) for i in range(2)]
        rec = sb("rec", [128, 512], F32); rec_b = Buf("rec")
        OaT = sb("OaT", [64, 8, 512], BF16); OaT_b = Buf("OaT")
        OmT = sb("OmT", [64, 4, 512], BF16); OmT_b = Buf("OmT")
        ysT = sb("ysT", [128, 2, 512], BF16); ysT_b = Buf("ysT")
        mgT = sb("mgT", [128, 8, 512], BF16); mgT_b = Buf("mgT")
        junk = mgT[:].rearrange("p f n -> p (f n)"); junk_b = mgT_b
        mkT = sb("mkT", [64, 4, 256], BF16); mkT_b = Buf("mkT")
        Vm = sb("Vm", [128, 2, 4, 128], BF16); Vm_b = Buf("Vm")
        memT = hT[:, :, 0:256]; memT_b = hT_b
        blo = sb("blo", [128, 4], F32); bw = sb("bw", [128, 4], F32); bmid = sb("bmid", [128, 4], F32)
        bcnt = sb("bcnt", [128, 4], F32); bpw = sb("bpw", [128, 4], F32); bmx = sb("bmx", [128, 4], F32)
        blo_b, bw_b, bmid_b, bcnt_b, bpw_b, bmx_b = (Buf(n) for n in ("blo", "bw", "bmid", "bcnt", "bpw", "bmx"))
        hsq = sb("hsq", [64, 512], BF16); hsq_b = Buf("hsq")
        hrs = sb("hrs", [64, 512], F32); hrs_b = Buf("hrs")
        s_init = sb("s_init", [128, 2, 8], F32); s_init_b = Buf("s_init")
        stmp = [sb("stmp%d" % i, [128, 512], F32) for i in range(6)]; stmp_b = [Buf("stmp%d" % i) for i in range(6)]
        xrb = sb("xrb", [128, 2, 512], BF16); xrb_b = Buf("xrb")
        ssm_small = sb("ssm_small", [128, 4, 4], F32); ssm_small_b = Buf("ssm_small")
        yf = sb("yf", [128, 128], F32); yf_b = Buf("yf")
        sg = [stmp[3], stmp[4]]; sg_b = [stmp_b[3], stmp_b[4]]
        rl = [stmp[4], stmp[5]]; rl_b = [stmp_b[4], stmp_b[5]]
        c.op(DVE, lambda: V.memset(Vaug[:], 1.0), writes=[Vaug_b])
        c.op(DVE, lambda: V.memset(Vm[:], 1.0), writes=[Vm_b])

        def rms_to_hT(x_ap_fn, gi, dst, dst_b, ntile, src_b):
            for t in range(ntile):
                xa = x_ap_fn(t)
                c.op(ACT, lambda xa=xa, t=t: A.activation(out=junk[:, 0:D], in_=xa, func=AF.Square, accum_out=ss[:, t:t + 1]),
                     reads=[src_b], writes=[junk_b, ss_b])
                c.op(ACT, lambda t=t: A.activation(out=ss[:, 4 + t:5 + t], in_=ss[:, t:t + 1], func=AF.Sqrt, bias=epsc[:, 0:1], scale=1.0 / D),
                     reads=[ss_b], writes=[ss_b])
                c.op(DVE, lambda t=t: V.reciprocal(out=ss[:, 4 + t:5 + t], in_=ss[:, 4 + t:5 + t]), reads=[ss_b], writes=[ss_b])
                c.op(DVE, lambda xa=xa, t=t: V.tensor_scalar(out=xs[:], in0=xa, scalar1=ss[:, 4 + t:5 + t], scalar2=None, op0=ALU.mult),
                     reads=[src_b, ss_b], writes=[xs_b])

                def tr():
                    ins = None
                    for kc in range(KC):
                        ins = T.transpose(psT[:, kc * 128:(kc + 1) * 128], xs[:, kc * 128:(kc + 1) * 128], ident[:])
                    return ins
                c.op(PE, tr, reads=[xs_b], writes=[psT_b])
                c.op(DVE, lambda t=t: V.tensor_tensor(out=dst[:, :, t * 128:(t + 1) * 128],
                                                      in0=psT[:].rearrange("p (c n) -> p c n", c=KC),
                                                      in1=gcol[:, gi, :].unsqueeze(2).to_broadcast([128, KC, 128]), op=ALU.mult),
                     reads=[psT_b], writes=[dst_b])

        def head_norm(bi, N, gidx, out_ap, out_b):
            c.op(ACT, lambda: A.activation(out=hsq[:, 0:N], in_=banks[bi][0:64, 0:N], func=AF.Square), reads=[bank_b[bi]], writes=[hsq_b])
            b2 = nb()
            mm_group(banks[b2][0:64, 0:N], [(ones[0:64, 0:64], hsq[:, 0:N])], reads=[hsq_b], writes=[bank_b[b2]])
            c.op(ACT, lambda: A.activation(out=hrs[:, 0:N], in_=banks[b2][0:64, 0:N], func=AF.Sqrt, bias=epsc[0:64, 0:1], scale=1.0 / 64),
                 reads=[bank_b[b2]], writes=[hrs_b])
            c.op(DVE, lambda: V.reciprocal(out=hrs[:, 0:N], in_=hrs[:, 0:N]), reads=[hrs_b], writes=[hrs_b])
            c.op(DVE, lambda: V.scalar_tensor_tensor(out=out_ap, in0=banks[bi][0:64, 0:N], scalar=hg[:, gidx:gidx + 1], in1=hrs[:, 0:N],
                                                     op0=ALU.mult, op1=ALU.mult), reads=[bank_b[bi], hrs_b], writes=[out_b])

        def ffn(tag, gi):
            c.barrier(Isc_b)
            rms_to_hT(lambda t: xt[:, t, :], gi, hT, hT_b, 4, xt_b)
            for i in range(11):
                wt, wb = piece((tag, "in", i))
                w4 = wt[:, 0:4096].rearrange("p (gu kc f) -> p gu kc f", gu=2, kc=8)
                for j in range(2):
                    f = 2 * i + j
                    bg, bu = nb(), nb()
                    mm_group(banks[bg][:, :], [(w4[:, 0, kc, j * 128:(j + 1) * 128], hT[:, kc, :]) for kc in range(KC)],
                             reads=[wb, hT_b], writes=[bank_b[bg]])
                    mm_group(banks[bu][:, :], [(w4[:, 1, kc, j * 128:(j + 1) * 128], hT[:, kc, :]) for kc in range(KC)],
                             reads=[wb, hT_b], writes=[bank_b[bu]])
                    si = f % 2
                    c.op(ACT, lambda bg=bg, si=si: A.activation(out=sg[si][:], in_=banks[bg][:, :], func=AF.Silu),
                         reads=[bank_b[bg]], writes=[sg_b[si]])
                    c.op(DVE, lambda bu=bu, si=si, f=f: V.tensor_tensor(out=actT[:, f, :], in0=sg[si][:], in1=banks[bu][:, :], op=ALU.mult),
                         reads=[sg_b[si], bank_b[bu]], writes=[actT_b])
            for hh in range(2):
                acc = [pin() for _ in range(4)]
                for pp in range(2):
                    wt, wb = piece((tag, "out", hh, pp))
                    w3 = wt[:, 0:5632].rearrange("p (f d) -> p f d", f=11)
                    for t in range(4):
                        mm_group(banks[acc[t]][:, :], [(actT[:, 11 * pp + fl, t * 128:(t + 1) * 128], w3[:, fl, :]) for fl in range(11)],
                                 reads=[wb, actT_b], writes=[bank_b[acc[t]]], first=(pp == 0), last=(pp == 1))
                for t in range(4):
                    xsl = xt[:, t, hh * 512:(hh + 1) * 512]
                    c.op(DVE, lambda t=t, xsl=xsl: V.scalar_tensor_tensor(out=xsl, in0=banks[acc[t]][:, :], scalar=0.5, in1=xsl,
                                                                          op0=ALU.mult, op1=ALU.add),
                         reads=[bank_b[acc[t]], xt_b], writes=[xt_b])
                    unpin(acc[t])

        for b in range(NB):
            for mc in range(2):
                c.dma(SP, xt[:, mc, :], mem_d[b * 256 + mc * 128: b * 256 + (mc + 1) * 128, :], sem_x, writes=[xt_b])
            rms_to_hT(lambda t: xt[:, t, :], 3, memT, memT_b, 2, xt_b)
            wt, wb = piece(("kv",))
            wk3 = wt[:, 0:4096].rearrange("p (kc d) -> p kc d", kc=8)
            for h in range(4):
                bi = nb()
                mm_group(banks[bi][0:64, 0:256], [(wk3[:, kc, h * 64:(h + 1) * 64], memT[:, kc, :]) for kc in range(KC)],
                         reads=[wb, memT_b], writes=[bank_b[bi]])
                head_norm(bi, 256, 3, mkT[:, h, :], mkT_b)
            for mc in range(2):
                bi = nb()
                mm_group(banks[bi][:, 0:256], [(memT[:, kc, mc * 128:(mc + 1) * 128], wk3[:, kc, 256:512]) for kc in range(KC)],
                         reads=[wb, memT_b], writes=[bank_b[bi]])
                c.op(DVE, lambda mc=mc, bi=bi: V.tensor_copy(Vm[:, mc, :, 0:64], banks[bi][:, 0:256].rearrange("p (h d) -> p h d", h=4)),
                     reads=[bank_b[bi]], writes=[Vm_b])
            c.op(DVE, lambda: V.memset(s_init[:], 0.0), writes=[s_init_b])

            for st in range(NT):
                row0 = b * S + st * 512
                c.dma(SP, xt[:], x_d[row0:row0 + 512, :].rearrange("(t p) d -> p t d", p=128), sem_x, writes=[xt_b])
                ffn("f1", 0)
                if "x1" in dbg_d and b == 0:
                    dump("x1", xt[:], xt_b, dbg_d["x1"][st * 512:(st + 1) * 512, :].rearrange("(t p) d -> p t d", p=128))
                rms_to_hT(lambda t: xt[:, t, :], 1, hT, hT_b, 4, xt_b)
                wt, wb = piece(("p1",))
                w3 = wt[:, 0:4096].rearrange("p (kc d) -> p kc d", kc=8)
                for h in range(8):
                    bi = nb()
                    mm_group(banks[bi][0:64, :], [(w3[:, kc, h * 64:(h + 1) * 64], hT[:, kc, :]) for kc in range(KC)],
                             reads=[wb, hT_b], writes=[bank_b[bi]])
                    head_norm(bi, 512, 0, qaT[:, h, :], qaT_b)
                wt, wb = piece(("p2",))
                w3 = wt[:, 0:8 * 452].rearrange("p (kc d) -> p kc d", kc=8)
                bi = nb()
                mm_group(banks[bi][0:64, :], [(w3[:, kc, 0:64], hT[:, kc, :]) for kc in range(KC)], reads=[wb, hT_b], writes=[bank_b[bi]])
                head_norm(bi, 512, 1, kT[:, st * 512:(st + 1) * 512], kT_b)
                bi = nb()
                mm_group(banks[bi][0:64, :], [(w3[:, kc, 384:448], hT[:, kc, :]) for kc in range(KC)], reads=[wb, hT_b], writes=[bank_b[bi]])
                c.op(ACT, lambda bi=bi: A.copy(out=kiT[:, st * 512:(st + 1) * 512], in_=banks[bi][0:64, :]), reads=[bank_b[bi]], writes=[kiT_b])
                for h in range(4):
                    bi = nb()
                    mm_group(banks[bi][0:64, :], [(w3[:, kc, 128 + h * 64:192 + h * 64], hT[:, kc, :]) for kc in range(KC)],
                             reads=[wb, hT_b], writes=[bank_b[bi]])
                    c.op(ACT, lambda bi=bi, h=h: A.copy(out=qiT[:, h, :], in_=banks[bi][0:64, :]), reads=[bank_b[bi]], writes=[qiT_b])
                for t in range(4):
                    bi = nb()
                    mm_group(banks[bi][:, 0:64], [(hT[:, kc, t * 128:(t + 1) * 128], w3[:, kc, 64:128]) for kc in range(KC)],
                             reads=[wb, hT_b], writes=[bank_b[bi]])
                    mm_group(banks[bi][:, 64:68], [(hT[:, kc, t * 128:(t + 1) * 128], w3[:, kc, 448:452]) for kc in range(KC)],
                             reads=[wb, hT_b], writes=[bank_b[bi]])
                    c.op(DVE, lambda bi=bi, t=t: V.tensor_copy(Vaug[:, st * 4 + t, 0:64], banks[bi][:, 0:64]), reads=[bank_b[bi]], writes=[Vaug_b])
                    c.op(DVE, lambda bi=bi, t=t: V.tensor_scalar(out=wq[:, t, :], in0=banks[bi][:, 64:68], scalar1=0.0625, scalar2=None, op0=ALU.mult),
                         reads=[bank_b[bi]], writes=[wq_b])
                wt, wb = piece(("p3",))
                w3 = wt[:, 0:4096].rearrange("p (kc d) -> p kc d", kc=8)
                for m in range(2):
                    bi = nb()
                    mm_group(banks[bi][:, :], [(w3[:, kc, m * 128:(m + 1) * 128], hT[:, kc, :]) for kc in range(KC)],
                             reads=[wb, hT_b], writes=[bank_b[bi]])
                    c.op(ACT, lambda bi=bi, m=m: A.copy(out=uT[:, m, :], in_=banks[bi][:, :]), reads=[bank_b[bi]], writes=[uT_b])
                for h in range(4):
                    bi = nb()
                    mm_group(banks[bi][0:64, :], [(w3[:, kc, 256 + h * 64:320 + h * 64], hT[:, kc, :]) for kc in range(KC)],
                             reads=[wb, hT_b], writes=[bank_b[bi]])
                    head_norm(bi, 512, 2, qmT[:, h, :], qmT_b)

                c.barrier([actT_b])
                for t in range(4):
                    j = st * 4 + t
                    L = (j + 1) * 128
                    for kb in range((L + 511) // 512):
                        k0 = kb * 512
                        kn = min(512, L - k0)
                        for h in range(4):
                            bi = nb()
                            mm_group(banks[bi][:, 0:kn], [(qiT[:, h, t * 128:(t + 1) * 128], kiT[:, k0:k0 + kn])],
                                     reads=[qiT_b, kiT_b], writes=[bank_b[bi]])
                            if h == 0:
                                c.op(DVE, lambda bi=bi, t=t, k0=k0, kn=kn: V.tensor_scalar(
                                    out=Isc[t][:, k0:k0 + kn], in0=banks[bi][:, 0:kn], scalar1=0.0, scalar2=wq[:, t, 0:1], op0=ALU.max, op1=ALU.mult),
                                    reads=[bank_b[bi], wq_b], writes=[Isc_b[t]])
                            else:
                                ri = h % 2
                                c.op(ACT, lambda bi=bi, ri=ri, kn=kn: A.activation(out=rl[ri][:, 0:kn], in_=banks[bi][:, 0:kn], func=AF.Relu),
                                     reads=[bank_b[bi]], writes=[rl_b[ri]])
                                c.op(DVE, lambda ri=ri, t=t, h=h, k0=k0, kn=kn: V.scalar_tensor_tensor(
                                    out=Isc[t][:, k0:k0 + kn], in0=rl[ri][:, 0:kn], scalar=wq[:, t, h:h + 1], in1=Isc[t][:, k0:k0 + kn],
                                    op0=ALU.mult, op1=ALU.add), reads=[rl_b[ri], wq_b, Isc_b[t]], writes=[Isc_b[t]])
                    c.op(DVE, lambda t=t, L=L: V.tensor_reduce(out=bmx[:, t:t + 1], in_=Isc[t][:, 0:L], axis=mybir.AxisListType.X, op=ALU.max),
                         reads=[Isc_b[t]], writes=[bmx_b])
                    c.op(DVE, lambda t=t, L=L: V.tensor_reduce(out=blo[:, t:t + 1], in_=Isc[t][:, 0:L], axis=mybir.AxisListType.X, op=ALU.min),
                         reads=[Isc_b[t]], writes=[blo_b])
                    c.op(DVE, lambda t=t, L=L: V.memset(Isc[t][0:64, L - 64:L], -BIG), reads=[Isc_b[t]], writes=[Isc_b[t]])
                need = [((st * 4 + t + 1) * 128 > TOPK) for t in range(4)]
                if any(need):
                    c.op(DVE, lambda: V.tensor_tensor(out=bw[:], in0=bmx[:], in1=blo[:], op=ALU.subtract), reads=[bmx_b, blo_b], writes=[bw_b])
                    c.op(DVE, lambda: V.tensor_scalar(out=bpw[:], in0=bw[:], scalar1=1e-3, scalar2=1e-6, op0=ALU.mult, op1=ALU.add),
                         reads=[bw_b], writes=[bpw_b])
                    c.op(DVE, lambda: V.tensor_tensor(out=blo[:], in0=blo[:], in1=bpw[:], op=ALU.subtract), reads=[bpw_b, blo_b], writes=[blo_b])
                    c.op(DVE, lambda: V.tensor_tensor(out=bw[:], in0=bmx[:], in1=blo[:], op=ALU.subtract), reads=[bmx_b, blo_b], writes=[bw_b])
                    for it in range(NITER):
                        c.op(DVE, lambda: V.tensor_scalar(out=bw[:], in0=bw[:], scalar1=0.5, scalar2=None, op0=ALU.mult), reads=[bw_b], writes=[bw_b])
                        c.op(DVE, lambda: V.tensor_tensor(out=bmid[:], in0=blo[:], in1=bw[:], op=ALU.add), reads=[blo_b, bw_b], writes=[bmid_b])
                        for t in range(4):
                            if not need[t]:
                                continue
                            L = (st * 4 + t + 1) * 128
                            c.op(DVE, lambda t=t, L=L: V.tensor_scalar(out=junk[:, 0:L], in0=Isc[t][:, 0:L], scalar1=bmid[:, t:t + 1], scalar2=0.0,
                                                                        op0=ALU.is_gt, op1=ALU.add, accum_out=bcnt[:, t:t + 1]),
                                 reads=[Isc_b[t], bmid_b], writes=[junk_b, bcnt_b])
                        c.op(DVE, lambda: V.scalar_tensor_tensor(out=bpw[:], in0=bcnt[:], scalar=float(TOPK), in1=bw[:], op0=ALU.is_ge, op1=ALU.mult),
                             reads=[bcnt_b, bw_b], writes=[bpw_b])
                        c.op(DVE, lambda: V.tensor_tensor(out=blo[:], in0=blo[:], in1=bpw[:], op=ALU.add), reads=[blo_b, bpw_b], writes=[blo_b])
                for t in range(4):
                    if not need[t]:
                        c.op(DVE, lambda t=t: V.memset(blo[:, t:t + 1], -1.0e29), reads=[blo_b], writes=[blo_b])
                if "isc" in dbg_d and b == 0 and st == NT - 1:
                    dump("isc", Isc[3][:, 0:S], Isc_b[3], dbg_d["isc"][:, :])
                    dump("lo", blo[:, :], blo_b, dbg_d["lo"][:, :])

                for t in range(4):
                    j = st * 4 + t
                    L = (j + 1) * 128
                    c.op(DVE, lambda t=t, L=L: V.tensor_scalar(out=MB[:, 0:L], in0=Isc[t][:, 0:L], scalar1=blo[:, t:t + 1], scalar2=1.0,
                                                                op0=ALU.is_gt, op1=ALU.subtract), reads=[Isc_b[t], blo_b], writes=[MB_b])
                    O = [pin(), pin()]
                    for kc in range(j + 1):
                        for g in range(2):
                            bi = nb()
                            prs = [(kT[:, kc * 128:(kc + 1) * 128], qaT[:, 4 * g:4 * g + 4, t * 128:(t + 1) * 128]),
                                   (MB[:, kc * 128:(kc + 1) * 128], I8big[:, :, :])]
                            if kc >= j - 1:
                                prs.append((ident[:], biasT[:, j - kc, 4 * g:4 * g + 4, :]))
                            mm_group(banks[bi][:, :], prs, reads=[kT_b, qaT_b, MB_b], writes=[bank_b[bi]])
                            e = (2 * kc + g) % 2
                            c.op(ACT, lambda bi=bi, e=e: A.activation(out=ET[e][:], in_=banks[bi][:, :], func=AF.Exp), reads=[bank_b[bi]], writes=[ET_b[e]])
                            mm_group(banks[O[g]][:, :], [(Vaug[:, kc, :], ET[e][:])], reads=[Vaug_b, ET_b[e]], writes=[bank_b[O[g]]],
                                     first=(kc == 0), last=(kc == j))
                    for g in range(2):
                        c.op(DVE, lambda g=g: V.reciprocal(out=rec[64:128, :], in_=banks[O[g]][64:128, :]), reads=[bank_b[O[g]]], writes=[rec_b])
                        c.op(DVE, lambda g=g, t=t: V.tensor_tensor(out=OaT[:, 4 * g:4 * g + 4, t * 128:(t + 1) * 128],
                                                                  in0=banks[O[g]][0:64, :].rearrange("p (h q) -> p h q", h=4),
                                                                  in1=rec[64:128, :].rearrange("p (h q) -> p h q", h=4), op=ALU.mult),
                             reads=[bank_b[O[g]], rec_b], writes=[OaT_b])
                        unpin(O[g])

                for sub in range(4):
                    tk = slice(sub * 128, (sub + 1) * 128)
                    for hh in range(2):
                        pr_, pi_ = nb(), nb()
                        for ri, bi in ((0, pr_), (1, pi_)):
                            def fn(ri=ri, bi=bi):
                                ins = None
                                for cl in range(4):
                                    ins = T.matmul(banks[bi][:, cl * 128:(cl + 1) * 128], BpT[:, ri, 4 * hh + cl, :], uT[:, hh, tk], start=True, stop=True)
                                return ins
                            c.op(PE, fn, reads=[uT_b], writes=[bank_b[bi]])
                        cs = cosT[:, 4 * hh:4 * hh + 4, :].rearrange("p c n -> p (c n)")
                        sn = sinT[:, 4 * hh:4 * hh + 4, :].rearrange("p c n -> p (c n)")
                        dc = decT[:, 4 * hh:4 * hh + 4, :].rearrange("p c n -> p (c n)")
                        s0, s1, s2, s3, s4, s5 = stmp
                        z0, z1, z2, z3, z4, z5 = stmp_b
                        PR, PI = banks[pr_][:, :], banks[pi_][:, :]
                        c.op(DVE, lambda: V.tensor_tensor(out=s0[:], in0=PR, in1=cs, op=ALU.mult), reads=[bank_b[pr_]], writes=[z0])
                        c.op(DVE, lambda: V.tensor_tensor(out=s1[:], in0=PI, in1=sn, op=ALU.mult), reads=[bank_b[pi_]], writes=[z1])
                        c.op(DVE, lambda: V.tensor_tensor(out=s0[:], in0=s0[:], in1=s1[:], op=ALU.add), reads=[z0, z1], writes=[z0])
                        c.op(DVE, lambda: V.tensor_tensor(out=s2[:], in0=PI, in1=cs, op=ALU.mult), reads=[bank_b[pi_]], writes=[z2])
                        c.op(DVE, lambda: V.tensor_tensor(out=s1[:], in0=PR, in1=sn, op=ALU.mult), reads=[bank_b[pr_], z1], writes=[z1])
                        c.op(DVE, lambda: V.tensor_tensor(out=s2[:], in0=s2[:], in1=s1[:], op=ALU.subtract), reads=[z2, z1], writes=[z2])
                        s0v = s0[:].rearrange("p (c n) -> p c n", c=4)[:, :, 0]
                        s2v = s2[:].rearrange("p (c n) -> p c n", c=4)[:, :, 0]
                        c.op(DVE, lambda: V.tensor_tensor(out=s0v, in0=s0v, in1=s_init[:, 0, 4 * hh:4 * hh + 4], op=ALU.add), reads=[z0, s_init_b], writes=[z0])
                        c.op(DVE, lambda: V.tensor_tensor(out=s2v, in0=s2v, in1=s_init[:, 1, 4 * hh:4 * hh + 4], op=ALU.add), reads=[z2, s_init_b], writes=[z2])
                        c.op(DVE, lambda: V.tensor_tensor_scan(out=s3[:], data0=dc, data1=s0[:], initial=0.0, op0=ALU.mult, op1=ALU.add), reads=[z0], writes=[z3])
                        c.op(DVE, lambda: V.tensor_tensor_scan(out=s4[:], data0=dc, data1=s2[:], initial=0.0, op0=ALU.mult, op1=ALU.add), reads=[z2], writes=[z4])
                        c.op(DVE, lambda: V.tensor_tensor(out=s0[:], in0=s3[:], in1=cs, op=ALU.mult), reads=[z3, z0], writes=[z0])
                        c.op(DVE, lambda: V.tensor_tensor(out=s1[:], in0=s4[:], in1=sn, op=ALU.mult), reads=[z4, z1], writes=[z1])
                        c.op(DVE, lambda: V.tensor_tensor(out=s0[:], in0=s0[:], in1=s1[:], op=ALU.subtract), reads=[z0, z1], writes=[z0])
                        c.op(DVE, lambda: V.tensor_tensor(out=s2[:], in0=s3[:], in1=sn, op=ALU.mult), reads=[z3, z2], writes=[z2])
                        c.op(DVE, lambda: V.tensor_tensor(out=s1[:], in0=s4[:], in1=cs, op=ALU.mult), reads=[z4, z1], writes=[z1])
                        c.op(DVE, lambda: V.tensor_tensor(out=s2[:], in0=s2[:], in1=s1[:], op=ALU.add), reads=[z2, z1], writes=[z2])
                        c.op(POOL, lambda: G.tensor_copy(xrb[:, 0, :], s0[:]), reads=[z0], writes=[xrb_b])
                        c.op(POOL, lambda: G.tensor_copy(xrb[:, 1, :], s2[:]), reads=[z2], writes=[xrb_b])
                        xl_r = s0[:].rearrange("p (c n) -> p c n", c=4)[:, :, 127]
                        xl_i = s2[:].rearrange("p (c n) -> p c n", c=4)[:, :, 127]
                        cth = ssmc[:, COS, 4 * hh:4 * hh + 4]; sth = ssmc[:, SIN, 4 * hh:4 * hh + 4]; mg_ = ssmc[:, MAG, 4 * hh:4 * hh + 4]
                        q = ssm_small
                        c.op(DVE, lambda: V.tensor_tensor(out=q[:, 0, :], in0=xl_r, in1=cth, op=ALU.mult), reads=[z0], writes=[ssm_small_b])
                        c.op(DVE, lambda: V.tensor_tensor(out=q[:, 1, :], in0=xl_i, in1=sth, op=ALU.mult), reads=[z2], writes=[ssm_small_b])
                        c.op(DVE, lambda: V.tensor_tensor(out=q[:, 2, :], in0=xl_r, in1=sth, op=ALU.mult), reads=[z0], writes=[ssm_small_b])
                        c.op(DVE, lambda: V.tensor_tensor(out=q[:, 3, :], in0=xl_i, in1=cth, op=ALU.mult), reads=[z2], writes=[ssm_small_b])
                        c.op(DVE, lambda: V.tensor_tensor(out=q[:, 0, :], in0=q[:, 0, :], in1=q[:, 1, :], op=ALU.subtract), reads=[ssm_small_b], writes=[ssm_small_b])
                        c.op(DVE, lambda: V.tensor_tensor(out=q[:, 2, :], in0=q[:, 2, :], in1=q[:, 3, :], op=ALU.add), reads=[ssm_small_b], writes=[ssm_small_b])
                        c.op(DVE, lambda: V.tensor_tensor(out=s_init[:, 0, 4 * hh:4 * hh + 4], in0=q[:, 0, :], in1=mg_, op=ALU.mult), reads=[ssm_small_b], writes=[s_init_b])
                        c.op(DVE, lambda: V.tensor_tensor(out=s_init[:, 1, 4 * hh:4 * hh + 4], in0=q[:, 2, :], in1=mg_, op=ALU.mult), reads=[ssm_small_b], writes=[s_init_b])
                        by = nb()
                        prs = []
                        for cl in range(4):
                            prs.append((Cp[:, 0, 4 * hh + cl, :], xrb[:, 0, cl * 128:(cl + 1) * 128]))
                            prs.append((Cp[:, 1, 4 * hh + cl, :], xrb[:, 1, cl * 128:(cl + 1) * 128]))
                        mm_group(banks[by][:, 0:128], prs, reads=[xrb_b], writes=[bank_b[by]])
                        c.op(DVE, lambda by=by: V.scalar_tensor_tensor(out=yf[:], in0=uT[:, hh, tk], scalar=dcol[:, hh:hh + 1], in1=banks[by][:, 0:128],
                                                                       op0=ALU.mult, op1=ALU.add), reads=[uT_b, bank_b[by]], writes=[yf_b])
                        c.op(ACT, lambda: A.activation(out=ysT[:, hh, tk], in_=yf[:], func=AF.Gelu), reads=[yf_b], writes=[ysT_b])

                for h in range(4):
                    om = pin()
                    for mc in range(2):
                        bi = nb()
                        mm_group(banks[bi][:, :], [(mkT[:, h, mc * 128:(mc + 1) * 128], qmT[:, h, :])], reads=[mkT_b, qmT_b], writes=[bank_b[bi]])
                        e = (2 * h + mc) % 2
                        c.op(ACT, lambda bi=bi, e=e: A.activation(out=ET[e][:], in_=banks[bi][:, :], func=AF.Exp), reads=[bank_b[bi]], writes=[ET_b[e]])
                        mm_group(banks[om][:, :], [(Vm[:, mc, h, :], ET[e][:])], reads=[Vm_b, ET_b[e]], writes=[bank_b[om]], first=(mc == 0), last=(mc == 1))
                    c.op(DVE, lambda om=om: V.reciprocal(out=rec[64:128, :], in_=banks[om][64:128, :]), reads=[bank_b[om]], writes=[rec_b])
                    c.op(DVE, lambda om=om, h=h: V.tensor_tensor(out=OmT[:, h, :], in0=banks[om][0:64, :], in1=rec[64:128, :], op=ALU.mult),
                         reads=[bank_b[om], rec_b], writes=[OmT_b])
                    unpin(om)

                for f in range(8):
                    wt, wb = piece(("mg", f))
                    wg = wt[:, 0:3072].rearrange("p (br kc d) -> p br kc d", br=3, kc=8)
                    woa = wt[0:64, 3072:4096].rearrange("p (h d) -> p h d", h=8)
                    wgl = wt[:, 4096:4608].rearrange("p (ab kc d) -> p ab kc d", ab=2, kc=2)
                    wom = wt[0:64, 4608:5120].rearrange("p (h d) -> p h d", h=4)
                    gb = [nb() for _ in range(3)]
                    for br in range(3):
                        mm_group(banks[gb[br]][:, :], [(wg[:, br, kc, :], hT[:, kc, :]) for kc in range(KC)], reads=[wb, hT_b], writes=[bank_b[gb[br]]])
                    ba = nb()
                    mm_group(banks[ba][:, :], [(woa[:, h, :], OaT[:, h, :]) for h in range(8)], reads=[wb, OaT_b], writes=[bank_b[ba]])
                    bga, bgb = nb(), nb()
                    mm_group(banks[bga][:, :], [(wgl[:, 0, kc, :], ysT[:, kc, :]) for kc in range(2)], reads=[wb, ysT_b], writes=[bank_b[bga]])
                    mm_group(banks[bgb][:, :], [(wgl[:, 1, kc, :], ysT[:, kc, :]) for kc in range(2)], reads=[wb, ysT_b], writes=[bank_b[bgb]])
                    bm = nb()
                    mm_group(banks[bm][:, :], [(wom[:, h, :], OmT[:, h, :]) for h in range(4)], reads=[wb, OmT_b], writes=[bank_b[bm]])
                    m0, m1, m2 = stmp[0], stmp[1], stmp[2]
                    y0, y1, y2 = stmp_b[0], stmp_b[1], stmp_b[2]
                    c.op(ACT, lambda: A.activation(out=m0[:], in_=banks[gb[0]][:, :], func=AF.Sigmoid), reads=[bank_b[gb[0]]], writes=[y0])
                    c.op(DVE, lambda: V.tensor_tensor(out=m0[:], in0=m0[:], in1=banks[ba][:, :], op=ALU.mult), reads=[y0, bank_b[ba]], writes=[y0])
                    c.op(ACT, lambda: A.activation(out=m1[:], in_=banks[bgb][:, :], func=AF.Sigmoid), reads=[bank_b[bgb]], writes=[y1])
                    c.op(DVE, lambda: V.tensor_tensor(out=m1[:], in0=m1[:], in1=banks[bga][:, :], op=ALU.mult), reads=[y1, bank_b[bga]], writes=[y1])
                    c.op(ACT, lambda: A.activation(out=m2[:], in_=banks[gb[1]][:, :], func=AF.Sigmoid), reads=[bank_b[gb[1]]], writes=[y2])
                    c.op(DVE, lambda: V.tensor_tensor(out=m1[:], in0=m1[:], in1=m2[:], op=ALU.mult), reads=[y1, y2], writes=[y1])
                    c.op(DVE, lambda: V.tensor_tensor(out=m0[:], in0=m0[:], in1=m1[:], op=ALU.add), reads=[y0, y1], writes=[y0])
                    c.op(ACT, lambda: A.activation(out=m2[:], in_=banks[gb[2]][:, :], func=AF.Sigmoid), reads=[bank_b[gb[2]]], writes=[y2])
                    c.op(DVE, lambda: V.tensor_tensor(out=m2[:], in0=m2[:], in1=banks[bm][:, :], op=ALU.mult), reads=[y2, bank_b[bm]], writes=[y2])
                    c.op(DVE, lambda f=f: V.tensor_tensor(out=mgT[:, f, :], in0=m0[:], in1=m2[:], op=ALU.add), reads=[y0, y2], writes=[mgT_b])
                for hh in range(2):
                    wt, wb = piece(("wo", hh))
                    w3 = wt[:, 0:4096].rearrange("p (kc d) -> p kc d", kc=8)
                    for t in range(4):
                        bi = nb()
                        mm_group(banks[bi][:, :], [(mgT[:, kc, t * 128:(t + 1) * 128], w3[:, kc, :]) for kc in range(KC)],
                                 reads=[wb, mgT_b], writes=[bank_b[bi]])
                        xsl = xt[:, t, hh * 512:(hh + 1) * 512]
                        c.op(DVE, lambda bi=bi, xsl=xsl: V.tensor_tensor(out=xsl, in0=xsl, in1=banks[bi][:, :], op=ALU.add),
                             reads=[bank_b[bi], xt_b], writes=[xt_b])
                if "x2" in dbg_d and b == 0:
                    dump("x2", xt[:], xt_b, dbg_d["x2"][st * 512:(st + 1) * 512, :].rearrange("(t p) d -> p t d", p=128))
                ffn("f2", 2)
                for t in range(4):
                    c.op(ACT, lambda t=t: A.activation(out=junk[:, 0:D], in_=xt[:, t, :], func=AF.Square, accum_out=ss[:, t:t + 1]),
                         reads=[xt_b], writes=[junk_b, ss_b])
                    c.op(ACT, lambda t=t: A.activation(out=ss[:, 4 + t:5 + t], in_=ss[:, t:t + 1], func=AF.Sqrt, bias=epsc[:, 0:1], scale=1.0 / D),
                         reads=[ss_b], writes=[ss_b])
                    c.op(DVE, lambda t=t: V.reciprocal(out=ss[:, 4 + t:5 + t], in_=ss[:, 4 + t:5 + t]), reads=[ss_b], writes=[ss_b])
                    c.op(DVE, lambda t=t: V.scalar_tensor_tensor(out=xt[:, t, :], in0=xt[:, t, :], scalar=ss[:, 4 + t:5 + t], in1=gfin[:],
                                                                 op0=ALU.mult, op1=ALU.mult), reads=[xt_b, ss_b], writes=[xt_b])
                c.dma(POOL, out_d[row0:row0 + 512, :].rearrange("(t p) d -> p t d", p=128), xt[:], sem_out, reads=[xt_b])
        G.wait_ge(sem_out.h, sem_out.n)
        if sem_misc.n:
            G.wait_ge(sem_misc.h, sem_misc.n)
    return nc


_CACHE = {}


def _run(inputs, NB, S, dbg=()):
    key = (NB, S, str(dbg))
    if key not in _CACHE:
        _CACHE[key] = build(NB, S, dbg)
    nc = _CACHE[key]
    x = np.ascontiguousarray(inputs["x"], dtype=np.float32)
    mem = np.ascontiguousarray(inputs["mem"], dtype=np.float32)
    ncore = 8
    wmap = {n: np.ascontiguousarray(np.asarray(inputs[n], dtype=np.float32).reshape(WSHAPES[n])) for n in WNAMES}
    wmap["onehot"] = _onehot_table()
    in_maps = []
    for i in range(ncore):
        m = dict(wmap)
        m["x"] = x[i * NB:(i + 1) * NB].reshape(NB * S, D)
        m["mem"] = mem[i * NB:(i + 1) * NB].reshape(NB * 256, D)
        in_maps.append(m)
    res = run_bass_kernel_spmd(nc, in_maps, core_ids=list(range(ncore)))
    return res


def kernel(**inputs):
    B, S = inputs["x"].shape[0], inputs["x"].shape[1]
    NB = B // 8
    res = _run(inputs, NB, S)
    out = np.concatenate([r["out"].reshape(NB, S, D) for r in res.results], axis=0)
    return out.astype(np.float32)
```

```python
import math
from contextlib import ExitStack
import numpy as np
import concourse.bass as bass
import concourse.mybir as mybir
from concourse.bass_utils import run_bass_kernel_spmd

F32 = mybir.dt.float32
BF16 = mybir.dt.bfloat16
I32 = mybir.dt.int32
AF = mybir.ActivationFunctionType
ALU = mybir.AluOpType

D = 1024
KC = 8
FFN = 2816
FC = 22
WIN = 4548
EPS = 1e-6
NEG = 30000.0
BIG = 1.0e30
SLOT = 5632
NITER = 22


class Tok:
    __slots__ = ("sem", "val")

    def __init__(self, sem, val):
        self.sem = sem
        self.val = val


class Sem:
    def __init__(self, h):
        self.h = h
        self.n = 0


class Buf:
    def __init__(self, name):
        self.name = name
        self.w = None
        self.r = {}


class Eng:
    def __init__(self, eng, sem):
        self.eng = eng
        self.sem = sem
        self.waited = {}

    def wait(self, tok):
        if tok is None:
            return
        k = id(tok.sem)
        if self.waited.get(k, 0) >= tok.val:
            return
        self.eng.wait_ge(tok.sem.h, tok.val)
        self.waited[k] = tok.val


class Ctx:
    def __init__(self, nc, es):
        self.nc = nc
        self.es = es
        self.PE = Eng(nc.tensor, self.sem("pe"))
        self.ACT = Eng(nc.scalar, self.sem("act"))
        self.DVE = Eng(nc.vector, self.sem("dve"))
        self.POOL = Eng(nc.gpsimd, self.sem("pool"))
        self.SP = Eng(nc.sync, self.sem("sp"))

    def sem(self, name):
        return Sem(self.es.enter_context(self.nc.semaphore(name)))

    def _deps(self, E, reads, writes):
        for b in reads:
            E.wait(b.w)
        pe = E is self.PE
        for b in writes:
            if b.w is not None and not (pe and b.w.sem is E.sem):
                E.wait(b.w)
            for t in b.r.values():
                if not (pe and t.sem is E.sem):
                    E.wait(t)

    def _commit(self, tok, reads, writes):
        for b in reads:
            b.r[id(tok.sem)] = tok
        for b in writes:
            b.w = tok
            b.r = {}

    def op(self, E, fn, reads=(), writes=()):
        self._deps(E, reads, writes)
        ins = fn()
        E.sem.n += 1
        ins.then_inc(E.sem.h, 1)
        tok = Tok(E.sem, E.sem.n)
        self._commit(tok, reads, writes)
        return tok

    def dma(self, Q, out, in_, sem, reads=(), writes=(), **kw):
        self._deps(Q, reads, writes)
        ins = Q.eng.dma_start(out=out, in_=in_, **kw)
        sem.n += 16
        ins.then_inc(sem.h, 16)
        tok = Tok(sem, sem.n)
        self._commit(tok, reads, writes)
        return tok

    def barrier(self, bufs):
        for E in (self.PE, self.ACT, self.DVE, self.POOL, self.SP):
            for b in bufs:
                E.wait(b.w)
                for t in b.r.values():
                    E.wait(t)


def _t5_bucket_np(rel):
    half = 16
    max_exact = 8
    n = np.abs(rel)
    nf = np.maximum(n, 1).astype(np.float32)
    large = max_exact + (np.log(nf / np.float32(max_exact)) / np.float32(math.log(128 / 8))
                         * np.float32(half - max_exact)).astype(np.int32)
    large = np.minimum(large, half - 1)
    return np.where(rel > 0, half, 0) + np.where(n < max_exact, n, large)


def _onehot_table():
    rel = np.arange(-255, 129, dtype=np.int32)
    bk = _t5_bucket_np(rel)
    oh = np.zeros((32, 384), np.float32)
    oh[bk, np.arange(384)] = 1.0
    return oh


WNAMES = ["ffn1_norm", "ffn1_w_in", "ffn1_w_out", "mix_norm", "w_in", "a_q_gain", "a_k_gain", "rel_bias",
          "w_o_a", "ssm_lambda_re", "ssm_lambda_im", "ssm_log_dt", "ssm_b_re", "ssm_b_im", "ssm_c_re",
          "ssm_c_im", "ssm_d", "w_glu", "mem_norm", "w_mem_kv", "m_q_gain", "m_k_gain", "w_o_m", "w_out",
          "ffn2_norm", "ffn2_w_in", "ffn2_w_out", "final_norm"]
WSHAPES = {"ffn1_norm": [1, D], "ffn1_w_in": [D, 2 * FFN], "ffn1_w_out": [FFN, D], "mix_norm": [1, D],
           "w_in": [D, WIN], "a_q_gain": [64, 1], "a_k_gain": [64, 1], "rel_bias": [32, 8],
           "w_o_a": [512, D], "ssm_lambda_re": [16, 64], "ssm_lambda_im": [16, 64], "ssm_log_dt": [1, 16],
           "ssm_b_re": [16, 64, 16], "ssm_b_im": [16, 64, 16], "ssm_c_re": [16, 16, 64],
           "ssm_c_im": [16, 16, 64], "ssm_d": [256, 1], "w_glu": [256, 2 * D], "mem_norm": [1, D],
           "w_mem_kv": [D, 512], "m_q_gain": [64, 1], "m_k_gain": [64, 1], "w_o_m": [256, D],
           "w_out": [D, D], "ffn2_norm": [1, D], "ffn2_w_in": [D, 2 * FFN], "ffn2_w_out": [FFN, D],
           "final_norm": [1, D]}


def build(NB, S, dbg=()):
    NT = S // 512
    TOPK = min(256, S // 4)
    nc = bass.Bass("TRN2", target_bir_lowering=False)
    x_d = nc.dram_tensor("x", [NB * S, D], F32, kind="ExternalInput").ap()
    mem_d = nc.dram_tensor("mem", [NB * 256, D], F32, kind="ExternalInput").ap()
    W = {n: nc.dram_tensor(n, WSHAPES[n], F32, kind="ExternalInput").ap() for n in WNAMES}
    oh_d = nc.dram_tensor("onehot", [32, 384], F32, kind="ExternalInput").ap()
    out_d = nc.dram_tensor("out", [NB * S, D], F32, kind="ExternalOutput").ap()
    dbg_d = {n: nc.dram_tensor("dbg_" + n, shp, F32, kind="ExternalOutput").ap() for n, shp in dbg}

    pieces = []

    def ffn_pieces(tag, win, wout):
        wi4 = win.rearrange("(kc p) (gu f) -> p kc gu f", p=128, gu=2)
        for i in range(11):
            srcs = [(0, 128, gu * 2048, [8, 256], wi4[:, :, gu, 256 * i:256 * i + 256]) for gu in range(2)]
            pieces.append(((tag, "in", i), srcs))
        wo3 = wout.rearrange("(f p) d -> p f d", p=128)
        for hh in range(2):
            for pp in range(2):
                pieces.append(((tag, "out", hh, pp),
                               [(0, 128, 0, [11, 512], wo3[:, 11 * pp:11 * pp + 11, 512 * hh:512 * hh + 512])]))

    win3 = W["w_in"].rearrange("(kc p) c -> p kc c", p=128)
    ffn_pieces("f1", W["ffn1_w_in"], W["ffn1_w_out"])
    pieces.append((("p1",), [(0, 128, 0, [8, 512], win3[:, :, 0:512])]))
    pieces.append((("p2",), [(0, 128, 0, [8, 452], win3[:, :, 512:964])]))
    pieces.append((("p3",), [(0, 128, 0, [8, 512], win3[:, :, 964:1476])]))
    woa3 = W["w_o_a"].rearrange("(h p) d -> p h d", p=64)
    wglu3 = W["w_glu"].rearrange("(kc p) d -> p kc d", p=128)
    wom3 = W["w_o_m"].rearrange("(h p) d -> p h d", p=64)
    for f in range(8):
        srcs = []
        for br in range(3):
            c0 = 1476 + br * 1024 + f * 128
            srcs.append((0, 128, br * 1024, [8, 128], win3[:, :, c0:c0 + 128]))
        srcs.append((0, 64, 3072, [8, 128], woa3[:, :, f * 128:f * 128 + 128]))
        for ab in range(2):
            srcs.append((0, 128, 4096 + ab * 256, [2, 128], wglu3[:, :, ab * 1024 + f * 128:ab * 1024 + f * 128 + 128]))
        srcs.append((0, 64, 4608, [4, 128], wom3[:, :, f * 128:f * 128 + 128]))
        pieces.append((("mg", f), srcs))
    wout3 = W["w_out"].rearrange("(kc p) d -> p kc d", p=128)
    for hh in range(2):
        pieces.append((("wo", hh), [(0, 128, 0, [8, 512], wout3[:, :, 512 * hh:512 * hh + 512])]))
    ffn_pieces("f2", W["ffn2_w_in"], W["ffn2_w_out"])
    wkv3 = W["w_mem_kv"].rearrange("(kc p) d -> p kc d", p=128)
    pieces.append((("kv",), [(0, 128, 0, [8, 512], wkv3[:, :, :])]))
    pidx = {k: i for i, (k, _) in enumerate(pieces)}
    NP = len(pieces)
    wscr = nc.dram_tensor("wscr", [NP, 128, SLOT], BF16, kind="Internal").ap()
    fd_d = nc.dram_tensor("fdscr", [8, 384], F32, kind="Internal").ap()

    with ExitStack() as es:
        c = Ctx(nc, es)
        PE, ACT, DVE, POOL, SP = c.PE, c.ACT, c.DVE, c.POOL, c.SP
        V, A, G, T = nc.vector, nc.scalar, nc.gpsimd, nc.tensor

        def sb(name, shape, dt):
            return es.enter_context(nc.sbuf_tensor(name, shape, dt))

        wscr_b = Buf("wscr")
        sem_pro = c.sem("pro")
        sem_const = c.sem("const")
        sem_out = c.sem("outst")
        sem_x = c.sem("xld")
        sem_misc = c.sem("misc")

        with ExitStack() as es2:
            stg32 = [es2.enter_context(nc.sbuf_tensor("stg32_%d" % i, [128, SLOT], F32)) for i in range(2)]
            stg16 = [es2.enter_context(nc.sbuf_tensor("stg16_%d" % i, [128, SLOT], BF16)) for i in range(2)]
            b32 = [Buf("s32a"), Buf("s32b")]
            b16 = [Buf("s16a"), Buf("s16b")]
            s32sem = [c.sem("s32a"), c.sem("s32b")]
            for i in range(2):
                c.op(POOL, lambda i=i: G.memset(stg32[i][:], 0.0), writes=[b32[i]])
            for k, (key, srcs) in enumerate(pieces):
                j = k % 2
                n = 0
                for (p0, p1, off, shp, src) in srcs:
                    sz = shp[0] * shp[1]
                    dst = stg32[j][p0:p1, off:off + sz].rearrange("p (a b) -> p a b", a=shp[0])
                    c.dma(SP, dst, src, s32sem[j], writes=[b32[j]])
                    n = max(n, off + sz)
                E = (ACT, DVE)[k % 2]
                if E is ACT:
                    c.op(ACT, lambda j=j, n=n: A.copy(out=stg16[j][:, 0:n], in_=stg32[j][:, 0:n]),
                         reads=[b32[j]], writes=[b16[j]])
                elif E is DVE:
                    c.op(DVE, lambda j=j, n=n: V.tensor_copy(stg16[j][:, 0:n], stg32[j][:, 0:n]),
                         reads=[b32[j]], writes=[b16[j]])
                else:
                    c.op(POOL, lambda j=j, n=n: G.tensor_copy(stg16[j][:, 0:n], stg32[j][:, 0:n]),
                         reads=[b32[j]], writes=[b16[j]])
                c.dma(SP, wscr[k, :, 0:n], stg16[j][:, 0:n], sem_pro, reads=[b16[j]], writes=[wscr_b])
            c.barrier(b32 + b16)

        ident_f = sb("ident_f", [128, 128], F32)
        ident = sb("ident", [128, 128], BF16)
        antiJ = sb("antiJ", [128, 128], BF16)
        I8big = sb("I8big", [128, 4, 128], BF16)
        ones = sb("ones", [128, 128], BF16)
        epsc = sb("epsc", [128, 1], F32)
        gcol = sb("gcol", [128, 4, KC], F32)
        gfin = sb("gfin", [128, D], F32)
        hg = sb("hg", [64, 4], F32)
        biasT = sb("biasT", [128, 2, 8, 128], BF16)
        cst_b = Buf("const")
        cosT = sb("cosT", [128, 8, 128], F32); sinT = sb("sinT", [128, 8, 128], F32)
        decT = sb("decT", [128, 8, 128], F32)
        BpT = sb("BpT", [128, 2, 8, 128], BF16)
        Cp = sb("Cp", [128, 2, 8, 128], BF16)
        ssmc = sb("ssmc", [128, 12, 8], F32)
        dcol = sb("dcol", [128, 2], F32)

        banks = [es.enter_context(nc.psum_tensor("bank%d" % i, [128, 512], F32)) for i in range(7)]
        bank_b = [Buf("bank%d" % i) for i in range(7)]
        psT = es.enter_context(nc.psum_tensor("psT", [128, 1024], BF16)); psT_b = Buf("psT")
        pinned = set()
        rr = [0]

        def nb():
            while True:
                i = rr[0] % 7
                rr[0] += 1
                if i not in pinned:
                    return i

        def pin():
            i = nb()
            pinned.add(i)
            return i

        def unpin(i):
            pinned.discard(i)

        def mm_group(out_ap, pairs, reads, writes, first=True, last=True):
            def fn():
                ins = None
                n = len(pairs)
                for i, (l, r) in enumerate(pairs):
                    ins = T.matmul(out_ap, l, r, start=(first and i == 0), stop=(last and i == n - 1))
                return ins
            return c.op(PE, fn, reads=reads, writes=writes)

        def dump(name, ap_sb, buf, dst):
            if name in dbg_d:
                c.dma(POOL, dst, ap_sb, sem_misc, reads=[buf])

        def cdma(out, in_, **kw):
            c.dma(SP, out, in_, sem_const, writes=[cst_b], **kw)

        for gi, nm in enumerate(["ffn1_norm", "mix_norm", "ffn2_norm", "mem_norm"]):
            cdma(gcol[:, gi, :], W[nm].rearrange("o (c p) -> p (o c)", p=128), allow_slow_non_contiguous=True)
        cdma(gfin[:], W["final_norm"][0:1, :].partition_broadcast(128))
        for gi, nm in enumerate(["a_q_gain", "a_k_gain", "m_q_gain", "m_k_gain"]):
            cdma(hg[:, gi:gi + 1], W[nm][:, :])
        es3 = ExitStack()
        sb_main = sb

        def sb(name, shape, dt):
            return es3.enter_context(nc.sbuf_tensor(name, shape, dt))
        rb_sb = sb("rb_sb", [32, 8], F32); oh_sb = sb("oh_sb", [32, 384], F32); rb15 = sb("rb15", [8, 1], F32)
        fsb = sb("fsb", [8, 384], F32); hank = sb("hank", [128, 2, 8, 128], F32); hankb = sb("hankb", [128, 2, 8, 128], BF16)
        cdma(rb_sb[:], W["rel_bias"][:, :])
        cdma(oh_sb[:], oh_d[:, :])
        cdma(rb15[:], W["rel_bias"][15:16, :].rearrange("o h -> h o"), allow_slow_non_contiguous=True)
        for t in range(2):
            cdma(ssmc[t * 64:(t + 1) * 64, 0, :], W["ssm_lambda_re"].rearrange("(c t) s -> t s c", t=2)[t], allow_slow_non_contiguous=True)
            cdma(ssmc[t * 64:(t + 1) * 64, 1, :], W["ssm_lambda_im"].rearrange("(c t) s -> t s c", t=2)[t], allow_slow_non_contiguous=True)
            cdma(ssmc[t * 64:(t + 1) * 64, 2, :], W["ssm_log_dt"].rearrange("o (c t) -> t o c", t=2)[t].partition_broadcast(64), allow_slow_non_contiguous=True)
        braw = sb("braw", [128, 2, 8, 16], F32); craw = sb("craw", [128, 2, 8, 16], F32)
        for t in range(2):
            cdma(braw[t * 64:(t + 1) * 64, 0, :, :], W["ssm_b_re"].rearrange("(c t) s h -> t s c h", t=2)[t])
            cdma(braw[t * 64:(t + 1) * 64, 1, :, :], W["ssm_b_im"].rearrange("(c t) s h -> t s c h", t=2)[t])
            for cc in range(8):
                cdma(craw[t * 64:(t + 1) * 64, 0, cc, :], W["ssm_c_re"].rearrange("(c t) o s -> t c s o", t=2)[t, cc], allow_slow_non_contiguous=True)
                cdma(craw[t * 64:(t + 1) * 64, 1, cc, :], W["ssm_c_im"].rearrange("(c t) o s -> t c s o", t=2)[t, cc], allow_slow_non_contiguous=True)
        cdma(dcol[:, :], W["ssm_d"].rearrange("(m p) o -> p (m o)", p=128), allow_slow_non_contiguous=True)

        def cop(E, fn):
            return c.op(E, fn, reads=[cst_b], writes=[cst_b])

        cop(POOL, lambda: G.memset(ident_f[:], 1.0))
        cop(POOL, lambda: G.affine_select(out=ident_f[:], in_=ident_f[:], pattern=[[-1, 128]], compare_op=ALU.is_equal,
                                          fill=0.0, base=0, channel_multiplier=1))
        cop(DVE, lambda: V.tensor_copy(ident[:], ident_f[:]))
        for i in range(4):
            cop(DVE, lambda i=i: V.tensor_scalar(out=I8big[:, i, :], in0=ident_f[:], scalar1=NEG, scalar2=None, op0=ALU.mult))
        cop(POOL, lambda: G.memset(ident_f[:], 1.0))
        cop(POOL, lambda: G.affine_select(out=ident_f[:], in_=ident_f[:], pattern=[[1, 128]], compare_op=ALU.is_equal,
                                          fill=0.0, base=-127, channel_multiplier=1))
        cop(DVE, lambda: V.tensor_copy(antiJ[:], ident_f[:]))
        cop(DVE, lambda: V.memset(ones[:], 1.0))
        cop(DVE, lambda: V.memset(epsc[:], EPS))
        cop(DVE, lambda: V.tensor_scalar(out=hg[:, 0:1], in0=hg[:, 0:1], scalar1=0.125, scalar2=None, op0=ALU.mult))
        cop(DVE, lambda: V.tensor_scalar(out=hg[:, 2:3], in0=hg[:, 2:3], scalar1=0.125, scalar2=None, op0=ALU.mult))
        b0 = 0
        c.op(PE, lambda: T.matmul(banks[b0][0:8, 0:384], rb_sb[:, :], oh_sb[:, :], start=True, stop=True),
             reads=[cst_b], writes=[bank_b[b0]])
        c.op(DVE, lambda: V.tensor_scalar(out=fsb[:], in0=banks[b0][0:8, 0:384], scalar1=rb15[:, 0:1], scalar2=None,
                                          op0=ALU.subtract), reads=[bank_b[b0], cst_b], writes=[cst_b])
        fd_b = Buf("fd")
        c.dma(SP, fd_d[:, :], fsb[:], sem_const, reads=[cst_b], writes=[fd_b])
        for di, delta in enumerate((0, 128)):
            src = bass.AP(tensor=fd_d.tensor, offset=128 - delta, ap=[[1, 128], [384, 8], [1, 128]])
            c.dma(SP, hank[:, di, :, :], src, sem_const, reads=[fd_b], writes=[cst_b])
        cop(DVE, lambda: V.tensor_copy(hankb[:], hank[:]))
        for di in range(2):
            for h in range(8):
                bi = nb()
                c.op(PE, lambda di=di, h=h, bi=bi: T.matmul(banks[bi][:, 0:128], hankb[:, di, h, :], antiJ[:], start=True, stop=True),
                     reads=[cst_b], writes=[bank_b[bi]])
                c.op(DVE, lambda di=di, h=h, bi=bi: V.tensor_copy(biasT[:, di, h, :], banks[bi][:, 0:128]),
                     reads=[bank_b[bi], cst_b], writes=[cst_b])

        LR, LI, LDT, DT, MAG, TH, COS, SIN, FR, FI, T1, T2 = range(12)
        sc = lambda i: ssmc[:, i, :]
        cop(ACT, lambda: A.activation(out=sc(DT), in_=sc(LDT), func=AF.Exp))
        cop(DVE, lambda: V.tensor_tensor(out=sc(T1), in0=sc(LR), in1=sc(DT), op=ALU.mult))
        cop(ACT, lambda: A.activation(out=sc(MAG), in_=sc(T1), func=AF.Exp))
        cop(DVE, lambda: V.tensor_tensor(out=sc(TH), in0=sc(LI), in1=sc(DT), op=ALU.mult))
        MAGIC = 12582912.0
        TWO_PI = 2.0 * math.pi

        def sin_of(out_ap, in_ap, tmp_ap, shift):
            cop(DVE, lambda: V.tensor_scalar(out=tmp_ap, in0=in_ap, scalar1=shift, scalar2=1.0 / TWO_PI, op0=ALU.add, op1=ALU.mult))
            cop(DVE, lambda: V.tensor_scalar(out=tmp_ap, in0=tmp_ap, scalar1=MAGIC, scalar2=None, op0=ALU.add))
            cop(DVE, lambda: V.tensor_scalar(out=tmp_ap, in0=tmp_ap, scalar1=-MAGIC, scalar2=-TWO_PI, op0=ALU.add, op1=ALU.mult))
            cop(DVE, lambda: V.scalar_tensor_tensor(out=tmp_ap, in0=in_ap, scalar=shift, in1=tmp_ap, op0=ALU.add, op1=ALU.add))
            cop(DVE, lambda: V.tensor_scalar(out=tmp_ap, in0=tmp_ap, scalar1=math.pi, scalar2=-math.pi, op0=ALU.min, op1=ALU.max))
            cop(ACT, lambda: A.activation(out=out_ap, in_=tmp_ap, func=AF.Sin))

        sin_of(sc(SIN), sc(TH), sc(T1), 0.0)
        sin_of(sc(COS), sc(TH), sc(T1), math.pi / 2)
        arr = sb("arr", [128, 6, 8], F32)
        a_ = lambda i: arr[:, i, :]
        cop(DVE, lambda: V.tensor_tensor(out=a_(0), in0=sc(MAG), in1=sc(COS), op=ALU.mult))
        cop(DVE, lambda: V.tensor_scalar(out=a_(0), in0=a_(0), scalar1=-1.0, scalar2=None, op0=ALU.add))
        cop(DVE, lambda: V.tensor_tensor(out=a_(1), in0=sc(MAG), in1=sc(SIN), op=ALU.mult))
        cop(DVE, lambda: V.tensor_tensor(out=a_(2), in0=sc(LR), in1=sc(LR), op=ALU.mult))
        cop(DVE, lambda: V.tensor_tensor(out=a_(3), in0=sc(LI), in1=sc(LI), op=ALU.mult))
        cop(DVE, lambda: V.tensor_tensor(out=a_(2), in0=a_(2), in1=a_(3), op=ALU.add))
        cop(DVE, lambda: V.reciprocal(out=a_(5), in_=a_(2)))
        cop(DVE, lambda: V.tensor_tensor(out=a_(3), in0=a_(0), in1=sc(LR), op=ALU.mult))
        cop(DVE, lambda: V.tensor_tensor(out=a_(4), in0=a_(1), in1=sc(LI), op=ALU.mult))
        cop(DVE, lambda: V.tensor_tensor(out=a_(3), in0=a_(3), in1=a_(4), op=ALU.add))
        cop(DVE, lambda: V.tensor_tensor(out=sc(FR), in0=a_(3), in1=a_(5), op=ALU.mult))
        cop(DVE, lambda: V.tensor_tensor(out=a_(3), in0=a_(1), in1=sc(LR), op=ALU.mult))
        cop(DVE, lambda: V.tensor_tensor(out=a_(4), in0=a_(0), in1=sc(LI), op=ALU.mult))
        cop(DVE, lambda: V.tensor_tensor(out=a_(3), in0=a_(3), in1=a_(4), op=ALU.subtract))
        cop(DVE, lambda: V.tensor_tensor(out=sc(FI), in0=a_(3), in1=a_(5), op=ALU.mult))
        bbar = sb("bbar", [128, 2, 8, 16], F32); btmp = sb("btmp", [128, 8, 16], F32)
        frb = ssmc[:, FR, :].unsqueeze(2).to_broadcast([128, 8, 16])
        fib = ssmc[:, FI, :].unsqueeze(2).to_broadcast([128, 8, 16])
        cop(DVE, lambda: V.tensor_tensor(out=bbar[:, 0, :, :], in0=braw[:, 0, :, :], in1=frb, op=ALU.mult))
        cop(DVE, lambda: V.tensor_tensor(out=btmp[:], in0=braw[:, 1, :, :], in1=fib, op=ALU.mult))
        cop(DVE, lambda: V.tensor_tensor(out=bbar[:, 0, :, :], in0=bbar[:, 0, :, :], in1=btmp[:], op=ALU.subtract))
        cop(DVE, lambda: V.tensor_tensor(out=bbar[:, 1, :, :], in0=braw[:, 1, :, :], in1=frb, op=ALU.mult))
        cop(DVE, lambda: V.tensor_tensor(out=btmp[:], in0=braw[:, 0, :, :], in1=fib, op=ALU.mult))
        cop(DVE, lambda: V.tensor_tensor(out=bbar[:, 1, :, :], in0=bbar[:, 1, :, :], in1=btmp[:], op=ALU.add))
        xpad = sb("xpad", [128, 128], BF16)
        for ri in range(2):
            for cc in range(8):
                cop(DVE, lambda: V.memset(xpad[:], 0.0))
                for t in range(2):
                    col = 32 * (cc % 4) + 16 * t
                    cop(DVE, lambda ri=ri, cc=cc, t=t, col=col: V.tensor_copy(xpad[t * 64:(t + 1) * 64, col:col + 16],
                                                                               bbar[t * 64:(t + 1) * 64, ri, cc, :]))
                c.op(PE, lambda: T.transpose(psT[:, 0:128], xpad[:], ident[:]), reads=[cst_b], writes=[psT_b])
                c.op(DVE, lambda ri=ri, cc=cc: V.tensor_copy(BpT[:, ri, cc, :], psT[:, 0:128]), reads=[psT_b, cst_b], writes=[cst_b])
        cop(DVE, lambda: V.memset(Cp[:], 0.0))
        for ri in range(2):
            for cc in range(8):
                for t in range(2):
                    col = 16 * ((2 * cc + t) % 8)
                    cop(DVE, lambda ri=ri, cc=cc, t=t, col=col: V.tensor_scalar(
                        out=Cp[t * 64:(t + 1) * 64, ri, cc, col:col + 16], in0=craw[t * 64:(t + 1) * 64, ri, cc, :],
                        scalar1=(1.0 if ri == 0 else -1.0), scalar2=None, op0=ALU.mult))
        tau_i = sb("tau_i", [128, 128], I32); tau = sb("tau", [128, 128], F32); ang = sb("ang", [128, 8, 128], F32)
        atmp = sb("atmp", [128, 8, 128], F32)
        cop(POOL, lambda: G.iota(tau_i[:], pattern=[[1, 128]], base=0, channel_multiplier=0))
        cop(DVE, lambda: V.tensor_copy(tau[:], tau_i[:]))
        for cc in range(8):
            cop(DVE, lambda cc=cc: V.tensor_scalar(out=ang[:, cc, :], in0=tau[:], scalar1=ssmc[:, TH, cc:cc + 1], scalar2=None, op0=ALU.mult))
        sin_of(sinT[:], ang[:], atmp[:], 0.0)
        sin_of(cosT[:], ang[:], atmp[:], math.pi / 2)
        cop(DVE, lambda: V.tensor_copy(decT[:], ssmc[:, MAG, :].unsqueeze(2).to_broadcast([128, 8, 128])))
        cop(DVE, lambda: V.memset(decT[:, :, 0:1], 0.0))
        c.barrier([cst_b])
        es3.close()
        sb = sb_main
        ring = [sb("ring%d" % i, [128, SLOT], BF16) for i in range(3)]
        ring_b = [Buf("ring%d" % i) for i in range(3)]
        ring_s = [c.sem("ring%d" % i) for i in range(3)]
        ring_ctr = [0]

        def piece(key):
            k = pidx[key]
            j = ring_ctr[0] % 3
            ring_ctr[0] += 1
            n = max(off + shp[0] * shp[1] for (_, _, off, shp, _) in pieces[k][1])
            c.dma(SP, ring[j][:, 0:n], wscr[k, :, 0:n], ring_s[j], reads=[wscr_b], writes=[ring_b[j]])
            return ring[j], ring_b[j]

        xt = sb("xt", [128, 4, D], F32); xt_b = Buf("xt")
        hT = sb("hT", [128, KC, 512], BF16); hT_b = Buf("hT")
        ss = sb("ss", [128, 8], F32); ss_b = Buf("ss")
        arena = sb("arena", [128, max(4 * S, FC * 256)], F32)
        actT = arena[:, 0:FC * 256].bitcast(BF16).rearrange("p (f n) -> p f n", f=FC); actT_b = Buf("actT")
        kT = sb("kT", [64, S], BF16); kT_b = Buf("kT")
        kiT = sb("kiT", [64, S], BF16); kiT_b = Buf("kiT")
        Vaug = sb("Vaug", [128, S // 128, 128], BF16); Vaug_b = Buf("Vaug")
        qaT = sb("qaT", [64, 8, 512], BF16); qaT_b = Buf("qaT")
        qiT = sb("qiT", [64, 4, 512], BF16); qiT_b = Buf("qiT")
        qmT = sb("qmT", [64, 4, 512], BF16); qmT_b = Buf("qmT")
        uT = sb("uT", [128, 2, 512], BF16); uT_b = Buf("uT")
        wq = sb("wq", [128, 4, 4], F32); wq_b = Buf("wq")
        Isc = [arena[:, i * S:(i + 1) * S] for i in range(4)]; Isc_b = [Buf("Isc%d" % i) for i in range(4)]
        MB = [sb("MB%d" % i, [128, S], BF16) for i in range(2)]; MB_b = [Buf("MB0"), Buf("MB1")]
        junkA = sb("junkA", [128, S], mybir.dt.float8e4); junkA_b = Buf("junkA")
        ET = [sb("ET%d" % i, [128, 512], BF16) for i in range(3)]; ET_b = [Buf("ET%d" % i) for i in range(3)]
        rec = sb("rec", [128, 512], F32); rec_b = Buf("rec")
        OaT = sb("OaT", [64, 8, 512], BF16); OaT_b = Buf("OaT")
        OmT = sb("OmT", [64, 4, 512], BF16); OmT_b = Buf("OmT")
        ysT = sb("ysT", [128, 2, 512], BF16); ysT_b = Buf("ysT")
        mgT = sb("mgT", [128, 8, 512], BF16); mgT_b = Buf("mgT")
        junk = mgT[:].rearrange("p f n -> p (f n)"); junk_b = mgT_b
        mkT = sb("mkT", [64, 4, 256], BF16); mkT_b = Buf("mkT")
        Vm = sb("Vm", [128, 2, 4, 128], BF16); Vm_b = Buf("Vm")
        memT = hT[:, :, 0:256]; memT_b = hT_b
        blo = sb("blo", [128, 4], F32); bw = sb("bw", [128, 4], F32); bmid = sb("bmid", [128, 4], F32)
        bcnt = sb("bcnt", [128, 4], F32); bpw = sb("bpw", [128, 4], F32); bmx = sb("bmx", [128, 4], F32)
        blo_b, bw_b, bmid_b, bcnt_b, bpw_b, bmx_b = (Buf(n) for n in ("blo", "bw", "bmid", "bcnt", "bpw", "bmx"))
        bthr = sb("bthr", [128, 4], F32); bthr_b = Buf("bthr")
        bcntA = sb("bcntA", [128, 4], F32); bcntA_b = Buf("bcntA")
        s_init = sb("s_init", [128, 2, 8], F32); s_init_b = Buf("s_init")
        stmp = [sb("stmp%d" % i, [128, 512], F32) for i in range(5)]; stmp_b = [Buf("stmp%d" % i) for i in range(5)]
        xrb = sb("xrb", [128, 2, 2, 512], BF16); xrb_b = [Buf("xrb0"), Buf("xrb1")]
        ssm_small = sb("ssm_small", [128, 4, 4], F32); ssm_small_b = Buf("ssm_small")
        yf = sb("yf", [128, 128], F32); yf_b = Buf("yf")
        sg = [stmp[3], stmp[4]]; sg_b = [stmp_b[3], stmp_b[4]]
        rl = [stmp[3], stmp[4]]; rl_b = [stmp_b[3], stmp_b[4]]
        hrs = stmp[0][0:64, :]; hrs_b = stmp_b[0]
        hsq = stmp[1][0:64, 0:256].bitcast(BF16); hsq_b = stmp_b[1]
        xs = stmp[2][:, :].bitcast(BF16); xs_b = stmp_b[2]
        c.op(DVE, lambda: V.memset(Vaug[:], 1.0), writes=[Vaug_b])
        c.op(DVE, lambda: V.memset(Vm[:], 1.0), writes=[Vm_b])

        def rms_to_hT(x_ap_fn, gi, dst, dst_b, ntile, src_b):
            for t in range(ntile):
                xa = x_ap_fn(t)
                c.op(ACT, lambda xa=xa, t=t: A.activation(out=junk[:, 0:D], in_=xa, func=AF.Square, accum_out=ss[:, t:t + 1]),
                     reads=[src_b], writes=[junk_b, ss_b])
                c.op(ACT, lambda t=t: A.activation(out=ss[:, 4 + t:5 + t], in_=ss[:, t:t + 1], func=AF.Ln, bias=epsc[:, 0:1], scale=1.0 / D),
                     reads=[ss_b], writes=[ss_b])
                c.op(ACT, lambda t=t: A.activation(out=ss[:, 4 + t:5 + t], in_=ss[:, 4 + t:5 + t], func=AF.Exp, scale=-0.5), reads=[ss_b], writes=[ss_b])
                c.op(DVE, lambda xa=xa, t=t: V.tensor_scalar(out=xs[:], in0=xa, scalar1=ss[:, 4 + t:5 + t], scalar2=None, op0=ALU.mult),
                     reads=[src_b, ss_b], writes=[xs_b])

                def tr():
                    ins = None
                    for kc in range(KC):
                        ins = T.transpose(psT[:, kc * 128:(kc + 1) * 128], xs[:, kc * 128:(kc + 1) * 128], ident[:])
                    return ins
                c.op(PE, tr, reads=[xs_b], writes=[psT_b])
                c.op(DVE, lambda t=t: V.tensor_tensor(out=dst[:, :, t * 128:(t + 1) * 128],
                                                      in0=psT[:].rearrange("p (c n) -> p c n", c=KC),
                                                      in1=gcol[:, gi, :].unsqueeze(2).to_broadcast([128, KC, 128]), op=ALU.mult),
                     reads=[psT_b], writes=[dst_b])

        def head_norm_a(bi, N):
            c.op(ACT, lambda: A.activation(out=hsq[:, 0:N], in_=banks[bi][0:64, 0:N], func=AF.Square), reads=[bank_b[bi]], writes=[hsq_b])

        def head_norm_b(bi, N, gidx, out_ap, out_b):
            b2 = nb()
            mm_group(banks[b2][0:64, 0:N], [(ones[0:64, 0:64], hsq[:, 0:N])], reads=[hsq_b], writes=[bank_b[b2]])
            c.op(ACT, lambda: A.activation(out=hrs[:, 0:N], in_=banks[b2][0:64, 0:N], func=AF.Ln, bias=epsc[0:64, 0:1], scale=1.0 / 64),
                 reads=[bank_b[b2]], writes=[hrs_b])
            c.op(ACT, lambda: A.activation(out=hrs[:, 0:N], in_=hrs[:, 0:N], func=AF.Exp, scale=-0.5), reads=[hrs_b], writes=[hrs_b])
            c.op(DVE, lambda: V.scalar_tensor_tensor(out=out_ap, in0=banks[bi][0:64, 0:N], scalar=hg[:, gidx:gidx + 1], in1=hrs[:, 0:N],
                                                     op0=ALU.mult, op1=ALU.mult), reads=[bank_b[bi], hrs_b], writes=[out_b])

        def head_pipeline(tasks):
            pend = None
            for (grp, N, gidx, out_ap, out_b) in tasks:
                bi = grp()
                if pend is not None:
                    head_norm_b(*pend)
                head_norm_a(bi, N)
                pend = (bi, N, gidx, out_ap, out_b)
            if pend is not None:
                head_norm_b(*pend)

        def ffn(tag, gi):
            c.barrier(Isc_b)
            rms_to_hT(lambda t: xt[:, t, :], gi, hT, hT_b, 4, xt_b)
            for i in range(11):
                wt, wb = piece((tag, "in", i))
                w4 = wt[:, 0:4096].rearrange("p (gu kc f) -> p gu kc f", gu=2, kc=8)
                for j in range(2):
                    f = 2 * i + j
                    bg, bu = nb(), nb()
                    mm_group(banks[bg][:, :], [(w4[:, 0, kc, j * 128:(j + 1) * 128], hT[:, kc, :]) for kc in range(KC)],
                             reads=[wb, hT_b], writes=[bank_b[bg]])
                    mm_group(banks[bu][:, :], [(w4[:, 1, kc, j * 128:(j + 1) * 128], hT[:, kc, :]) for kc in range(KC)],
                             reads=[wb, hT_b], writes=[bank_b[bu]])
                    si = f % 2
                    c.op(ACT, lambda bg=bg, si=si: A.activation(out=sg[si][:], in_=banks[bg][:, :], func=AF.Silu),
                         reads=[bank_b[bg]], writes=[sg_b[si]])
                    c.op(DVE, lambda bu=bu, si=si, f=f: V.tensor_tensor(out=actT[:, f, :], in0=sg[si][:], in1=banks[bu][:, :], op=ALU.mult),
                         reads=[sg_b[si], bank_b[bu]], writes=[actT_b])
            for hh in range(2):
                acc = [pin() for _ in range(4)]
                for pp in range(2):
                    wt, wb = piece((tag, "out", hh, pp))
                    w3 = wt[:, 0:5632].rearrange("p (f d) -> p f d", f=11)
                    for t in range(4):
                        mm_group(banks[acc[t]][:, :], [(actT[:, 11 * pp + fl, t * 128:(t + 1) * 128], w3[:, fl, :]) for fl in range(11)],
                                 reads=[wb, actT_b], writes=[bank_b[acc[t]]], first=(pp == 0), last=(pp == 1))
                for t in range(4):
                    xsl = xt[:, t, hh * 512:(hh + 1) * 512]
                    c.op(DVE, lambda t=t, xsl=xsl: V.scalar_tensor_tensor(out=xsl, in0=banks[acc[t]][:, :], scalar=0.5, in1=xsl,
                                                                          op0=ALU.mult, op1=ALU.add),
                         reads=[bank_b[acc[t]], xt_b], writes=[xt_b])
                    unpin(acc[t])

        for b in range(NB):
            for mc in range(2):
                c.dma(SP, xt[:, mc, :], mem_d[b * 256 + mc * 128: b * 256 + (mc + 1) * 128, :], sem_x, writes=[xt_b])
            rms_to_hT(lambda t: xt[:, t, :], 3, memT, memT_b, 2, xt_b)
            wt, wb = piece(("kv",))
            wk3 = wt[:, 0:4096].rearrange("p (kc d) -> p kc d", kc=8)
            def mk_grp(h):
                def g():
                    bi = nb()
                    mm_group(banks[bi][0:64, 0:256], [(wk3[:, kc, h * 64:(h + 1) * 64], memT[:, kc, :]) for kc in range(KC)],
                             reads=[wb, memT_b], writes=[bank_b[bi]])
                    return bi
                return g
            head_pipeline([(mk_grp(h), 256, 3, mkT[:, h, :], mkT_b) for h in range(4)])
            for mc in range(2):
                bi = nb()
                mm_group(banks[bi][:, 0:256], [(memT[:, kc, mc * 128:(mc + 1) * 128], wk3[:, kc, 256:512]) for kc in range(KC)],
                         reads=[wb, memT_b], writes=[bank_b[bi]])
                c.op(DVE, lambda mc=mc, bi=bi: V.tensor_copy(Vm[:, mc, :, 0:64], banks[bi][:, 0:256].rearrange("p (h d) -> p h d", h=4)),
                     reads=[bank_b[bi]], writes=[Vm_b])
            c.op(DVE, lambda: V.memset(s_init[:], 0.0), writes=[s_init_b])

            for st in range(NT):
                row0 = b * S + st * 512
                c.dma(SP, xt[:], x_d[row0:row0 + 512, :].rearrange("(t p) d -> p t d", p=128), sem_x, writes=[xt_b])
                ffn("f1", 0)
                if "x1" in dbg_d and b == 0:
                    dump("x1", xt[:], xt_b, dbg_d["x1"][st * 512:(st + 1) * 512, :].rearrange("(t p) d -> p t d", p=128))
                rms_to_hT(lambda t: xt[:, t, :], 1, hT, hT_b, 4, xt_b)
                wt, wb = piece(("p1",))
                w3 = wt[:, 0:4096].rearrange("p (kc d) -> p kc d", kc=8)
                def hd_grp(w3, wb, c0):
                    def g():
                        bi = nb()
                        mm_group(banks[bi][0:64, :], [(w3[:, kc, c0:c0 + 64], hT[:, kc, :]) for kc in range(KC)],
                                 reads=[wb, hT_b], writes=[bank_b[bi]])
                        return bi
                    return g
                head_pipeline([(hd_grp(w3, wb, h * 64), 512, 0, qaT[:, h, :], qaT_b) for h in range(8)])
                wt, wb = piece(("p2",))
                w3 = wt[:, 0:8 * 452].rearrange("p (kc d) -> p kc d", kc=8)
                head_pipeline([(hd_grp(w3, wb, 0), 512, 1, kT[:, st * 512:(st + 1) * 512], kT_b)])
                bi = nb()
                mm_group(banks[bi][0:64, :], [(w3[:, kc, 384:448], hT[:, kc, :]) for kc in range(KC)], reads=[wb, hT_b], writes=[bank_b[bi]])
                c.op(ACT, lambda bi=bi: A.copy(out=kiT[:, st * 512:(st + 1) * 512], in_=banks[bi][0:64, :]), reads=[bank_b[bi]], writes=[kiT_b])
                for h in range(4):
                    bi = nb()
                    mm_group(banks[bi][0:64, :], [(w3[:, kc, 128 + h * 64:192 + h * 64], hT[:, kc, :]) for kc in range(KC)],
                             reads=[wb, hT_b], writes=[bank_b[bi]])
                    c.op(ACT, lambda bi=bi, h=h: A.copy(out=qiT[:, h, :], in_=banks[bi][0:64, :]), reads=[bank_b[bi]], writes=[qiT_b])
                for t in range(4):
                    bi = nb()
                    mm_group(banks[bi][:, 0:64], [(hT[:, kc, t * 128:(t + 1) * 128], w3[:, kc, 64:128]) for kc in range(KC)],
                             reads=[wb, hT_b], writes=[bank_b[bi]])
                    mm_group(banks[bi][:, 64:68], [(hT[:, kc, t * 128:(t + 1) * 128], w3[:, kc, 448:452]) for kc in range(KC)],
                             reads=[wb, hT_b], writes=[bank_b[bi]])
                    c.op(DVE, lambda bi=bi, t=t: V.tensor_copy(Vaug[:, st * 4 + t, 0:64], banks[bi][:, 0:64]), reads=[bank_b[bi]], writes=[Vaug_b])
                    c.op(DVE, lambda bi=bi, t=t: V.tensor_scalar(out=wq[:, t, :], in0=banks[bi][:, 64:68], scalar1=0.0625, scalar2=None, op0=ALU.mult),
                         reads=[bank_b[bi]], writes=[wq_b])
                wt, wb = piece(("p3",))
                w3 = wt[:, 0:4096].rearrange("p (kc d) -> p kc d", kc=8)
                for m in range(2):
                    bi = nb()
                    mm_group(banks[bi][:, :], [(w3[:, kc, m * 128:(m + 1) * 128], hT[:, kc, :]) for kc in range(KC)],
                             reads=[wb, hT_b], writes=[bank_b[bi]])
                    c.op(ACT, lambda bi=bi, m=m: A.copy(out=uT[:, m, :], in_=banks[bi][:, :]), reads=[bank_b[bi]], writes=[uT_b])
                head_pipeline([(hd_grp(w3, wb, 256 + h * 64), 512, 2, qmT[:, h, :], qmT_b) for h in range(4)])

                c.barrier([actT_b])
                for t in range(4):
                    j = st * 4 + t
                    L = (j + 1) * 128
                    for kb in range((L + 511) // 512):
                        k0 = kb * 512
                        kn = min(512, L - k0)
                        for h in range(4):
                            bi = nb()
                            mm_group(banks[bi][:, 0:kn], [(qiT[:, h, t * 128:(t + 1) * 128], kiT[:, k0:k0 + kn])],
                                     reads=[qiT_b, kiT_b], writes=[bank_b[bi]])
                            if h == 0:
                                c.op(DVE, lambda bi=bi, t=t, k0=k0, kn=kn: V.tensor_scalar(
                                    out=Isc[t][:, k0:k0 + kn], in0=banks[bi][:, 0:kn], scalar1=0.0, scalar2=wq[:, t, 0:1], op0=ALU.max, op1=ALU.mult),
                                    reads=[bank_b[bi], wq_b], writes=[Isc_b[t]])
                            else:
                                ri = h % 2
                                c.op(ACT, lambda bi=bi, ri=ri, kn=kn: A.activation(out=rl[ri][:, 0:kn], in_=banks[bi][:, 0:kn], func=AF.Relu),
                                     reads=[bank_b[bi]], writes=[rl_b[ri]])
                                c.op(DVE, lambda ri=ri, t=t, h=h, k0=k0, kn=kn: V.scalar_tensor_tensor(
                                    out=Isc[t][:, k0:k0 + kn], in0=rl[ri][:, 0:kn], scalar=wq[:, t, h:h + 1], in1=Isc[t][:, k0:k0 + kn],
                                    op0=ALU.mult, op1=ALU.add), reads=[rl_b[ri], wq_b, Isc_b[t]], writes=[Isc_b[t]])
                    c.op(DVE, lambda t=t, L=L: V.tensor_reduce(out=bmx[:, t:t + 1], in_=Isc[t][:, 0:L], axis=mybir.AxisListType.X, op=ALU.max),
                         reads=[Isc_b[t]], writes=[bmx_b])
                    c.op(DVE, lambda t=t, L=L: V.tensor_reduce(out=blo[:, t:t + 1], in_=Isc[t][:, 0:L], axis=mybir.AxisListType.X, op=ALU.min),
                         reads=[Isc_b[t]], writes=[blo_b])
                    c.op(DVE, lambda t=t, L=L: V.memset(Isc[t][0:64, L - 64:L], -BIG), reads=[Isc_b[t]], writes=[Isc_b[t]])
                need = [((st * 4 + t + 1) * 128 > TOPK) for t in range(4)]
                Ls = [(st * 4 + t + 1) * 128 for t in range(4)]
                on_act = [False, True, True, False]
                if any(need):
                    for t in range(4):
                        thr = float(Ls[t] - 2 * TOPK) if on_act[t] else float(Ls[t] - TOPK)
                        c.op(DVE, lambda t=t, thr=thr: V.memset(bthr[:, t:t + 1], thr), writes=[bthr_b])
                    c.op(DVE, lambda: V.memset(bcnt[:], 0.0), writes=[bcnt_b])
                    c.op(DVE, lambda: V.tensor_tensor(out=bw[:], in0=bmx[:], in1=blo[:], op=ALU.subtract), reads=[bmx_b, blo_b], writes=[bw_b])
                    c.op(DVE, lambda: V.tensor_scalar(out=bpw[:], in0=bw[:], scalar1=1e-3, scalar2=1e-6, op0=ALU.mult, op1=ALU.add),
                         reads=[bw_b], writes=[bpw_b])
                    c.op(DVE, lambda: V.tensor_tensor(out=blo[:], in0=blo[:], in1=bpw[:], op=ALU.subtract), reads=[bpw_b, blo_b], writes=[blo_b])
                    c.op(DVE, lambda: V.tensor_tensor(out=bw[:], in0=bmx[:], in1=blo[:], op=ALU.subtract), reads=[bmx_b, blo_b], writes=[bw_b])
                    for it in range(NITER):
                        c.op(DVE, lambda: V.tensor_scalar(out=bw[:], in0=bw[:], scalar1=0.5, scalar2=None, op0=ALU.mult), reads=[bw_b], writes=[bw_b])
                        c.op(DVE, lambda: V.tensor_tensor(out=bmid[:], in0=blo[:], in1=bw[:], op=ALU.add), reads=[blo_b, bw_b], writes=[bmid_b])
                        for t in (1, 2, 3, 0):
                            if not need[t]:
                                continue
                            L = Ls[t]
                            if on_act[t]:
                                c.op(ACT, lambda t=t, L=L: A.activation(out=junkA[:, 0:L], in_=Isc[t][:, 0:L], func=AF.Sign, bias=bmid[:, t:t + 1], scale=-1.0, saturate=False,
                                                                        accum_out=bcntA[:, t:t + 1]),
                                     reads=[Isc_b[t], bmid_b], writes=[junkA_b, bcntA_b])
                            else:
                                c.op(DVE, lambda t=t, L=L: V.tensor_scalar(out=junk[:, 0:L], in0=Isc[t][:, 0:L], scalar1=bmid[:, t:t + 1], scalar2=0.0,
                                                                            op0=ALU.is_le, op1=ALU.add, accum_out=bcnt[:, t:t + 1]),
                                     reads=[Isc_b[t], bmid_b], writes=[junk_b, bcnt_b])
                        for t in range(4):
                            if need[t] and on_act[t]:
                                c.op(DVE, lambda t=t: V.tensor_copy(bcnt[:, t:t + 1], bcntA[:, t:t + 1]), reads=[bcntA_b, bcnt_b], writes=[bcnt_b])
                        c.op(DVE, lambda: V.tensor_tensor(out=bpw[:], in0=bcnt[:], in1=bthr[:], op=ALU.is_le), reads=[bcnt_b, bthr_b], writes=[bpw_b])
                        c.op(DVE, lambda: V.tensor_tensor(out=bpw[:], in0=bpw[:], in1=bw[:], op=ALU.mult), reads=[bpw_b, bw_b], writes=[bpw_b])
                        c.op(DVE, lambda: V.tensor_tensor(out=blo[:], in0=blo[:], in1=bpw[:], op=ALU.add), reads=[blo_b, bpw_b], writes=[blo_b])
                for t in range(4):
                    if not need[t]:
                        c.op(DVE, lambda t=t: V.memset(blo[:, t:t + 1], -1.0e29), reads=[blo_b], writes=[blo_b])
                if "isc" in dbg_d and b == 0 and st == NT - 1:
                    dump("isc", Isc[3][:, 0:S], Isc_b[3], dbg_d["isc"][:, :])
                    dump("lo", blo[:, :], blo_b, dbg_d["lo"][:, :])

                def emit_mask(t):
                    L = Ls[t]
                    m = t % 2
                    c.op(DVE, lambda: V.tensor_scalar(out=MB[m][:, 0:L], in0=Isc[t][:, 0:L], scalar1=blo[:, t:t + 1], scalar2=1.0,
                                                      op0=ALU.is_gt, op1=ALU.subtract), reads=[Isc_b[t], blo_b], writes=[MB_b[m]])

                def attention(t):
                    j = st * 4 + t
                    m = t % 2
                    O = [pin(), pin()]
                    steps = [(kc, g) for kc in range(j + 1) for g in range(2)]
                    LOOK = 2
                    for i in range(len(steps) + LOOK):
                        if i < len(steps):
                            kc, g = steps[i]
                            bi = nb()
                            prs = [(kT[:, kc * 128:(kc + 1) * 128], qaT[:, 4 * g:4 * g + 4, t * 128:(t + 1) * 128]),
                                   (MB[m][:, kc * 128:(kc + 1) * 128], I8big[:, :, :])]
                            if kc >= j - 1:
                                prs.append((ident[:], biasT[:, j - kc, 4 * g:4 * g + 4, :]))
                            mm_group(banks[bi][:, :], prs, reads=[kT_b, qaT_b, MB_b[m]], writes=[bank_b[bi]])
                            e = i % 3
                            c.op(ACT, lambda bi=bi, e=e: A.activation(out=ET[e][:], in_=banks[bi][:, :], func=AF.Exp), reads=[bank_b[bi]], writes=[ET_b[e]])
                        if i >= LOOK:
                            kc, g = steps[i - LOOK]
                            e = (i - LOOK) % 3
                            mm_group(banks[O[g]][:, :], [(Vaug[:, kc, :], ET[e][:])], reads=[Vaug_b, ET_b[e]], writes=[bank_b[O[g]]],
                                     first=(kc == 0), last=(kc == j))
                    for g in range(2):
                        c.op(ACT, lambda g=g: A.activation(out=rec[64:128, :], in_=banks[O[g]][64:128, :], func=AF.Ln), reads=[bank_b[O[g]]], writes=[rec_b])
                        c.op(ACT, lambda: A.activation(out=rec[64:128, :], in_=rec[64:128, :], func=AF.Exp, scale=-1.0), reads=[rec_b], writes=[rec_b])
                        c.op(DVE, lambda g=g: V.tensor_tensor(out=OaT[:, 4 * g:4 * g + 4, t * 128:(t + 1) * 128],
                                                             in0=banks[O[g]][0:64, :].rearrange("p (h q) -> p h q", h=4),
                                                             in1=rec[64:128, :].rearrange("p (h q) -> p h q", h=4), op=ALU.mult),
                             reads=[bank_b[O[g]], rec_b], writes=[OaT_b])
                        unpin(O[g])

                ssm_banks = {}

                def ssm_B(sub):
                    tk = slice(sub * 128, (sub + 1) * 128)
                    for hh in range(2):
                        pr_, pi_ = pin(), pin()
                        ssm_banks[(sub, hh)] = (pr_, pi_)
                        for ri, bi in ((0, pr_), (1, pi_)):
                            def fn(ri=ri, bi=bi, hh=hh):
                                ins = None
                                for cl in range(4):
                                    ins = T.matmul(banks[bi][:, cl * 128:(cl + 1) * 128], BpT[:, ri, 4 * hh + cl, :], uT[:, hh, tk], start=True, stop=True)
                                return ins
                            c.op(PE, fn, reads=[uT_b], writes=[bank_b[bi]])

                def ssm_dve(sub):
                    for hh in range(2):
                        pr_, pi_ = ssm_banks[(sub, hh)]
                        cs = cosT[:, 4 * hh:4 * hh + 4, :].rearrange("p c n -> p (c n)")
                        sn = sinT[:, 4 * hh:4 * hh + 4, :].rearrange("p c n -> p (c n)")
                        dc = decT[:, 4 * hh:4 * hh + 4, :].rearrange("p c n -> p (c n)")
                        s0, s1, s2, s3, s4 = stmp
                        z0, z1, z2, z3, z4 = stmp_b
                        PR, PI = banks[pr_][:, :], banks[pi_][:, :]
                        c.op(DVE, lambda: V.tensor_tensor(out=s0[:], in0=PR, in1=cs, op=ALU.mult), reads=[bank_b[pr_]], writes=[z0])
                        c.op(DVE, lambda: V.tensor_tensor(out=s1[:], in0=PI, in1=sn, op=ALU.mult), reads=[bank_b[pi_]], writes=[z1])
                        c.op(DVE, lambda: V.tensor_tensor(out=s0[:], in0=s0[:], in1=s1[:], op=ALU.add), reads=[z0, z1], writes=[z0])
                        c.op(DVE, lambda: V.tensor_tensor(out=s2[:], in0=PI, in1=cs, op=ALU.mult), reads=[bank_b[pi_]], writes=[z2])
                        c.op(DVE, lambda: V.tensor_tensor(out=s1[:], in0=PR, in1=sn, op=ALU.mult), reads=[bank_b[pr_], z1], writes=[z1])
                        c.op(DVE, lambda: V.tensor_tensor(out=s2[:], in0=s2[:], in1=s1[:], op=ALU.subtract), reads=[z2, z1], writes=[z2])
                        unpin(pr_); unpin(pi_)
                        s0v = s0[:].rearrange("p (c n) -> p c n", c=4)[:, :, 0]
                        s2v = s2[:].rearrange("p (c n) -> p c n", c=4)[:, :, 0]
                        c.op(DVE, lambda: V.tensor_tensor(out=s0v, in0=s0v, in1=s_init[:, 0, 4 * hh:4 * hh + 4], op=ALU.add), reads=[z0, s_init_b], writes=[z0])
                        c.op(DVE, lambda: V.tensor_tensor(out=s2v, in0=s2v, in1=s_init[:, 1, 4 * hh:4 * hh + 4], op=ALU.add), reads=[z2, s_init_b], writes=[z2])
                        c.op(DVE, lambda: V.tensor_tensor_scan(out=s3[:], data0=dc, data1=s0[:], initial=0.0, op0=ALU.mult, op1=ALU.add), reads=[z0], writes=[z3])
                        c.op(DVE, lambda: V.tensor_tensor_scan(out=s4[:], data0=dc, data1=s2[:], initial=0.0, op0=ALU.mult, op1=ALU.add), reads=[z2], writes=[z4])
                        c.op(DVE, lambda: V.tensor_tensor(out=s0[:], in0=s3[:], in1=cs, op=ALU.mult), reads=[z3, z0], writes=[z0])
                        c.op(DVE, lambda: V.tensor_tensor(out=s1[:], in0=s4[:], in1=sn, op=ALU.mult), reads=[z4, z1], writes=[z1])
                        c.op(DVE, lambda: V.tensor_tensor(out=s0[:], in0=s0[:], in1=s1[:], op=ALU.subtract), reads=[z0, z1], writes=[z0])
                        c.op(DVE, lambda: V.tensor_tensor(out=s2[:], in0=s3[:], in1=sn, op=ALU.mult), reads=[z3, z2], writes=[z2])
                        c.op(DVE, lambda: V.tensor_tensor(out=s1[:], in0=s4[:], in1=cs, op=ALU.mult), reads=[z4, z1], writes=[z1])
                        c.op(DVE, lambda: V.tensor_tensor(out=s2[:], in0=s2[:], in1=s1[:], op=ALU.add), reads=[z2, z1], writes=[z2])
                        c.op(ACT, lambda hh=hh: A.copy(out=xrb[:, hh, 0, :], in_=s0[:]), reads=[z0], writes=[xrb_b[hh]])
                        c.op(ACT, lambda hh=hh: A.copy(out=xrb[:, hh, 1, :], in_=s2[:]), reads=[z2], writes=[xrb_b[hh]])
                        xl_r = s0[:].rearrange("p (c n) -> p c n", c=4)[:, :, 127]
                        xl_i = s2[:].rearrange("p (c n) -> p c n", c=4)[:, :, 127]
                        cth = ssmc[:, COS, 4 * hh:4 * hh + 4]; sth = ssmc[:, SIN, 4 * hh:4 * hh + 4]; mg_ = ssmc[:, MAG, 4 * hh:4 * hh + 4]
                        q = ssm_small
                        c.op(DVE, lambda: V.tensor_tensor(out=q[:, 0, :], in0=xl_r, in1=cth, op=ALU.mult), reads=[z0], writes=[ssm_small_b])
                        c.op(DVE, lambda: V.tensor_tensor(out=q[:, 1, :], in0=xl_i, in1=sth, op=ALU.mult), reads=[z2], writes=[ssm_small_b])
                        c.op(DVE, lambda: V.tensor_tensor(out=q[:, 2, :], in0=xl_r, in1=sth, op=ALU.mult), reads=[z0], writes=[ssm_small_b])
                        c.op(DVE, lambda: V.tensor_tensor(out=q[:, 3, :], in0=xl_i, in1=cth, op=ALU.mult), reads=[z2], writes=[ssm_small_b])
                        c.op(DVE, lambda: V.tensor_tensor(out=q[:, 0, :], in0=q[:, 0, :], in1=q[:, 1, :], op=ALU.subtract), reads=[ssm_small_b], writes=[ssm_small_b])
                        c.op(DVE, lambda: V.tensor_tensor(out=q[:, 2, :], in0=q[:, 2, :], in1=q[:, 3, :], op=ALU.add), reads=[ssm_small_b], writes=[ssm_small_b])
                        c.op(DVE, lambda: V.tensor_tensor(out=s_init[:, 0, 4 * hh:4 * hh + 4], in0=q[:, 0, :], in1=mg_, op=ALU.mult), reads=[ssm_small_b], writes=[s_init_b])
                        c.op(DVE, lambda: V.tensor_tensor(out=s_init[:, 1, 4 * hh:4 * hh + 4], in0=q[:, 2, :], in1=mg_, op=ALU.mult), reads=[ssm_small_b], writes=[s_init_b])

                def ssm_C(sub):
                    tk = slice(sub * 128, (sub + 1) * 128)
                    for hh in range(2):
                        by = nb()
                        prs = []
                        for cl in range(4):
                            prs.append((Cp[:, 0, 4 * hh + cl, :], xrb[:, hh, 0, cl * 128:(cl + 1) * 128]))
                            prs.append((Cp[:, 1, 4 * hh + cl, :], xrb[:, hh, 1, cl * 128:(cl + 1) * 128]))
                        mm_group(banks[by][:, 0:128], prs, reads=[xrb_b[hh]], writes=[bank_b[by]])
                        c.op(DVE, lambda by=by, hh=hh: V.scalar_tensor_tensor(out=yf[:], in0=uT[:, hh, tk], scalar=dcol[:, hh:hh + 1], in1=banks[by][:, 0:128],
                                                                              op0=ALU.mult, op1=ALU.add), reads=[uT_b, bank_b[by]], writes=[yf_b])
                        c.op(ACT, lambda hh=hh: A.activation(out=ysT[:, hh, tk], in_=yf[:], func=AF.Gelu), reads=[yf_b], writes=[ysT_b])

                def mem_attention():
                    steps = [(h, mc) for h in range(4) for mc in range(2)]
                    om = {}
                    LOOK = 2
                    for i in range(len(steps) + LOOK):
                        if i < len(steps):
                            h, mc = steps[i]
                            bi = nb()
                            mm_group(banks[bi][:, :], [(mkT[:, h, mc * 128:(mc + 1) * 128], qmT[:, h, :])], reads=[mkT_b, qmT_b], writes=[bank_b[bi]])
                            e = i % 3
                            c.op(ACT, lambda bi=bi, e=e: A.activation(out=ET[e][:], in_=banks[bi][:, :], func=AF.Exp), reads=[bank_b[bi]], writes=[ET_b[e]])
                        if i >= LOOK:
                            h, mc = steps[i - LOOK]
                            e = (i - LOOK) % 3
                            if mc == 0:
                                om[h] = pin()
                            mm_group(banks[om[h]][:, :], [(Vm[:, mc, h, :], ET[e][:])], reads=[Vm_b, ET_b[e]], writes=[bank_b[om[h]]], first=(mc == 0), last=(mc == 1))
                            if mc == 1:
                                o_ = om[h]
                                c.op(ACT, lambda o_=o_: A.activation(out=rec[64:128, :], in_=banks[o_][64:128, :], func=AF.Ln), reads=[bank_b[o_]], writes=[rec_b])
                                c.op(ACT, lambda: A.activation(out=rec[64:128, :], in_=rec[64:128, :], func=AF.Exp, scale=-1.0), reads=[rec_b], writes=[rec_b])
                                c.op(DVE, lambda o_=o_, h=h: V.tensor_tensor(out=OmT[:, h, :], in0=banks[o_][0:64, :], in1=rec[64:128, :], op=ALU.mult),
                                     reads=[bank_b[o_], rec_b], writes=[OmT_b])
                                unpin(o_)

                emit_mask(0)
                emit_mask(1)
                attention(0)
                ssm_B(0); ssm_dve(0)
                emit_mask(2)
                attention(1)
                ssm_C(0); ssm_B(1); ssm_dve(1)
                emit_mask(3)
                attention(2)
                ssm_C(1); ssm_B(2); ssm_dve(2)
                attention(3)
                ssm_C(2); ssm_B(3); ssm_dve(3)
                mem_attention()
                ssm_C(3)

                for f in range(8):
                    wt, wb = piece(("mg", f))
                    wg = wt[:, 0:3072].rearrange("p (br kc d) -> p br kc d", br=3, kc=8)
                    woa = wt[0:64, 3072:4096].rearrange("p (h d) -> p h d", h=8)
                    wgl = wt[:, 4096:4608].rearrange("p (ab kc d) -> p ab kc d", ab=2, kc=2)
                    wom = wt[0:64, 4608:5120].rearrange("p (h d) -> p h d", h=4)
                    gb = [nb() for _ in range(3)]
                    for br in range(3):
                        mm_group(banks[gb[br]][:, :], [(wg[:, br, kc, :], hT[:, kc, :]) for kc in range(KC)], reads=[wb, hT_b], writes=[bank_b[gb[br]]])
                    ba = nb()
                    mm_group(banks[ba][:, :], [(woa[:, h, :], OaT[:, h, :]) for h in range(8)], reads=[wb, OaT_b], writes=[bank_b[ba]])
                    bga, bgb = nb(), nb()
                    mm_group(banks[bga][:, :], [(wgl[:, 0, kc, :], ysT[:, kc, :]) for kc in range(2)], reads=[wb, ysT_b], writes=[bank_b[bga]])
                    mm_group(banks[bgb][:, :], [(wgl[:, 1, kc, :], ysT[:, kc, :]) for kc in range(2)], reads=[wb, ysT_b], writes=[bank_b[bgb]])
                    bm = nb()
                    mm_group(banks[bm][:, :], [(wom[:, h, :], OmT[:, h, :]) for h in range(4)], reads=[wb, OmT_b], writes=[bank_b[bm]])
                    m0, m1, m2 = stmp[0], stmp[1], stmp[2]
                    y0, y1, y2 = stmp_b[0], stmp_b[1], stmp_b[2]
                    c.op(ACT, lambda: A.activation(out=m0[:], in_=banks[gb[0]][:, :], func=AF.Sigmoid), reads=[bank_b[gb[0]]], writes=[y0])
                    c.op(DVE, lambda: V.tensor_tensor(out=m0[:], in0=m0[:], in1=banks[ba][:, :], op=ALU.mult), reads=[y0, bank_b[ba]], writes=[y0])
                    c.op(ACT, lambda: A.activation(out=m1[:], in_=banks[bgb][:, :], func=AF.Sigmoid), reads=[bank_b[bgb]], writes=[y1])
                    c.op(DVE, lambda: V.tensor_tensor(out=m1[:], in0=m1[:], in1=banks[bga][:, :], op=ALU.mult), reads=[y1, bank_b[bga]], writes=[y1])
                    c.op(ACT, lambda: A.activation(out=m2[:], in_=banks[gb[1]][:, :], func=AF.Sigmoid), reads=[bank_b[gb[1]]], writes=[y2])
                    c.op(DVE, lambda: V.tensor_tensor(out=m1[:], in0=m1[:], in1=m2[:], op=ALU.mult), reads=[y1, y2], writes=[y1])
                    c.op(DVE, lambda: V.tensor_tensor(out=m0[:], in0=m0[:], in1=m1[:], op=ALU.add), reads=[y0, y1], writes=[y0])
                    c.op(ACT, lambda: A.activation(out=m2[:], in_=banks[gb[2]][:, :], func=AF.Sigmoid), reads=[bank_b[gb[2]]], writes=[y2])
                    c.op(DVE, lambda: V.tensor_tensor(out=m2[:], in0=m2[:], in1=banks[bm][:, :], op=ALU.mult), reads=[y2, bank_b[bm]], writes=[y2])
                    c.op(DVE, lambda f=f: V.tensor_tensor(out=mgT[:, f, :], in0=m0[:], in1=m2[:], op=ALU.add), reads=[y0, y2], writes=[mgT_b])
                for hh in range(2):
                    wt, wb = piece(("wo", hh))
                    w3 = wt[:, 0:4096].rearrange("p (kc d) -> p kc d", kc=8)
                    for t in range(4):
                        bi = nb()
                        mm_group(banks[bi][:, :], [(mgT[:, kc, t * 128:(t + 1) * 128], w3[:, kc, :]) for kc in range(KC)],
                                 reads=[wb, mgT_b], writes=[bank_b[bi]])
                        xsl = xt[:, t, hh * 512:(hh + 1) * 512]
                        c.op(DVE, lambda bi=bi, xsl=xsl: V.tensor_tensor(out=xsl, in0=xsl, in1=banks[bi][:, :], op=ALU.add),
                             reads=[bank_b[bi], xt_b], writes=[xt_b])
                if "x2" in dbg_d and b == 0:
                    dump("x2", xt[:], xt_b, dbg_d["x2"][st * 512:(st + 1) * 512, :].rearrange("(t p) d -> p t d", p=128))
                ffn("f2", 2)
                for t in range(4):
                    c.op(ACT, lambda t=t: A.activation(out=junk[:, 0:D], in_=xt[:, t, :], func=AF.Square, accum_out=ss[:, t:t + 1]),
                         reads=[xt_b], writes=[junk_b, ss_b])
                    c.op(ACT, lambda t=t: A.activation(out=ss[:, 4 + t:5 + t], in_=ss[:, t:t + 1], func=AF.Ln, bias=epsc[:, 0:1], scale=1.0 / D),
                         reads=[ss_b], writes=[ss_b])
                    c.op(ACT, lambda t=t: A.activation(out=ss[:, 4 + t:5 + t], in_=ss[:, 4 + t:5 + t], func=AF.Exp, scale=-0.5), reads=[ss_b], writes=[ss_b])
                    c.op(DVE, lambda t=t: V.scalar_tensor_tensor(out=xt[:, t, :], in0=xt[:, t, :], scalar=ss[:, 4 + t:5 + t], in1=gfin[:],
                                                                 op0=ALU.mult, op1=ALU.mult), reads=[xt_b, ss_b], writes=[xt_b])
                c.dma(POOL, out_d[row0:row0 + 512, :].rearrange("(t p) d -> p t d", p=128), xt[:], sem_out, reads=[xt_b])
        G.wait_ge(sem_out.h, sem_out.n)
        if sem_misc.n:
            G.wait_ge(sem_misc.h, sem_misc.n)
    return nc


_CACHE = {}


def _run(inputs, NB, S, dbg=()):
    key = (NB, S, str(dbg))
    if key not in _CACHE:
        _CACHE[key] = build(NB, S, dbg)
    nc = _CACHE[key]
    x = np.ascontiguousarray(inputs["x"], dtype=np.float32)
    mem = np.ascontiguousarray(inputs["mem"], dtype=np.float32)
    ncore = 8
    wmap = {n: np.ascontiguousarray(np.asarray(inputs[n], dtype=np.float32).reshape(WSHAPES[n])) for n in WNAMES}
    wmap["onehot"] = _onehot_table()
    in_maps = []
    for i in range(ncore):
        m = dict(wmap)
        m["x"] = x[i * NB:(i + 1) * NB].reshape(NB * S, D)
        m["mem"] = mem[i * NB:(i + 1) * NB].reshape(NB * 256, D)
        in_maps.append(m)
    res = run_bass_kernel_spmd(nc, in_maps, core_ids=list(range(ncore)))
    return res


def kernel(**inputs):
    B, S = inputs["x"].shape[0], inputs["x"].shape[1]
    NB = B // 8
    res = _run(inputs, NB, S)
    out = np.concatenate([r["out"].reshape(NB, S, D) for r in res.results], axis=0)
    return out.astype(np.float32)
```

```python
import math
from contextlib import ExitStack
import numpy as np
import concourse.bass as bass
import concourse.mybir as mybir
from concourse.bass_utils import run_bass_kernel_spmd

F32 = mybir.dt.float32
BF16 = mybir.dt.bfloat16
I32 = mybir.dt.int32
AF = mybir.ActivationFunctionType
ALU = mybir.AluOpType

D = 1024
KC = 8
FFN = 2816
FC = 22
WIN = 4548
EPS = 1e-6
NEG = 30000.0
BIG = 1.0e30
SLOT = 5632
NITER = 22


class Tok:
    __slots__ = ("sem", "val")

    def __init__(self, sem, val):
        self.sem = sem
        self.val = val


class Sem:
    def __init__(self, h):
        self.h = h
        self.n = 0


class Buf:
    def __init__(self, name):
        self.name = name
        self.w = None
        self.r = {}


class Eng:
    def __init__(self, eng, sem):
        self.eng = eng
        self.sem = sem
        self.waited = {}

    def wait(self, tok):
        if tok is None:
            return
        k = id(tok.sem)
        if self.waited.get(k, 0) >= tok.val:
            return
        self.eng.wait_ge(tok.sem.h, tok.val)
        self.waited[k] = tok.val


class Ctx:
    def __init__(self, nc, es):
        self.nc = nc
        self.es = es
        self.PE = Eng(nc.tensor, self.sem("pe"))
        self.ACT = Eng(nc.scalar, self.sem("act"))
        self.DVE = Eng(nc.vector, self.sem("dve"))
        self.POOL = Eng(nc.gpsimd, self.sem("pool"))
        self.SP = Eng(nc.sync, self.sem("sp"))

    def sem(self, name):
        return Sem(self.es.enter_context(self.nc.semaphore(name)))

    def _deps(self, E, reads, writes):
        for b in reads:
            E.wait(b.w)
        pe = E is self.PE
        for b in writes:
            if b.w is not None and not (pe and b.w.sem is E.sem):
                E.wait(b.w)
            for t in b.r.values():
                if not (pe and t.sem is E.sem):
                    E.wait(t)

    def _commit(self, tok, reads, writes):
        for b in reads:
            b.r[id(tok.sem)] = tok
        for b in writes:
            b.w = tok
            b.r = {}

    def op(self, E, fn, reads=(), writes=()):
        self._deps(E, reads, writes)
        ins = fn()
        E.sem.n += 1
        ins.then_inc(E.sem.h, 1)
        tok = Tok(E.sem, E.sem.n)
        self._commit(tok, reads, writes)
        return tok

    def dma(self, Q, out, in_, sem, reads=(), writes=(), **kw):
        self._deps(Q, reads, writes)
        ins = Q.eng.dma_start(out=out, in_=in_, **kw)
        sem.n += 16
        ins.then_inc(sem.h, 16)
        tok = Tok(sem, sem.n)
        self._commit(tok, reads, writes)
        return tok

    def barrier(self, bufs):
        for E in (self.PE, self.ACT, self.DVE, self.POOL, self.SP):
            for b in bufs:
                E.wait(b.w)
                for t in b.r.values():
                    E.wait(t)


def _t5_bucket_np(rel):
    half = 16
    max_exact = 8
    n = np.abs(rel)
    nf = np.maximum(n, 1).astype(np.float32)
    large = max_exact + (np.log(nf / np.float32(max_exact)) / np.float32(math.log(128 / 8))
                         * np.float32(half - max_exact)).astype(np.int32)
    large = np.minimum(large, half - 1)
    return np.where(rel > 0, half, 0) + np.where(n < max_exact, n, large)


def _onehot_table():
    rel = np.arange(-255, 129, dtype=np.int32)
    bk = _t5_bucket_np(rel)
    oh = np.zeros((32, 384), np.float32)
    oh[bk, np.arange(384)] = 1.0
    return oh


WNAMES = ["ffn1_norm", "ffn1_w_in", "ffn1_w_out", "mix_norm", "w_in", "a_q_gain", "a_k_gain", "rel_bias",
          "w_o_a", "ssm_lambda_re", "ssm_lambda_im", "ssm_log_dt", "ssm_b_re", "ssm_b_im", "ssm_c_re",
          "ssm_c_im", "ssm_d", "w_glu", "mem_norm", "w_mem_kv", "m_q_gain", "m_k_gain", "w_o_m", "w_out",
          "ffn2_norm", "ffn2_w_in", "ffn2_w_out", "final_norm"]
WSHAPES = {"ffn1_norm": [1, D], "ffn1_w_in": [D, 2 * FFN], "ffn1_w_out": [FFN, D], "mix_norm": [1, D],
           "w_in": [D, WIN], "a_q_gain": [64, 1], "a_k_gain": [64, 1], "rel_bias": [32, 8],
           "w_o_a": [512, D], "ssm_lambda_re": [16, 64], "ssm_lambda_im": [16, 64], "ssm_log_dt": [1, 16],
           "ssm_b_re": [16, 64, 16], "ssm_b_im": [16, 64, 16], "ssm_c_re": [16, 16, 64],
           "ssm_c_im": [16, 16, 64], "ssm_d": [256, 1], "w_glu": [256, 2 * D], "mem_norm": [1, D],
           "w_mem_kv": [D, 512], "m_q_gain": [64, 1], "m_k_gain": [64, 1], "w_o_m": [256, D],
           "w_out": [D, D], "ffn2_norm": [1, D], "ffn2_w_in": [D, 2 * FFN], "ffn2_w_out": [FFN, D],
           "final_norm": [1, D]}


def build(NB, S, dbg=()):
    NT = S // 512
    TOPK = min(256, S // 4)
    nc = bass.Bass("TRN2", target_bir_lowering=False)
    x_d = nc.dram_tensor("x", [NB * S, D], F32, kind="ExternalInput").ap()
    mem_d = nc.dram_tensor("mem", [NB * 256, D], F32, kind="ExternalInput").ap()
    W = {n: nc.dram_tensor(n, WSHAPES[n], F32, kind="ExternalInput").ap() for n in WNAMES}
    oh_d = nc.dram_tensor("onehot", [32, 384], F32, kind="ExternalInput").ap()
    out_d = nc.dram_tensor("out", [NB * S, D], F32, kind="ExternalOutput").ap()
    dbg_d = {n: nc.dram_tensor("dbg_" + n, shp, F32, kind="ExternalOutput").ap() for n, shp in dbg}

    pieces = []

    def ffn_pieces(tag, win, wout):
        wi4 = win.rearrange("(kc p) (gu f) -> p kc gu f", p=128, gu=2)
        for i in range(11):
            srcs = [(0, 128, gu * 2048, [8, 256], wi4[:, :, gu, 256 * i:256 * i + 256]) for gu in range(2)]
            pieces.append(((tag, "in", i), srcs))
        wo3 = wout.rearrange("(f p) d -> p f d", p=128)
        for hh in range(2):
            for pp in range(2):
                pieces.append(((tag, "out", hh, pp),
                               [(0, 128, 0, [11, 512], wo3[:, 11 * pp:11 * pp + 11, 512 * hh:512 * hh + 512])]))

    win3 = W["w_in"].rearrange("(kc p) c -> p kc c", p=128)
    ffn_pieces("f1", W["ffn1_w_in"], W["ffn1_w_out"])
    pieces.append((("p1",), [(0, 128, 0, [8, 512], win3[:, :, 0:512])]))
    pieces.append((("p2",), [(0, 128, 0, [8, 452], win3[:, :, 512:964])]))
    pieces.append((("p3",), [(0, 128, 0, [8, 512], win3[:, :, 964:1476])]))
    woa3 = W["w_o_a"].rearrange("(h p) d -> p h d", p=64)
    wglu3 = W["w_glu"].rearrange("(kc p) d -> p kc d", p=128)
    wom3 = W["w_o_m"].rearrange("(h p) d -> p h d", p=64)
    for f in range(8):
        srcs = []
        for br in range(3):
            c0 = 1476 + br * 1024 + f * 128
            srcs.append((0, 128, br * 1024, [8, 128], win3[:, :, c0:c0 + 128]))
        srcs.append((0, 64, 3072, [8, 128], woa3[:, :, f * 128:f * 128 + 128]))
        for ab in range(2):
            srcs.append((0, 128, 4096 + ab * 256, [2, 128], wglu3[:, :, ab * 1024 + f * 128:ab * 1024 + f * 128 + 128]))
        srcs.append((0, 64, 4608, [4, 128], wom3[:, :, f * 128:f * 128 + 128]))
        pieces.append((("mg", f), srcs))
    wout3 = W["w_out"].rearrange("(kc p) d -> p kc d", p=128)
    for hh in range(2):
        pieces.append((("wo", hh), [(0, 128, 0, [8, 512], wout3[:, :, 512 * hh:512 * hh + 512])]))
    ffn_pieces("f2", W["ffn2_w_in"], W["ffn2_w_out"])
    wkv3 = W["w_mem_kv"].rearrange("(kc p) d -> p kc d", p=128)
    pieces.append((("kv",), [(0, 128, 0, [8, 512], wkv3[:, :, :])]))
    pidx = {k: i for i, (k, _) in enumerate(pieces)}
    NP = len(pieces)
    wscr = nc.dram_tensor("wscr", [NP, 128, SLOT], BF16, kind="Internal").ap()
    fd_d = nc.dram_tensor("fdscr", [8, 384], F32, kind="Internal").ap()

    with ExitStack() as es:
        c = Ctx(nc, es)
        PE, ACT, DVE, POOL, SP = c.PE, c.ACT, c.DVE, c.POOL, c.SP
        V, A, G, T = nc.vector, nc.scalar, nc.gpsimd, nc.tensor

        def sb(name, shape, dt):
            return es.enter_context(nc.sbuf_tensor(name, shape, dt))

        wscr_b = Buf("wscr")
        sem_pro = c.sem("pro")
        sem_const = c.sem("const")
        sem_out = c.sem("outst")
        sem_x = c.sem("xld")
        sem_misc = c.sem("misc")

        ident_f = sb("ident_f", [128, 128], F32)
        ident = sb("ident", [128, 128], BF16)
        antiJ = sb("antiJ", [128, 128], BF16)
        I8big = sb("I8big", [128, 4, 128], BF16)
        ones = sb("ones", [128, 128], BF16)
        epsc = sb("epsc", [128, 1], F32)
        gcol = sb("gcol", [128, 4, KC], F32)
        gfin = sb("gfin", [128, D], F32)
        hg = sb("hg", [64, 4], F32)
        biasT = sb("biasT", [128, 2, 8, 128], BF16)
        cst_b = Buf("const")
        cosT = sb("cosT", [128, 8, 128], F32); sinT = sb("sinT", [128, 8, 128], F32)
        decT = sb("decT", [128, 8, 128], F32)
        BpT = sb("BpT", [128, 2, 8, 128], BF16)
        Cp = sb("Cp", [128, 2, 8, 128], BF16)
        ssmc = sb("ssmc", [128, 12, 8], F32)
        dcol = sb("dcol", [128, 2], F32)

        banks = [es.enter_context(nc.psum_tensor("bank%d" % i, [128, 512], F32)) for i in range(7)]
        bank_b = [Buf("bank%d" % i) for i in range(7)]
        psT = es.enter_context(nc.psum_tensor("psT", [128, 1024], BF16)); psT_b = Buf("psT")
        pinned = set()
        rr = [0]

        def nb():
            while True:
                i = rr[0] % 7
                rr[0] += 1
                if i not in pinned:
                    return i

        def pin():
            i = nb()
            pinned.add(i)
            return i

        def unpin(i):
            pinned.discard(i)

        def mm_group(out_ap, pairs, reads, writes, first=True, last=True):
            def fn():
                ins = None
                n = len(pairs)
                for i, (l, r) in enumerate(pairs):
                    ins = T.matmul(out_ap, l, r, start=(first and i == 0), stop=(last and i == n - 1))
                return ins
            return c.op(PE, fn, reads=reads, writes=writes)

        def dump(name, ap_sb, buf, dst):
            if name in dbg_d:
                c.dma(POOL, dst, ap_sb, sem_misc, reads=[buf])

        def cdma(out, in_, **kw):
            c.dma(SP, out, in_, sem_const, writes=[cst_b], **kw)

        for gi, nm in enumerate(["ffn1_norm", "mix_norm", "ffn2_norm", "mem_norm"]):
            cdma(gcol[:, gi, :], W[nm].rearrange("o (c p) -> p (o c)", p=128), allow_slow_non_contiguous=True)
        cdma(gfin[:], W["final_norm"][0:1, :].partition_broadcast(128))
        for gi, nm in enumerate(["a_q_gain", "a_k_gain", "m_q_gain", "m_k_gain"]):
            cdma(hg[:, gi:gi + 1], W[nm][:, :])
        es3 = ExitStack()
        sb_main = sb

        def sb(name, shape, dt):
            return es3.enter_context(nc.sbuf_tensor(name, shape, dt))
        rb_sb = sb("rb_sb", [32, 8], F32); oh_sb = sb("oh_sb", [32, 384], F32); rb15 = sb("rb15", [8, 1], F32)
        fsb = sb("fsb", [8, 384], F32); hank = sb("hank", [128, 2, 8, 128], F32); hankb = sb("hankb", [128, 2, 8, 128], BF16)
        cdma(rb_sb[:], W["rel_bias"][:, :])
        cdma(oh_sb[:], oh_d[:, :])
        cdma(rb15[:], W["rel_bias"][15:16, :].rearrange("o h -> h o"), allow_slow_non_contiguous=True)
        for t in range(2):
            cdma(ssmc[t * 64:(t + 1) * 64, 0, :], W["ssm_lambda_re"].rearrange("(c t) s -> t s c", t=2)[t], allow_slow_non_contiguous=True)
            cdma(ssmc[t * 64:(t + 1) * 64, 1, :], W["ssm_lambda_im"].rearrange("(c t) s -> t s c", t=2)[t], allow_slow_non_contiguous=True)
            cdma(ssmc[t * 64:(t + 1) * 64, 2, :], W["ssm_log_dt"].rearrange("o (c t) -> t o c", t=2)[t].partition_broadcast(64), allow_slow_non_contiguous=True)
        braw = sb("braw", [128, 2, 8, 16], F32); craw = sb("craw", [128, 2, 8, 16], F32)
        for t in range(2):
            cdma(braw[t * 64:(t + 1) * 64, 0, :, :], W["ssm_b_re"].rearrange("(c t) s h -> t s c h", t=2)[t])
            cdma(braw[t * 64:(t + 1) * 64, 1, :, :], W["ssm_b_im"].rearrange("(c t) s h -> t s c h", t=2)[t])
            for cc in range(8):
                cdma(craw[t * 64:(t + 1) * 64, 0, cc, :], W["ssm_c_re"].rearrange("(c t) o s -> t c s o", t=2)[t, cc], allow_slow_non_contiguous=True)
                cdma(craw[t * 64:(t + 1) * 64, 1, cc, :], W["ssm_c_im"].rearrange("(c t) o s -> t c s o", t=2)[t, cc], allow_slow_non_contiguous=True)
        cdma(dcol[:, :], W["ssm_d"].rearrange("(m p) o -> p (m o)", p=128), allow_slow_non_contiguous=True)

        def cop(E, fn):
            return c.op(E, fn, reads=[cst_b], writes=[cst_b])

        cop(POOL, lambda: G.memset(ident_f[:], 1.0))
        cop(POOL, lambda: G.affine_select(out=ident_f[:], in_=ident_f[:], pattern=[[-1, 128]], compare_op=ALU.is_equal,
                                          fill=0.0, base=0, channel_multiplier=1))
        cop(DVE, lambda: V.tensor_copy(ident[:], ident_f[:]))
        for i in range(4):
            cop(DVE, lambda i=i: V.tensor_scalar(out=I8big[:, i, :], in0=ident_f[:], scalar1=NEG, scalar2=None, op0=ALU.mult))
        cop(POOL, lambda: G.memset(ident_f[:], 1.0))
        cop(POOL, lambda: G.affine_select(out=ident_f[:], in_=ident_f[:], pattern=[[1, 128]], compare_op=ALU.is_equal,
                                          fill=0.0, base=-127, channel_multiplier=1))
        cop(DVE, lambda: V.tensor_copy(antiJ[:], ident_f[:]))
        cop(DVE, lambda: V.memset(ones[:], 1.0))
        cop(DVE, lambda: V.memset(epsc[:], EPS))
        cop(DVE, lambda: V.tensor_scalar(out=hg[:, 0:1], in0=hg[:, 0:1], scalar1=0.125, scalar2=None, op0=ALU.mult))
        cop(DVE, lambda: V.tensor_scalar(out=hg[:, 2:3], in0=hg[:, 2:3], scalar1=0.125, scalar2=None, op0=ALU.mult))
        b0 = 0
        c.op(PE, lambda: T.matmul(banks[b0][0:8, 0:384], rb_sb[:, :], oh_sb[:, :], start=True, stop=True),
             reads=[cst_b], writes=[bank_b[b0]])
        c.op(DVE, lambda: V.tensor_scalar(out=fsb[:], in0=banks[b0][0:8, 0:384], scalar1=rb15[:, 0:1], scalar2=None,
                                          op0=ALU.subtract), reads=[bank_b[b0], cst_b], writes=[cst_b])
        fd_b = Buf("fd")
        c.dma(SP, fd_d[:, :], fsb[:], sem_const, reads=[cst_b], writes=[fd_b])
        for di, delta in enumerate((0, 128)):
            src = bass.AP(tensor=fd_d.tensor, offset=128 - delta, ap=[[1, 128], [384, 8], [1, 128]])
            c.dma(SP, hank[:, di, :, :], src, sem_const, reads=[fd_b], writes=[cst_b])
        cop(DVE, lambda: V.tensor_copy(hankb[:], hank[:]))
        for di in range(2):
            for h in range(8):
                bi = nb()
                c.op(PE, lambda di=di, h=h, bi=bi: T.matmul(banks[bi][:, 0:128], hankb[:, di, h, :], antiJ[:], start=True, stop=True),
                     reads=[cst_b], writes=[bank_b[bi]])
                c.op(DVE, lambda di=di, h=h, bi=bi: V.tensor_copy(biasT[:, di, h, :], banks[bi][:, 0:128]),
                     reads=[bank_b[bi], cst_b], writes=[cst_b])

        LR, LI, LDT, DT, MAG, TH, COS, SIN, FR, FI, T1, T2 = range(12)
        sc = lambda i: ssmc[:, i, :]
        cop(ACT, lambda: A.activation(out=sc(DT), in_=sc(LDT), func=AF.Exp))
        cop(DVE, lambda: V.tensor_tensor(out=sc(T1), in0=sc(LR), in1=sc(DT), op=ALU.mult))
        cop(ACT, lambda: A.activation(out=sc(MAG), in_=sc(T1), func=AF.Exp))
        cop(DVE, lambda: V.tensor_tensor(out=sc(TH), in0=sc(LI), in1=sc(DT), op=ALU.mult))
        MAGIC = 12582912.0
        TWO_PI = 2.0 * math.pi

        def sin_of(out_ap, in_ap, tmp_ap, shift):
            cop(DVE, lambda: V.tensor_scalar(out=tmp_ap, in0=in_ap, scalar1=shift, scalar2=1.0 / TWO_PI, op0=ALU.add, op1=ALU.mult))
            cop(DVE, lambda: V.tensor_scalar(out=tmp_ap, in0=tmp_ap, scalar1=MAGIC, scalar2=None, op0=ALU.add))
            cop(DVE, lambda: V.tensor_scalar(out=tmp_ap, in0=tmp_ap, scalar1=-MAGIC, scalar2=-TWO_PI, op0=ALU.add, op1=ALU.mult))
            cop(DVE, lambda: V.scalar_tensor_tensor(out=tmp_ap, in0=in_ap, scalar=shift, in1=tmp_ap, op0=ALU.add, op1=ALU.add))
            cop(DVE, lambda: V.tensor_scalar(out=tmp_ap, in0=tmp_ap, scalar1=math.pi, scalar2=-math.pi, op0=ALU.min, op1=ALU.max))
            cop(ACT, lambda: A.activation(out=out_ap, in_=tmp_ap, func=AF.Sin))

        sin_of(sc(SIN), sc(TH), sc(T1), 0.0)
        sin_of(sc(COS), sc(TH), sc(T1), math.pi / 2)
        arr = sb("arr", [128, 6, 8], F32)
        a_ = lambda i: arr[:, i, :]
        cop(DVE, lambda: V.tensor_tensor(out=a_(0), in0=sc(MAG), in1=sc(COS), op=ALU.mult))
        cop(DVE, lambda: V.tensor_scalar(out=a_(0), in0=a_(0), scalar1=-1.0, scalar2=None, op0=ALU.add))
        cop(DVE, lambda: V.tensor_tensor(out=a_(1), in0=sc(MAG), in1=sc(SIN), op=ALU.mult))
        cop(DVE, lambda: V.tensor_tensor(out=a_(2), in0=sc(LR), in1=sc(LR), op=ALU.mult))
        cop(DVE, lambda: V.tensor_tensor(out=a_(3), in0=sc(LI), in1=sc(LI), op=ALU.mult))
        cop(DVE, lambda: V.tensor_tensor(out=a_(2), in0=a_(2), in1=a_(3), op=ALU.add))
        cop(DVE, lambda: V.reciprocal(out=a_(5), in_=a_(2)))
        cop(DVE, lambda: V.tensor_tensor(out=a_(3), in0=a_(0), in1=sc(LR), op=ALU.mult))
        cop(DVE, lambda: V.tensor_tensor(out=a_(4), in0=a_(1), in1=sc(LI), op=ALU.mult))
        cop(DVE, lambda: V.tensor_tensor(out=a_(3), in0=a_(3), in1=a_(4), op=ALU.add))
        cop(DVE, lambda: V.tensor_tensor(out=sc(FR), in0=a_(3), in1=a_(5), op=ALU.mult))
        cop(DVE, lambda: V.tensor_tensor(out=a_(3), in0=a_(1), in1=sc(LR), op=ALU.mult))
        cop(DVE, lambda: V.tensor_tensor(out=a_(4), in0=a_(0), in1=sc(LI), op=ALU.mult))
        cop(DVE, lambda: V.tensor_tensor(out=a_(3), in0=a_(3), in1=a_(4), op=ALU.subtract))
        cop(DVE, lambda: V.tensor_tensor(out=sc(FI), in0=a_(3), in1=a_(5), op=ALU.mult))
        bbar = sb("bbar", [128, 2, 8, 16], F32); btmp = sb("btmp", [128, 8, 16], F32)
        frb = ssmc[:, FR, :].unsqueeze(2).to_broadcast([128, 8, 16])
        fib = ssmc[:, FI, :].unsqueeze(2).to_broadcast([128, 8, 16])
        cop(DVE, lambda: V.tensor_tensor(out=bbar[:, 0, :, :], in0=braw[:, 0, :, :], in1=frb, op=ALU.mult))
        cop(DVE, lambda: V.tensor_tensor(out=btmp[:], in0=braw[:, 1, :, :], in1=fib, op=ALU.mult))
        cop(DVE, lambda: V.tensor_tensor(out=bbar[:, 0, :, :], in0=bbar[:, 0, :, :], in1=btmp[:], op=ALU.subtract))
        cop(DVE, lambda: V.tensor_tensor(out=bbar[:, 1, :, :], in0=braw[:, 1, :, :], in1=frb, op=ALU.mult))
        cop(DVE, lambda: V.tensor_tensor(out=btmp[:], in0=braw[:, 0, :, :], in1=fib, op=ALU.mult))
        cop(DVE, lambda: V.tensor_tensor(out=bbar[:, 1, :, :], in0=bbar[:, 1, :, :], in1=btmp[:], op=ALU.add))
        xpad = sb("xpad", [128, 128], BF16)
        for ri in range(2):
            for cc in range(8):
                cop(DVE, lambda: V.memset(xpad[:], 0.0))
                for t in range(2):
                    col = 32 * (cc % 4) + 16 * t
                    cop(DVE, lambda ri=ri, cc=cc, t=t, col=col: V.tensor_copy(xpad[t * 64:(t + 1) * 64, col:col + 16],
                                                                               bbar[t * 64:(t + 1) * 64, ri, cc, :]))
                c.op(PE, lambda: T.transpose(psT[:, 0:128], xpad[:], ident[:]), reads=[cst_b], writes=[psT_b])
                c.op(DVE, lambda ri=ri, cc=cc: V.tensor_copy(BpT[:, ri, cc, :], psT[:, 0:128]), reads=[psT_b, cst_b], writes=[cst_b])
        cop(DVE, lambda: V.memset(Cp[:], 0.0))
        for ri in range(2):
            for cc in range(8):
                for t in range(2):
                    col = 16 * ((2 * cc + t) % 8)
                    cop(DVE, lambda ri=ri, cc=cc, t=t, col=col: V.tensor_scalar(
                        out=Cp[t * 64:(t + 1) * 64, ri, cc, col:col + 16], in0=craw[t * 64:(t + 1) * 64, ri, cc, :],
                        scalar1=(1.0 if ri == 0 else -1.0), scalar2=None, op0=ALU.mult))
        tau_i = sb("tau_i", [128, 128], I32); tau = sb("tau", [128, 128], F32); ang = sb("ang", [128, 8, 128], F32)
        atmp = sb("atmp", [128, 8, 128], F32)
        cop(POOL, lambda: G.iota(tau_i[:], pattern=[[1, 128]], base=0, channel_multiplier=0))
        cop(DVE, lambda: V.tensor_copy(tau[:], tau_i[:]))
        for cc in range(8):
            cop(DVE, lambda cc=cc: V.tensor_scalar(out=ang[:, cc, :], in0=tau[:], scalar1=ssmc[:, TH, cc:cc + 1], scalar2=None, op0=ALU.mult))
        sin_of(sinT[:], ang[:], atmp[:], 0.0)
        sin_of(cosT[:], ang[:], atmp[:], math.pi / 2)
        cop(DVE, lambda: V.tensor_copy(decT[:], ssmc[:, MAG, :].unsqueeze(2).to_broadcast([128, 8, 128])))
        cop(DVE, lambda: V.memset(decT[:, :, 0:1], 0.0))
        NSTG = 4
        with ExitStack() as es2:
            stg32 = [es2.enter_context(nc.sbuf_tensor("stg32_%d" % i, [128, SLOT], F32)) for i in range(NSTG)]
            stg16 = [es2.enter_context(nc.sbuf_tensor("stg16_%d" % i, [128, SLOT], BF16)) for i in range(NSTG)]
            b32 = [Buf("s32_%d" % i) for i in range(NSTG)]
            b16 = [Buf("s16_%d" % i) for i in range(NSTG)]
            s32sem = [c.sem("s32_%d" % i) for i in range(NSTG)]
            for i in range(NSTG):
                c.op(POOL, lambda i=i: G.memset(stg32[i][:], 0.0), writes=[b32[i]])
            for k, (key, srcs) in enumerate(pieces):
                j = k % NSTG
                n = 0
                for (p0, p1, off, shp, src) in srcs:
                    sz = shp[0] * shp[1]
                    dst = stg32[j][p0:p1, off:off + sz].rearrange("p (a b) -> p a b", a=shp[0])
                    c.dma(SP, dst, src, s32sem[j], writes=[b32[j]])
                    n = max(n, off + sz)
                E = (ACT, DVE)[k % 2]
                if E is ACT:
                    c.op(ACT, lambda j=j, n=n: A.copy(out=stg16[j][:, 0:n], in_=stg32[j][:, 0:n]),
                         reads=[b32[j]], writes=[b16[j]])
                elif E is DVE:
                    c.op(DVE, lambda j=j, n=n: V.tensor_copy(stg16[j][:, 0:n], stg32[j][:, 0:n]),
                         reads=[b32[j]], writes=[b16[j]])
                else:
                    c.op(POOL, lambda j=j, n=n: G.tensor_copy(stg16[j][:, 0:n], stg32[j][:, 0:n]),
                         reads=[b32[j]], writes=[b16[j]])
                c.dma(POOL, wscr[k, :, 0:n], stg16[j][:, 0:n], sem_pro, reads=[b16[j]], writes=[wscr_b])
            c.barrier(b32 + b16)

        c.barrier([cst_b])
        es3.close()
        sb = sb_main
        ring = [sb("ring%d" % i, [128, SLOT], BF16) for i in range(3)]
        ring_b = [Buf("ring%d" % i) for i in range(3)]
        ring_s = [c.sem("ring%d" % i) for i in range(3)]
        ring_ctr = [0]

        def piece(key):
            k = pidx[key]
            j = ring_ctr[0] % 3
            ring_ctr[0] += 1
            n = max(off + shp[0] * shp[1] for (_, _, off, shp, _) in pieces[k][1])
            c.dma(SP, ring[j][:, 0:n], wscr[k, :, 0:n], ring_s[j], reads=[wscr_b], writes=[ring_b[j]])
            return ring[j], ring_b[j]

        xt = sb("xt", [128, 4, D], F32); xt_b = [Buf("xt%d" % i) for i in range(4)]
        hT = sb("hT", [128, KC, 512], BF16); hT_b = Buf("hT")
        ss = sb("ss", [128, 8], F32); ss_b = Buf("ss")
        arena = sb("arena", [128, max(4 * S, FC * 256)], F32)
        actT = arena[:, 0:FC * 256].bitcast(BF16).rearrange("p (f n) -> p f n", f=FC); actT_b = Buf("actT")
        kT = sb("kT", [64, S], BF16); kT_b = Buf("kT")
        kiT = sb("kiT", [64, S], BF16); kiT_b = Buf("kiT")
        Vaug = sb("Vaug", [128, S // 128, 128], BF16); Vaug_b = Buf("Vaug")
        qaT = sb("qaT", [64, 8, 512], BF16); qaT_b = Buf("qaT")
        qiT = sb("qiT", [64, 4, 512], BF16); qiT_b = Buf("qiT")
        qmT = sb("qmT", [64, 4, 512], BF16); qmT_b = Buf("qmT")
        uT = sb("uT", [128, 2, 512], BF16); uT_b = Buf("uT")
        wq = sb("wq", [128, 4, 4], F32); wq_b = Buf("wq")
        Isc = [arena[:, i * S:(i + 1) * S] for i in range(4)]; Isc_b = [Buf("Isc%d" % i) for i in range(4)]
        MB = [sb("MB%d" % i, [128, S], BF16) for i in range(2)]; MB_b = [Buf("MB0"), Buf("MB1")]
        junkA = sb("junkA", [128, S], mybir.dt.float8e4); junkA_b = Buf("junkA")
        ET = [sb("ET%d" % i, [128, 512], BF16) for i in range(3)]; ET_b = [Buf("ET%d" % i) for i in range(3)]
        rec = sb("rec", [128, 512], F32); rec_b = Buf("rec")
        OaT = sb("OaT", [64, 8, 512], BF16); OaT_b = Buf("OaT")
        OmT = sb("OmT", [64, 4, 512], BF16); OmT_b = Buf("OmT")
        ysT = sb("ysT", [128, 2, 512], BF16); ysT_b = Buf("ysT")
        mgT = sb("mgT", [128, 8, 512], BF16); mgT_b = Buf("mgT")
        junk = mgT[:].rearrange("p f n -> p (f n)"); junk_b = mgT_b
        mkT = sb("mkT", [64, 4, 256], BF16); mkT_b = Buf("mkT")
        Vm = sb("Vm", [128, 2, 4, 128], BF16); Vm_b = Buf("Vm")
        memT = hT[:, :, 0:256]; memT_b = hT_b
        blo = sb("blo", [128, 4], F32); bw = sb("bw", [128, 4], F32); bmid = sb("bmid", [128, 4], F32)
        bcnt = sb("bcnt", [128, 4], F32); bpw = sb("bpw", [128, 4], F32); bmx = sb("bmx", [128, 4], F32)
        blo_b, bw_b, bmid_b, bcnt_b, bpw_b, bmx_b = (Buf(n) for n in ("blo", "bw", "bmid", "bcnt", "bpw", "bmx"))
        bthr = sb("bthr", [128, 4], F32); bthr_b = Buf("bthr")
        bcnt_c = [Buf("bcnt%d" % i) for i in range(4)]
        s_init = sb("s_init", [128, 2, 8], F32); s_init_b = Buf("s_init")
        stmp = [sb("stmp%d" % i, [128, 512], F32) for i in range(5)]; stmp_b = [Buf("stmp%d" % i) for i in range(5)]
        xrb = sb("xrb", [128, 2, 2, 512], BF16); xrb_b = [Buf("xrb0"), Buf("xrb1")]
        ssm_small = sb("ssm_small", [128, 4, 4], F32); ssm_small_b = Buf("ssm_small")
        yf = sb("yf", [128, 128], F32); yf_b = Buf("yf")
        sg = [stmp[3], stmp[4]]; sg_b = [stmp_b[3], stmp_b[4]]
        rl = [stmp[3], stmp[4]]; rl_b = [stmp_b[3], stmp_b[4]]
        hrs = stmp[0][0:64, :]; hrs_b = stmp_b[0]
        hsq = stmp[1][0:64, 0:256].bitcast(BF16); hsq_b = stmp_b[1]
        xs = stmp[2][:, :].bitcast(BF16); xs_b = stmp_b[2]
        c.op(DVE, lambda: V.memset(Vaug[:], 1.0), writes=[Vaug_b])
        c.op(DVE, lambda: V.memset(Vm[:], 1.0), writes=[Vm_b])

        def rms_to_hT(x_ap_fn, gi, dst, dst_b, ntile, src_b):
            for t in range(ntile):
                xa = x_ap_fn(t)
                c.op(ACT, lambda xa=xa, t=t: A.activation(out=junk[:, 0:D], in_=xa, func=AF.Square, accum_out=ss[:, t:t + 1]),
                     reads=[src_b[t]], writes=[junk_b, ss_b])
                c.op(ACT, lambda t=t: A.activation(out=ss[:, 4 + t:5 + t], in_=ss[:, t:t + 1], func=AF.Ln, bias=epsc[:, 0:1], scale=1.0 / D),
                     reads=[ss_b], writes=[ss_b])
                c.op(ACT, lambda t=t: A.activation(out=ss[:, 4 + t:5 + t], in_=ss[:, 4 + t:5 + t], func=AF.Exp, scale=-0.5), reads=[ss_b], writes=[ss_b])
                c.op(DVE, lambda xa=xa, t=t: V.tensor_scalar(out=xs[:], in0=xa, scalar1=ss[:, 4 + t:5 + t], scalar2=None, op0=ALU.mult),
                     reads=[src_b[t], ss_b], writes=[xs_b])

                def tr():
                    ins = None
                    for kc in range(KC):
                        ins = T.transpose(psT[:, kc * 128:(kc + 1) * 128], xs[:, kc * 128:(kc + 1) * 128], ident[:])
                    return ins
                c.op(PE, tr, reads=[xs_b], writes=[psT_b])
                c.op(DVE, lambda t=t: V.tensor_tensor(out=dst[:, :, t * 128:(t + 1) * 128],
                                                      in0=psT[:].rearrange("p (c n) -> p c n", c=KC),
                                                      in1=gcol[:, gi, :].unsqueeze(2).to_broadcast([128, KC, 128]), op=ALU.mult),
                     reads=[psT_b], writes=[dst_b])

        def head_norm_a(bi, N):
            c.op(ACT, lambda: A.activation(out=hsq[:, 0:N], in_=banks[bi][0:64, 0:N], func=AF.Square), reads=[bank_b[bi]], writes=[hsq_b])

        def head_norm_b(bi, N, gidx, out_ap, out_b):
            b2 = nb()
            mm_group(banks[b2][0:64, 0:N], [(ones[0:64, 0:64], hsq[:, 0:N])], reads=[hsq_b], writes=[bank_b[b2]])
            c.op(ACT, lambda: A.activation(out=hrs[:, 0:N], in_=banks[b2][0:64, 0:N], func=AF.Ln, bias=epsc[0:64, 0:1], scale=1.0 / 64),
                 reads=[bank_b[b2]], writes=[hrs_b])
            c.op(ACT, lambda: A.activation(out=hrs[:, 0:N], in_=hrs[:, 0:N], func=AF.Exp, scale=-0.5), reads=[hrs_b], writes=[hrs_b])
            c.op(DVE, lambda: V.scalar_tensor_tensor(out=out_ap, in0=banks[bi][0:64, 0:N], scalar=hg[:, gidx:gidx + 1], in1=hrs[:, 0:N],
                                                     op0=ALU.mult, op1=ALU.mult), reads=[bank_b[bi], hrs_b], writes=[out_b])

        def head_pipeline(tasks):
            pend = None
            for (grp, N, gidx, out_ap, out_b) in tasks:
                bi = grp()
                if pend is not None:
                    head_norm_b(*pend)
                head_norm_a(bi, N)
                pend = (bi, N, gidx, out_ap, out_b)
            if pend is not None:
                head_norm_b(*pend)

        def ffn(tag, gi):
            c.barrier(Isc_b)
            rms_to_hT(lambda t: xt[:, t, :], gi, hT, hT_b, 4, xt_b)
            for i in range(11):
                wt, wb = piece((tag, "in", i))
                w4 = wt[:, 0:4096].rearrange("p (gu kc f) -> p gu kc f", gu=2, kc=8)
                for j in range(2):
                    f = 2 * i + j
                    bg, bu = nb(), nb()
                    mm_group(banks[bg][:, :], [(w4[:, 0, kc, j * 128:(j + 1) * 128], hT[:, kc, :]) for kc in range(KC)],
                             reads=[wb, hT_b], writes=[bank_b[bg]])
                    mm_group(banks[bu][:, :], [(w4[:, 1, kc, j * 128:(j + 1) * 128], hT[:, kc, :]) for kc in range(KC)],
                             reads=[wb, hT_b], writes=[bank_b[bu]])
                    si = f % 2
                    c.op(ACT, lambda bg=bg, si=si: A.activation(out=sg[si][:], in_=banks[bg][:, :], func=AF.Silu),
                         reads=[bank_b[bg]], writes=[sg_b[si]])
                    c.op(DVE, lambda bu=bu, si=si, f=f: V.tensor_tensor(out=actT[:, f, :], in0=sg[si][:], in1=banks[bu][:, :], op=ALU.mult),
                         reads=[sg_b[si], bank_b[bu]], writes=[actT_b])
            for hh in range(2):
                acc = [pin() for _ in range(4)]
                for pp in range(2):
                    wt, wb = piece((tag, "out", hh, pp))
                    w3 = wt[:, 0:5632].rearrange("p (f d) -> p f d", f=11)
                    for t in range(4):
                        mm_group(banks[acc[t]][:, :], [(actT[:, 11 * pp + fl, t * 128:(t + 1) * 128], w3[:, fl, :]) for fl in range(11)],
                                 reads=[wb, actT_b], writes=[bank_b[acc[t]]], first=(pp == 0), last=(pp == 1))
                for t in range(4):
                    xsl = xt[:, t, hh * 512:(hh + 1) * 512]
                    c.op(DVE, lambda t=t, xsl=xsl: V.scalar_tensor_tensor(out=xsl, in0=banks[acc[t]][:, :], scalar=0.5, in1=xsl,
                                                                          op0=ALU.mult, op1=ALU.add),
                         reads=[bank_b[acc[t]], xt_b[t]], writes=[xt_b[t]])
                    unpin(acc[t])

        for b in range(NB):
            for mc in range(2):
                c.dma(SP, xt[:, mc, :], mem_d[b * 256 + mc * 128: b * 256 + (mc + 1) * 128, :], sem_x, writes=[xt_b[mc]])
            rms_to_hT(lambda t: xt[:, t, :], 3, memT, memT_b, 2, xt_b)
            wt, wb = piece(("kv",))
            wk3 = wt[:, 0:4096].rearrange("p (kc d) -> p kc d", kc=8)
            def mk_grp(h):
                def g():
                    bi = nb()
                    mm_group(banks[bi][0:64, 0:256], [(wk3[:, kc, h * 64:(h + 1) * 64], memT[:, kc, :]) for kc in range(KC)],
                             reads=[wb, memT_b], writes=[bank_b[bi]])
                    return bi
                return g
            head_pipeline([(mk_grp(h), 256, 3, mkT[:, h, :], mkT_b) for h in range(4)])
            for mc in range(2):
                bi = nb()
                mm_group(banks[bi][:, 0:256], [(memT[:, kc, mc * 128:(mc + 1) * 128], wk3[:, kc, 256:512]) for kc in range(KC)],
                         reads=[wb, memT_b], writes=[bank_b[bi]])
                c.op(DVE, lambda mc=mc, bi=bi: V.tensor_copy(Vm[:, mc, :, 0:64], banks[bi][:, 0:256].rearrange("p (h d) -> p h d", h=4)),
                     reads=[bank_b[bi]], writes=[Vm_b])
            c.op(DVE, lambda: V.memset(s_init[:], 0.0), writes=[s_init_b])

            for st in range(NT):
                row0 = b * S + st * 512
                for t in range(4):
                    c.dma(SP, xt[:, t, :], x_d[row0 + t * 128:row0 + (t + 1) * 128, :], sem_x, writes=[xt_b[t]])
                ffn("f1", 0)
                if "x1" in dbg_d and b == 0:
                    for t in range(4):
                        dump("x1", xt[:, t, :], xt_b[t], dbg_d["x1"][st * 512 + t * 128:st * 512 + (t + 1) * 128, :])
                rms_to_hT(lambda t: xt[:, t, :], 1, hT, hT_b, 4, xt_b)
                def emit_mask(t):
                    L = Ls[t]
                    m = t % 2
                    c.op(DVE, lambda: V.tensor_scalar(out=MB[m][:, 0:L], in0=Isc[t][:, 0:L], scalar1=blo[:, t:t + 1], scalar2=1.0,
                                                      op0=ALU.is_gt, op1=ALU.subtract), reads=[Isc_b[t], blo_b], writes=[MB_b[m]])

                def attention(t):
                    j = st * 4 + t
                    m = t % 2
                    O = [pin(), pin()]
                    steps = [(kc, g) for kc in range(j + 1) for g in range(2)]
                    LOOK = 2
                    for i in range(len(steps) + LOOK):
                        if i < len(steps):
                            kc, g = steps[i]
                            bi = nb()
                            prs = [(kT[:, kc * 128:(kc + 1) * 128], qaT[:, 4 * g:4 * g + 4, t * 128:(t + 1) * 128]),
                                   (MB[m][:, kc * 128:(kc + 1) * 128], I8big[:, :, :])]
                            if kc >= j - 1:
                                prs.append((ident[:], biasT[:, j - kc, 4 * g:4 * g + 4, :]))
                            mm_group(banks[bi][:, :], prs, reads=[kT_b, qaT_b, MB_b[m]], writes=[bank_b[bi]])
                            e = i % 3
                            c.op(ACT, lambda bi=bi, e=e: A.activation(out=ET[e][:], in_=banks[bi][:, :], func=AF.Exp), reads=[bank_b[bi]], writes=[ET_b[e]])
                        if i >= LOOK:
                            kc, g = steps[i - LOOK]
                            e = (i - LOOK) % 3
                            mm_group(banks[O[g]][:, :], [(Vaug[:, kc, :], ET[e][:])], reads=[Vaug_b, ET_b[e]], writes=[bank_b[O[g]]],
                                     first=(kc == 0), last=(kc == j))
                    for g in range(2):
                        c.op(ACT, lambda g=g: A.activation(out=rec[64:128, :], in_=banks[O[g]][64:128, :], func=AF.Ln), reads=[bank_b[O[g]]], writes=[rec_b])
                        c.op(ACT, lambda: A.activation(out=rec[64:128, :], in_=rec[64:128, :], func=AF.Exp, scale=-1.0), reads=[rec_b], writes=[rec_b])
                        c.op(DVE, lambda g=g: V.tensor_tensor(out=OaT[:, 4 * g:4 * g + 4, t * 128:(t + 1) * 128],
                                                             in0=banks[O[g]][0:64, :].rearrange("p (h q) -> p h q", h=4),
                                                             in1=rec[64:128, :].rearrange("p (h q) -> p h q", h=4), op=ALU.mult),
                             reads=[bank_b[O[g]], rec_b], writes=[OaT_b])
                        unpin(O[g])

                ssm_banks = {}

                def ssm_B(sub):
                    tk = slice(sub * 128, (sub + 1) * 128)
                    for hh in range(2):
                        pr_, pi_ = pin(), pin()
                        ssm_banks[(sub, hh)] = (pr_, pi_)
                        for ri, bi in ((0, pr_), (1, pi_)):
                            def fn(ri=ri, bi=bi, hh=hh):
                                ins = None
                                for cl in range(4):
                                    ins = T.matmul(banks[bi][:, cl * 128:(cl + 1) * 128], BpT[:, ri, 4 * hh + cl, :], uT[:, hh, tk], start=True, stop=True)
                                return ins
                            c.op(PE, fn, reads=[uT_b], writes=[bank_b[bi]])

                def ssm_dve(sub):
                    for hh in range(2):
                        pr_, pi_ = ssm_banks[(sub, hh)]
                        cs = cosT[:, 4 * hh:4 * hh + 4, :].rearrange("p c n -> p (c n)")
                        sn = sinT[:, 4 * hh:4 * hh + 4, :].rearrange("p c n -> p (c n)")
                        dc = decT[:, 4 * hh:4 * hh + 4, :].rearrange("p c n -> p (c n)")
                        s0, s1, s2, s3, s4 = stmp
                        z0, z1, z2, z3, z4 = stmp_b
                        PR, PI = banks[pr_][:, :], banks[pi_][:, :]
                        c.op(DVE, lambda: V.tensor_tensor(out=s0[:], in0=PR, in1=cs, op=ALU.mult), reads=[bank_b[pr_]], writes=[z0])
                        c.op(DVE, lambda: V.tensor_tensor(out=s1[:], in0=PI, in1=sn, op=ALU.mult), reads=[bank_b[pi_]], writes=[z1])
                        c.op(DVE, lambda: V.tensor_tensor(out=s0[:], in0=s0[:], in1=s1[:], op=ALU.add), reads=[z0, z1], writes=[z0])
                        c.op(DVE, lambda: V.tensor_tensor(out=s2[:], in0=PI, in1=cs, op=ALU.mult), reads=[bank_b[pi_]], writes=[z2])
                        c.op(DVE, lambda: V.tensor_tensor(out=s1[:], in0=PR, in1=sn, op=ALU.mult), reads=[bank_b[pr_], z1], writes=[z1])
                        c.op(DVE, lambda: V.tensor_tensor(out=s2[:], in0=s2[:], in1=s1[:], op=ALU.subtract), reads=[z2, z1], writes=[z2])
                        unpin(pr_); unpin(pi_)
                        s0v = s0[:].rearrange("p (c n) -> p c n", c=4)[:, :, 0]
                        s2v = s2[:].rearrange("p (c n) -> p c n", c=4)[:, :, 0]
                        c.op(DVE, lambda: V.tensor_tensor(out=s0v, in0=s0v, in1=s_init[:, 0, 4 * hh:4 * hh + 4], op=ALU.add), reads=[z0, s_init_b], writes=[z0])
                        c.op(DVE, lambda: V.tensor_tensor(out=s2v, in0=s2v, in1=s_init[:, 1, 4 * hh:4 * hh + 4], op=ALU.add), reads=[z2, s_init_b], writes=[z2])
                        c.op(DVE, lambda: V.tensor_tensor_scan(out=s3[:], data0=dc, data1=s0[:], initial=0.0, op0=ALU.mult, op1=ALU.add), reads=[z0], writes=[z3])
                        c.op(DVE, lambda: V.tensor_tensor_scan(out=s4[:], data0=dc, data1=s2[:], initial=0.0, op0=ALU.mult, op1=ALU.add), reads=[z2], writes=[z4])
                        q = ssm_small
                        lastc = lambda ap: ap.rearrange("p (c n) -> p c n", c=4)[:, :, 127]
                        c.op(DVE, lambda: V.tensor_tensor(out=s0[:], in0=s3[:], in1=cs, op=ALU.mult), reads=[z3, z0], writes=[z0])
                        c.op(DVE, lambda: V.tensor_tensor(out=s1[:], in0=s4[:], in1=sn, op=ALU.mult), reads=[z4, z1], writes=[z1])
                        c.op(DVE, lambda hh=hh: V.tensor_tensor(out=xrb[:, hh, 0, :], in0=s0[:], in1=s1[:], op=ALU.subtract), reads=[z0, z1], writes=[xrb_b[hh]])
                        c.op(DVE, lambda: V.tensor_tensor(out=q[:, 0, :], in0=lastc(s0[:]), in1=lastc(s1[:]), op=ALU.subtract), reads=[z0, z1], writes=[ssm_small_b])
                        c.op(DVE, lambda: V.tensor_tensor(out=s2[:], in0=s3[:], in1=sn, op=ALU.mult), reads=[z3, z2], writes=[z2])
                        c.op(DVE, lambda: V.tensor_tensor(out=s1[:], in0=s4[:], in1=cs, op=ALU.mult), reads=[z4, z1], writes=[z1])
                        c.op(DVE, lambda hh=hh: V.tensor_tensor(out=xrb[:, hh, 1, :], in0=s2[:], in1=s1[:], op=ALU.add), reads=[z2, z1], writes=[xrb_b[hh]])
                        c.op(DVE, lambda: V.tensor_tensor(out=q[:, 2, :], in0=lastc(s2[:]), in1=lastc(s1[:]), op=ALU.add), reads=[z2, z1], writes=[ssm_small_b])
                        cth = ssmc[:, COS, 4 * hh:4 * hh + 4]; sth = ssmc[:, SIN, 4 * hh:4 * hh + 4]; mg_ = ssmc[:, MAG, 4 * hh:4 * hh + 4]
                        c.op(DVE, lambda: V.tensor_tensor(out=q[:, 1, :], in0=q[:, 2, :], in1=sth, op=ALU.mult), reads=[ssm_small_b], writes=[ssm_small_b])
                        c.op(DVE, lambda: V.tensor_tensor(out=q[:, 3, :], in0=q[:, 2, :], in1=cth, op=ALU.mult), reads=[ssm_small_b], writes=[ssm_small_b])
                        c.op(DVE, lambda: V.tensor_tensor(out=q[:, 2, :], in0=q[:, 0, :], in1=sth, op=ALU.mult), reads=[ssm_small_b], writes=[ssm_small_b])
                        c.op(DVE, lambda: V.tensor_tensor(out=q[:, 0, :], in0=q[:, 0, :], in1=cth, op=ALU.mult), reads=[ssm_small_b], writes=[ssm_small_b])
                        c.op(DVE, lambda: V.tensor_tensor(out=q[:, 0, :], in0=q[:, 0, :], in1=q[:, 1, :], op=ALU.subtract), reads=[ssm_small_b], writes=[ssm_small_b])
                        c.op(DVE, lambda: V.tensor_tensor(out=q[:, 2, :], in0=q[:, 2, :], in1=q[:, 3, :], op=ALU.add), reads=[ssm_small_b], writes=[ssm_small_b])
                        c.op(DVE, lambda: V.tensor_tensor(out=s_init[:, 0, 4 * hh:4 * hh + 4], in0=q[:, 0, :], in1=mg_, op=ALU.mult), reads=[ssm_small_b], writes=[s_init_b])
                        c.op(DVE, lambda: V.tensor_tensor(out=s_init[:, 1, 4 * hh:4 * hh + 4], in0=q[:, 2, :], in1=mg_, op=ALU.mult), reads=[ssm_small_b], writes=[s_init_b])

                def ssm_C(sub):
                    tk = slice(sub * 128, (sub + 1) * 128)
                    for hh in range(2):
                        by = nb()
                        prs = []
                        for cl in range(4):
                            prs.append((Cp[:, 0, 4 * hh + cl, :], xrb[:, hh, 0, cl * 128:(cl + 1) * 128]))
                            prs.append((Cp[:, 1, 4 * hh + cl, :], xrb[:, hh, 1, cl * 128:(cl + 1) * 128]))
                        mm_group(banks[by][:, 0:128], prs, reads=[xrb_b[hh]], writes=[bank_b[by]])
                        c.op(DVE, lambda by=by, hh=hh: V.scalar_tensor_tensor(out=yf[:], in0=uT[:, hh, tk], scalar=dcol[:, hh:hh + 1], in1=banks[by][:, 0:128],
                                                                              op0=ALU.mult, op1=ALU.add), reads=[uT_b, bank_b[by]], writes=[yf_b])
                        c.op(ACT, lambda hh=hh: A.activation(out=ysT[:, hh, tk], in_=yf[:], func=AF.Gelu), reads=[yf_b], writes=[ysT_b])

                def mem_attention():
                    steps = [(h, mc) for h in range(4) for mc in range(2)]
                    om = {}
                    LOOK = 2
                    for i in range(len(steps) + LOOK):
                        if i < len(steps):
                            h, mc = steps[i]
                            bi = nb()
                            mm_group(banks[bi][:, :], [(mkT[:, h, mc * 128:(mc + 1) * 128], qmT[:, h, :])], reads=[mkT_b, qmT_b], writes=[bank_b[bi]])
                            e = i % 3
                            c.op(ACT, lambda bi=bi, e=e: A.activation(out=ET[e][:], in_=banks[bi][:, :], func=AF.Exp), reads=[bank_b[bi]], writes=[ET_b[e]])
                        if i >= LOOK:
                            h, mc = steps[i - LOOK]
                            e = (i - LOOK) % 3
                            if mc == 0:
                                om[h] = pin()
                            mm_group(banks[om[h]][:, :], [(Vm[:, mc, h, :], ET[e][:])], reads=[Vm_b, ET_b[e]], writes=[bank_b[om[h]]], first=(mc == 0), last=(mc == 1))
                            if mc == 1:
                                o_ = om[h]
                                c.op(ACT, lambda o_=o_: A.activation(out=rec[64:128, :], in_=banks[o_][64:128, :], func=AF.Ln), reads=[bank_b[o_]], writes=[rec_b])
                                c.op(ACT, lambda: A.activation(out=rec[64:128, :], in_=rec[64:128, :], func=AF.Exp, scale=-1.0), reads=[rec_b], writes=[rec_b])
                                c.op(DVE, lambda o_=o_, h=h: V.tensor_tensor(out=OmT[:, h, :], in0=banks[o_][0:64, :], in1=rec[64:128, :], op=ALU.mult),
                                     reads=[bank_b[o_], rec_b], writes=[OmT_b])
                                unpin(o_)

                def hd_grp(w3, wb, c0):
                    def g():
                        bi = nb()
                        mm_group(banks[bi][0:64, :], [(w3[:, kc, c0:c0 + 64], hT[:, kc, :]) for kc in range(KC)],
                                 reads=[wb, hT_b], writes=[bank_b[bi]])
                        return bi
                    return g
                wt, wb = piece(("p2",))
                w3 = wt[:, 0:8 * 452].rearrange("p (kc d) -> p kc d", kc=8)
                head_pipeline([(hd_grp(w3, wb, 0), 512, 1, kT[:, st * 512:(st + 1) * 512], kT_b)])
                bi = nb()
                mm_group(banks[bi][0:64, :], [(w3[:, kc, 384:448], hT[:, kc, :]) for kc in range(KC)], reads=[wb, hT_b], writes=[bank_b[bi]])
                c.op(ACT, lambda bi=bi: A.copy(out=kiT[:, st * 512:(st + 1) * 512], in_=banks[bi][0:64, :]), reads=[bank_b[bi]], writes=[kiT_b])
                for h in range(4):
                    bi = nb()
                    mm_group(banks[bi][0:64, :], [(w3[:, kc, 128 + h * 64:192 + h * 64], hT[:, kc, :]) for kc in range(KC)],
                             reads=[wb, hT_b], writes=[bank_b[bi]])
                    c.op(ACT, lambda bi=bi, h=h: A.copy(out=qiT[:, h, :], in_=banks[bi][0:64, :]), reads=[bank_b[bi]], writes=[qiT_b])
                for t in range(4):
                    bi = nb()
                    mm_group(banks[bi][:, 0:64], [(hT[:, kc, t * 128:(t + 1) * 128], w3[:, kc, 64:128]) for kc in range(KC)],
                             reads=[wb, hT_b], writes=[bank_b[bi]])
                    mm_group(banks[bi][:, 64:68], [(hT[:, kc, t * 128:(t + 1) * 128], w3[:, kc, 448:452]) for kc in range(KC)],
                             reads=[wb, hT_b], writes=[bank_b[bi]])
                    c.op(DVE, lambda bi=bi, t=t: V.tensor_copy(Vaug[:, st * 4 + t, 0:64], banks[bi][:, 0:64]), reads=[bank_b[bi]], writes=[Vaug_b])
                    c.op(DVE, lambda bi=bi, t=t: V.tensor_scalar(out=wq[:, t, :], in0=banks[bi][:, 64:68], scalar1=0.0625, scalar2=None, op0=ALU.mult),
                         reads=[bank_b[bi]], writes=[wq_b])
                wt1, wb1 = piece(("p1",))
                w31 = wt1[:, 0:4096].rearrange("p (kc d) -> p kc d", kc=8)
                wt3, wb3 = piece(("p3",))
                w33 = wt3[:, 0:4096].rearrange("p (kc d) -> p kc d", kc=8)
                hp_pend = [None]

                def hp_push(task):
                    grp, N, gidx, out_ap, out_b = task
                    bi = grp()
                    if hp_pend[0] is not None:
                        head_norm_b(*hp_pend[0])
                    head_norm_a(bi, N)
                    hp_pend[0] = (bi, N, gidx, out_ap, out_b)

                def hp_flush():
                    if hp_pend[0] is not None:
                        head_norm_b(*hp_pend[0])
                        hp_pend[0] = None

                def u_grp(m):
                    bi = nb()
                    mm_group(banks[bi][:, :], [(w33[:, kc, m * 128:(m + 1) * 128], hT[:, kc, :]) for kc in range(KC)],
                             reads=[wb3, hT_b], writes=[bank_b[bi]])
                    c.op(ACT, lambda: A.copy(out=uT[:, m, :], in_=banks[bi][:, :]), reads=[bank_b[bi]], writes=[uT_b])

                fillers = []
                for h in range(8):
                    fillers.append(lambda h=h: hp_push((hd_grp(w31, wb1, h * 64), 512, 0, qaT[:, h, :], qaT_b)))
                for m in range(2):
                    fillers.append(lambda m=m: u_grp(m))
                for h in range(4):
                    fillers.append(lambda h=h: hp_push((hd_grp(w33, wb3, 256 + h * 64), 512, 2, qmT[:, h, :], qmT_b)))
                fillers.append(hp_flush)
                fillers.append(lambda: mem_attention())

                c.barrier([actT_b])
                for t in range(4):
                    j = st * 4 + t
                    L = (j + 1) * 128
                    for kb in range((L + 511) // 512):
                        k0 = kb * 512
                        kn = min(512, L - k0)
                        for h in range(4):
                            bi = nb()
                            mm_group(banks[bi][:, 0:kn], [(qiT[:, h, t * 128:(t + 1) * 128], kiT[:, k0:k0 + kn])],
                                     reads=[qiT_b, kiT_b], writes=[bank_b[bi]])
                            if h == 0:
                                c.op(DVE, lambda bi=bi, t=t, k0=k0, kn=kn: V.tensor_scalar(
                                    out=Isc[t][:, k0:k0 + kn], in0=banks[bi][:, 0:kn], scalar1=0.0, scalar2=wq[:, t, 0:1], op0=ALU.max, op1=ALU.mult),
                                    reads=[bank_b[bi], wq_b], writes=[Isc_b[t]])
                            else:
                                ri = h % 2
                                c.op(ACT, lambda bi=bi, ri=ri, kn=kn: A.activation(out=rl[ri][:, 0:kn], in_=banks[bi][:, 0:kn], func=AF.Relu),
                                     reads=[bank_b[bi]], writes=[rl_b[ri]])
                                c.op(DVE, lambda ri=ri, t=t, h=h, k0=k0, kn=kn: V.scalar_tensor_tensor(
                                    out=Isc[t][:, k0:k0 + kn], in0=rl[ri][:, 0:kn], scalar=wq[:, t, h:h + 1], in1=Isc[t][:, k0:k0 + kn],
                                    op0=ALU.mult, op1=ALU.add), reads=[rl_b[ri], wq_b, Isc_b[t]], writes=[Isc_b[t]])
                    c.op(DVE, lambda t=t, L=L: V.tensor_reduce(out=bmx[:, t:t + 1], in_=Isc[t][:, 0:L], axis=mybir.AxisListType.X, op=ALU.max),
                         reads=[Isc_b[t]], writes=[bmx_b])
                    c.op(DVE, lambda t=t, L=L: V.tensor_reduce(out=blo[:, t:t + 1], in_=Isc[t][:, 0:L], axis=mybir.AxisListType.X, op=ALU.min),
                         reads=[Isc_b[t]], writes=[blo_b])
                    c.op(DVE, lambda t=t, L=L: V.memset(Isc[t][0:64, L - 64:L], -BIG), reads=[Isc_b[t]], writes=[Isc_b[t]])
                need = [((st * 4 + t + 1) * 128 > TOPK) for t in range(4)]
                Ls = [(st * 4 + t + 1) * 128 for t in range(4)]
                on_act = [False, True, True, False]
                if any(need):
                    for t in range(4):
                        thr = float(Ls[t] - 2 * TOPK) if on_act[t] else float(Ls[t] - TOPK)
                        c.op(DVE, lambda t=t, thr=thr: V.memset(bthr[:, t:t + 1], thr), writes=[bthr_b])
                    c.op(DVE, lambda: V.tensor_tensor(out=bw[:], in0=bmx[:], in1=blo[:], op=ALU.subtract), reads=[bmx_b, blo_b], writes=[bw_b])
                    c.op(DVE, lambda: V.tensor_scalar(out=bpw[:], in0=bw[:], scalar1=1e-3, scalar2=1e-6, op0=ALU.mult, op1=ALU.add),
                         reads=[bw_b], writes=[bpw_b])
                    c.op(DVE, lambda: V.tensor_tensor(out=blo[:], in0=blo[:], in1=bpw[:], op=ALU.subtract), reads=[bpw_b, blo_b], writes=[blo_b])
                    c.op(DVE, lambda: V.tensor_tensor(out=bw[:], in0=bmx[:], in1=blo[:], op=ALU.subtract), reads=[bmx_b, blo_b], writes=[bw_b])
                    for t in range(4):
                        if not need[t]:
                            c.op(DVE, lambda t=t: V.memset(bcnt[:, t:t + 1], 0.0), writes=[bcnt_c[t]])
                    for it in range(NITER):
                        hw_ = 0.5 ** (it + 1)
                        c.op(DVE, lambda hw_=hw_: V.scalar_tensor_tensor(out=bmid[:], in0=bw[:], scalar=hw_, in1=blo[:], op0=ALU.mult, op1=ALU.add),
                             reads=[blo_b, bw_b], writes=[bmid_b])
                        for t in (1, 2, 3, 0):
                            if not need[t]:
                                continue
                            L = Ls[t]
                            if on_act[t]:
                                c.op(ACT, lambda t=t, L=L: A.activation(out=junkA[:, 0:L], in_=Isc[t][:, 0:L], func=AF.Sign, bias=bmid[:, t:t + 1], scale=-1.0, saturate=False,
                                                                        accum_out=bcnt[:, t:t + 1]),
                                     reads=[Isc_b[t], bmid_b], writes=[junkA_b, bcnt_c[t]])
                            else:
                                c.op(DVE, lambda t=t, L=L: V.tensor_scalar(out=junk[:, 0:L], in0=Isc[t][:, 0:L], scalar1=bmid[:, t:t + 1], scalar2=0.0,
                                                                            op0=ALU.is_le, op1=ALU.add, accum_out=bcnt[:, t:t + 1]),
                                     reads=[Isc_b[t], bmid_b], writes=[junk_b, bcnt_c[t]])
                        c.op(DVE, lambda: V.tensor_tensor(out=bpw[:], in0=bcnt[:], in1=bthr[:], op=ALU.is_le), reads=bcnt_c + [bthr_b], writes=[bpw_b])
                        c.op(DVE, lambda: V.tensor_tensor(out=bpw[:], in0=bpw[:], in1=bw[:], op=ALU.mult), reads=[bpw_b, bw_b], writes=[bpw_b])
                        c.op(DVE, lambda hw_=hw_: V.scalar_tensor_tensor(out=blo[:], in0=bpw[:], scalar=hw_, in1=blo[:], op0=ALU.mult, op1=ALU.add),
                             reads=[blo_b, bpw_b], writes=[blo_b])
                        if fillers:
                            fillers.pop(0)()
                while fillers:
                    fillers.pop(0)()
                for t in range(4):
                    if not need[t]:
                        c.op(DVE, lambda t=t: V.memset(blo[:, t:t + 1], -1.0e29), reads=[blo_b], writes=[blo_b])
                if "isc" in dbg_d and b == 0 and st == NT - 1:
                    dump("isc", Isc[3][:, 0:S], Isc_b[3], dbg_d["isc"][:, :])
                    dump("lo", blo[:, :], blo_b, dbg_d["lo"][:, :])

                emit_mask(0)
                emit_mask(1)
                attention(0)
                ssm_B(0); ssm_dve(0)
                emit_mask(2)
                attention(1)
                ssm_C(0); ssm_B(1); ssm_dve(1)
                emit_mask(3)
                attention(2)
                ssm_C(1); ssm_B(2); ssm_dve(2)
                attention(3)
                ssm_C(2); ssm_B(3); ssm_dve(3)
                ssm_C(3)

                for f in range(8):
                    wt, wb = piece(("mg", f))
                    wg = wt[:, 0:3072].rearrange("p (br kc d) -> p br kc d", br=3, kc=8)
                    woa = wt[0:64, 3072:4096].rearrange("p (h d) -> p h d", h=8)
                    wgl = wt[:, 4096:4608].rearrange("p (ab kc d) -> p ab kc d", ab=2, kc=2)
                    wom = wt[0:64, 4608:5120].rearrange("p (h d) -> p h d", h=4)
                    gb = [nb() for _ in range(3)]
                    for br in range(3):
                        mm_group(banks[gb[br]][:, :], [(wg[:, br, kc, :], hT[:, kc, :]) for kc in range(KC)], reads=[wb, hT_b], writes=[bank_b[gb[br]]])
                    ba = nb()
                    mm_group(banks[ba][:, :], [(woa[:, h, :], OaT[:, h, :]) for h in range(8)], reads=[wb, OaT_b], writes=[bank_b[ba]])
                    bga, bgb = nb(), nb()
                    mm_group(banks[bga][:, :], [(wgl[:, 0, kc, :], ysT[:, kc, :]) for kc in range(2)], reads=[wb, ysT_b], writes=[bank_b[bga]])
                    mm_group(banks[bgb][:, :], [(wgl[:, 1, kc, :], ysT[:, kc, :]) for kc in range(2)], reads=[wb, ysT_b], writes=[bank_b[bgb]])
                    bm = nb()
                    mm_group(banks[bm][:, :], [(wom[:, h, :], OmT[:, h, :]) for h in range(4)], reads=[wb, OmT_b], writes=[bank_b[bm]])
                    m0, m1, m2 = stmp[0], stmp[1], stmp[2]
                    y0, y1, y2 = stmp_b[0], stmp_b[1], stmp_b[2]
                    c.op(ACT, lambda: A.activation(out=m0[:], in_=banks[gb[0]][:, :], func=AF.Sigmoid), reads=[bank_b[gb[0]]], writes=[y0])
                    c.op(DVE, lambda: V.tensor_tensor(out=m0[:], in0=m0[:], in1=banks[ba][:, :], op=ALU.mult), reads=[y0, bank_b[ba]], writes=[y0])
                    c.op(ACT, lambda: A.activation(out=m1[:], in_=banks[bgb][:, :], func=AF.Sigmoid), reads=[bank_b[bgb]], writes=[y1])
                    c.op(DVE, lambda: V.tensor_tensor(out=m1[:], in0=m1[:], in1=banks[bga][:, :], op=ALU.mult), reads=[y1, bank_b[bga]], writes=[y1])
                    c.op(ACT, lambda: A.activation(out=m2[:], in_=banks[gb[1]][:, :], func=AF.Sigmoid), reads=[bank_b[gb[1]]], writes=[y2])
                    c.op(DVE, lambda: V.tensor_tensor(out=m1[:], in0=m1[:], in1=m2[:], op=ALU.mult), reads=[y1, y2], writes=[y1])
                    c.op(DVE, lambda: V.tensor_tensor(out=m0[:], in0=m0[:], in1=m1[:], op=ALU.add), reads=[y0, y1], writes=[y0])
                    c.op(ACT, lambda: A.activation(out=m2[:], in_=banks[gb[2]][:, :], func=AF.Sigmoid), reads=[bank_b[gb[2]]], writes=[y2])
                    c.op(DVE, lambda: V.tensor_tensor(out=m2[:], in0=m2[:], in1=banks[bm][:, :], op=ALU.mult), reads=[y2, bank_b[bm]], writes=[y2])
                    c.op(DVE, lambda f=f: V.tensor_tensor(out=mgT[:, f, :], in0=m0[:], in1=m2[:], op=ALU.add), reads=[y0, y2], writes=[mgT_b])
                for hh in range(2):
                    wt, wb = piece(("wo", hh))
                    w3 = wt[:, 0:4096].rearrange("p (kc d) -> p kc d", kc=8)
                    for t in range(4):
                        bi = nb()
                        mm_group(banks[bi][:, :], [(mgT[:, kc, t * 128:(t + 1) * 128], w3[:, kc, :]) for kc in range(KC)],
                                 reads=[wb, mgT_b], writes=[bank_b[bi]])
                        xsl = xt[:, t, hh * 512:(hh + 1) * 512]
                        c.op(DVE, lambda bi=bi, xsl=xsl: V.tensor_tensor(out=xsl, in0=xsl, in1=banks[bi][:, :], op=ALU.add),
                             reads=[bank_b[bi], xt_b[t]], writes=[xt_b[t]])
                if "x2" in dbg_d and b == 0:
                    for t in range(4):
                        dump("x2", xt[:, t, :], xt_b[t], dbg_d["x2"][st * 512 + t * 128:st * 512 + (t + 1) * 128, :])
                ffn("f2", 2)
                for t in range(4):
                    c.op(ACT, lambda t=t: A.activation(out=junk[:, 0:D], in_=xt[:, t, :], func=AF.Square, accum_out=ss[:, t:t + 1]),
                         reads=[xt_b[t]], writes=[junk_b, ss_b])
                    c.op(ACT, lambda t=t: A.activation(out=ss[:, 4 + t:5 + t], in_=ss[:, t:t + 1], func=AF.Ln, bias=epsc[:, 0:1], scale=1.0 / D),
                         reads=[ss_b], writes=[ss_b])
                    c.op(ACT, lambda t=t: A.activation(out=ss[:, 4 + t:5 + t], in_=ss[:, 4 + t:5 + t], func=AF.Exp, scale=-0.5), reads=[ss_b], writes=[ss_b])
                    c.op(DVE, lambda t=t: V.scalar_tensor_tensor(out=xt[:, t, :], in0=xt[:, t, :], scalar=ss[:, 4 + t:5 + t], in1=gfin[:],
                                                                 op0=ALU.mult, op1=ALU.mult), reads=[xt_b[t], ss_b], writes=[xt_b[t]])
                    c.dma(POOL, out_d[row0 + t * 128:row0 + (t + 1) * 128, :], xt[:, t, :], sem_out, reads=[xt_b[t]])
        G.wait_ge(sem_out.h, sem_out.n)
        if sem_misc.n:
            G.wait_ge(sem_misc.h, sem_misc.n)
    return nc


_CACHE = {}


def _run(inputs, NB, S, dbg=()):
    key = (NB, S, str(dbg))
    if key not in _CACHE:
        _CACHE[key] = build(NB, S, dbg)
    nc = _CACHE[key]
    x = np.ascontiguousarray(inputs["x"], dtype=np.float32)
    mem = np.ascontiguousarray(inputs["mem"], dtype=np.float32)
    ncore = 8
    wmap = {n: np.ascontiguousarray(np.asarray(inputs[n], dtype=np.float32).reshape(WSHAPES[n])) for n in WNAMES}
    wmap["onehot"] = _onehot_table()
    in_maps = []
    for i in range(ncore):
        m = dict(wmap)
        m["x"] = x[i * NB:(i + 1) * NB].reshape(NB * S, D)
        m["mem"] = mem[i * NB:(i + 1) * NB].reshape(NB * 256, D)
        in_maps.append(m)
    res = run_bass_kernel_spmd(nc, in_maps, core_ids=list(range(ncore)))
    return res


def kernel(**inputs):
    B, S = inputs["x"].shape[0], inputs["x"].shape[1]
    NB = B // 8
    res = _run(inputs, NB, S)
    out = np.concatenate([r["out"].reshape(NB, S, D) for r in res.results], axis=0)
    return out.astype(np.float32)
```

```python
import math
from contextlib import ExitStack
import numpy as np
import concourse.bass as bass
import concourse.mybir as mybir
from concourse.bass_utils import run_bass_kernel_spmd

F32 = mybir.dt.float32
BF16 = mybir.dt.bfloat16
I32 = mybir.dt.int32
AF = mybir.ActivationFunctionType
ALU = mybir.AluOpType

D = 1024
KC = 8
FFN = 2816
FC = 22
WIN = 4548
EPS = 1e-6
NEG = 30000.0
BIG = 1.0e30
SLOT = 5632
NITER = 22


class Tok:
    __slots__ = ("sem", "val")

    def __init__(self, sem, val):
        self.sem = sem
        self.val = val


class Sem:
    def __init__(self, h):
        self.h = h
        self.n = 0


class Buf:
    def __init__(self, name):
        self.name = name
        self.w = None
        self.r = {}


class Eng:
    def __init__(self, eng, sem):
        self.eng = eng
        self.sem = sem
        self.waited = {}

    def wait(self, tok):
        if tok is None:
            return
        k = id(tok.sem)
        if self.waited.get(k, 0) >= tok.val:
            return
        self.eng.wait_ge(tok.sem.h, tok.val)
        self.waited[k] = tok.val


class Ctx:
    def __init__(self, nc, es):
        self.nc = nc
        self.es = es
        self.PE = Eng(nc.tensor, self.sem("pe"))
        self.ACT = Eng(nc.scalar, self.sem("act"))
        self.DVE = Eng(nc.vector, self.sem("dve"))
        self.POOL = Eng(nc.gpsimd, self.sem("pool"))
        self.SP = Eng(nc.sync, self.sem("sp"))

    def sem(self, name):
        return Sem(self.es.enter_context(self.nc.semaphore(name)))

    def _deps(self, E, reads, writes):
        for b in reads:
            E.wait(b.w)
        pe = E is self.PE
        for b in writes:
            if b.w is not None and not (pe and b.w.sem is E.sem):
                E.wait(b.w)
            for t in b.r.values():
                if not (pe and t.sem is E.sem):
                    E.wait(t)

    def _commit(self, tok, reads, writes):
        for b in reads:
            b.r[id(tok.sem)] = tok
        for b in writes:
            b.w = tok
            b.r = {}

    def op(self, E, fn, reads=(), writes=()):
        self._deps(E, reads, writes)
        ins = fn()
        E.sem.n += 1
        ins.then_inc(E.sem.h, 1)
        tok = Tok(E.sem, E.sem.n)
        self._commit(tok, reads, writes)
        return tok

    def dma(self, Q, out, in_, sem, reads=(), writes=(), **kw):
        self._deps(Q, reads, writes)
        ins = Q.eng.dma_start(out=out, in_=in_, **kw)
        sem.n += 16
        ins.then_inc(sem.h, 16)
        tok = Tok(sem, sem.n)
        self._commit(tok, reads, writes)
        return tok

    def barrier(self, bufs):
        for E in (self.PE, self.ACT, self.DVE, self.POOL, self.SP):
            for b in bufs:
                E.wait(b.w)
                for t in b.r.values():
                    E.wait(t)


def _t5_bucket_np(rel):
    half = 16
    max_exact = 8
    n = np.abs(rel)
    nf = np.maximum(n, 1).astype(np.float32)
    large = max_exact + (np.log(nf / np.float32(max_exact)) / np.float32(math.log(128 / 8))
                         * np.float32(half - max_exact)).astype(np.int32)
    large = np.minimum(large, half - 1)
    return np.where(rel > 0, half, 0) + np.where(n < max_exact, n, large)


def _onehot_table():
    rel = np.arange(-255, 129, dtype=np.int32)
    bk = _t5_bucket_np(rel)
    oh = np.zeros((32, 384), np.float32)
    oh[bk, np.arange(384)] = 1.0
    return oh


WNAMES = ["ffn1_norm", "ffn1_w_in", "ffn1_w_out", "mix_norm", "w_in", "a_q_gain", "a_k_gain", "rel_bias",
          "w_o_a", "ssm_lambda_re", "ssm_lambda_im", "ssm_log_dt", "ssm_b_re", "ssm_b_im", "ssm_c_re",
          "ssm_c_im", "ssm_d", "w_glu", "mem_norm", "w_mem_kv", "m_q_gain", "m_k_gain", "w_o_m", "w_out",
          "ffn2_norm", "ffn2_w_in", "ffn2_w_out", "final_norm"]
WSHAPES = {"ffn1_norm": [1, D], "ffn1_w_in": [D, 2 * FFN], "ffn1_w_out": [FFN, D], "mix_norm": [1, D],
           "w_in": [D, WIN], "a_q_gain": [64, 1], "a_k_gain": [64, 1], "rel_bias": [32, 8],
           "w_o_a": [512, D], "ssm_lambda_re": [16, 64], "ssm_lambda_im": [16, 64], "ssm_log_dt": [1, 16],
           "ssm_b_re": [16, 64, 16], "ssm_b_im": [16, 64, 16], "ssm_c_re": [16, 16, 64],
           "ssm_c_im": [16, 16, 64], "ssm_d": [256, 1], "w_glu": [256, 2 * D], "mem_norm": [1, D],
           "w_mem_kv": [D, 512], "m_q_gain": [64, 1], "m_k_gain": [64, 1], "w_o_m": [256, D],
           "w_out": [D, D], "ffn2_norm": [1, D], "ffn2_w_in": [D, 2 * FFN], "ffn2_w_out": [FFN, D],
           "final_norm": [1, D]}


def build(NB, S, dbg=()):
    NT = S // 512
    TOPK = min(256, S // 4)
    nc = bass.Bass("TRN2", target_bir_lowering=False)
    x_d = nc.dram_tensor("x", [NB * S, D], F32, kind="ExternalInput").ap()
    mem_d = nc.dram_tensor("mem", [NB * 256, D], F32, kind="ExternalInput").ap()
    W = {n: nc.dram_tensor(n, WSHAPES[n], F32, kind="ExternalInput").ap() for n in WNAMES}
    oh_d = nc.dram_tensor("onehot", [32, 384], F32, kind="ExternalInput").ap()
    out_d = nc.dram_tensor("out", [NB * S, D], F32, kind="ExternalOutput").ap()
    dbg_d = {n: nc.dram_tensor("dbg_" + n, shp, F32, kind="ExternalOutput").ap() for n, shp in dbg}

    pieces = []

    def ffn_pieces(tag, win, wout):
        wi4 = win.rearrange("(kc p) (gu f) -> p kc gu f", p=128, gu=2)
        for i in range(11):
            srcs = [(0, 128, gu * 2048, [8, 256], wi4[:, :, gu, 256 * i:256 * i + 256]) for gu in range(2)]
            pieces.append(((tag, "in", i), srcs))
        wo3 = wout.rearrange("(f p) d -> p f d", p=128)
        for hh in range(2):
            for pp in range(2):
                pieces.append(((tag, "out", hh, pp),
                               [(0, 128, 0, [11, 512], wo3[:, 11 * pp:11 * pp + 11, 512 * hh:512 * hh + 512])]))

    win3 = W["w_in"].rearrange("(kc p) c -> p kc c", p=128)
    ffn_pieces("f1", W["ffn1_w_in"], W["ffn1_w_out"])
    pieces.append((("p1",), [(0, 128, 0, [8, 512], win3[:, :, 0:512])]))
    pieces.append((("p2",), [(0, 128, 0, [8, 452], win3[:, :, 512:964])]))
    pieces.append((("p3",), [(0, 128, 0, [8, 512], win3[:, :, 964:1476])]))
    woa3 = W["w_o_a"].rearrange("(h p) d -> p h d", p=64)
    wglu3 = W["w_glu"].rearrange("(kc p) d -> p kc d", p=128)
    wom3 = W["w_o_m"].rearrange("(h p) d -> p h d", p=64)
    for f in range(8):
        srcs = []
        for br in range(3):
            c0 = 1476 + br * 1024 + f * 128
            srcs.append((0, 128, br * 1024, [8, 128], win3[:, :, c0:c0 + 128]))
        srcs.append((0, 64, 3072, [8, 128], woa3[:, :, f * 128:f * 128 + 128]))
        for ab in range(2):
            srcs.append((0, 128, 4096 + ab * 256, [2, 128], wglu3[:, :, ab * 1024 + f * 128:ab * 1024 + f * 128 + 128]))
        srcs.append((0, 64, 4608, [4, 128], wom3[:, :, f * 128:f * 128 + 128]))
        pieces.append((("mg", f), srcs))
    wout3 = W["w_out"].rearrange("(kc p) d -> p kc d", p=128)
    for hh in range(2):
        pieces.append((("wo", hh), [(0, 128, 0, [8, 512], wout3[:, :, 512 * hh:512 * hh + 512])]))
    ffn_pieces("f2", W["ffn2_w_in"], W["ffn2_w_out"])
    wkv3 = W["w_mem_kv"].rearrange("(kc p) d -> p kc d", p=128)
    pieces.append((("kv",), [(0, 128, 0, [8, 512], wkv3[:, :, :])]))
    pidx = {k: i for i, (k, _) in enumerate(pieces)}
    NP = len(pieces)
    wscr = nc.dram_tensor("wscr", [NP, 128, SLOT], BF16, kind="Internal").ap()
    fd_d = nc.dram_tensor("fdscr", [8, 384], F32, kind="Internal").ap()

    with ExitStack() as es:
        c = Ctx(nc, es)
        PE, ACT, DVE, POOL, SP = c.PE, c.ACT, c.DVE, c.POOL, c.SP
        V, A, G, T = nc.vector, nc.scalar, nc.gpsimd, nc.tensor

        def sb(name, shape, dt):
            return es.enter_context(nc.sbuf_tensor(name, shape, dt))

        wscr_b = Buf("wscr")
        sem_pro = c.sem("pro")
        sem_const = c.sem("const")
        sem_out = c.sem("outst")
        sem_x = c.sem("xld")
        sem_misc = c.sem("misc")

        ident_f = sb("ident_f", [128, 128], F32)
        ident = sb("ident", [128, 128], BF16)
        antiJ = sb("antiJ", [128, 128], BF16)
        I8big = sb("I8big", [128, 4, 128], BF16)
        ones = sb("ones", [128, 128], BF16)
        epsc = sb("epsc", [128, 1], F32)
        gcol = sb("gcol", [128, 4, KC], F32)
        gfin = sb("gfin", [128, D], F32)
        hg = sb("hg", [64, 4], F32)
        biasT = sb("biasT", [128, 2, 8, 128], BF16)
        cst_b = Buf("const")
        cosT = sb("cosT", [128, 8, 128], F32); sinT = sb("sinT", [128, 8, 128], F32)
        BpT = sb("BpT", [128, 2, 8, 128], BF16)
        Cp = sb("Cp", [128, 2, 8, 128], BF16)
        ssmc = sb("ssmc", [128, 12, 8], F32)
        dcol = sb("dcol", [128, 2], F32)

        banks = [es.enter_context(nc.psum_tensor("bank%d" % i, [128, 512], F32)) for i in range(7)]
        bank_b = [Buf("bank%d" % i) for i in range(7)]
        psT = es.enter_context(nc.psum_tensor("psT", [128, 1024], BF16)); psT_b = Buf("psT")
        pinned = set()
        rr = [0]

        def nb():
            while True:
                i = rr[0] % 7
                rr[0] += 1
                if i not in pinned:
                    return i

        def pin():
            i = nb()
            pinned.add(i)
            return i

        def unpin(i):
            pinned.discard(i)

        def mm_group(out_ap, pairs, reads, writes, first=True, last=True):
            def fn():
                ins = None
                n = len(pairs)
                for i, (l, r) in enumerate(pairs):
                    ins = T.matmul(out_ap, l, r, start=(first and i == 0), stop=(last and i == n - 1))
                return ins
            return c.op(PE, fn, reads=reads, writes=writes)

        def dump(name, ap_sb, buf, dst):
            if name in dbg_d:
                c.dma(POOL, dst, ap_sb, sem_misc, reads=[buf])

        def cdma(out, in_, **kw):
            c.dma(SP, out, in_, sem_const, writes=[cst_b], **kw)

        for gi, nm in enumerate(["ffn1_norm", "mix_norm", "ffn2_norm", "mem_norm"]):
            cdma(gcol[:, gi, :], W[nm].rearrange("o (c p) -> p (o c)", p=128), allow_slow_non_contiguous=True)
        cdma(gfin[:], W["final_norm"][0:1, :].partition_broadcast(128))
        for gi, nm in enumerate(["a_q_gain", "a_k_gain", "m_q_gain", "m_k_gain"]):
            cdma(hg[:, gi:gi + 1], W[nm][:, :])
        es3 = ExitStack()
        sb_main = sb

        def sb(name, shape, dt):
            return es3.enter_context(nc.sbuf_tensor(name, shape, dt))
        rb_sb = sb("rb_sb", [32, 8], F32); oh_sb = sb("oh_sb", [32, 384], F32); rb15 = sb("rb15", [8, 1], F32)
        fsb = sb("fsb", [8, 384], F32); hank = sb("hank", [128, 2, 8, 128], F32); hankb = sb("hankb", [128, 2, 8, 128], BF16)
        cdma(rb_sb[:], W["rel_bias"][:, :])
        cdma(oh_sb[:], oh_d[:, :])
        cdma(rb15[:], W["rel_bias"][15:16, :].rearrange("o h -> h o"), allow_slow_non_contiguous=True)
        for t in range(2):
            cdma(ssmc[t * 64:(t + 1) * 64, 0, :], W["ssm_lambda_re"].rearrange("(c t) s -> t s c", t=2)[t], allow_slow_non_contiguous=True)
            cdma(ssmc[t * 64:(t + 1) * 64, 1, :], W["ssm_lambda_im"].rearrange("(c t) s -> t s c", t=2)[t], allow_slow_non_contiguous=True)
            cdma(ssmc[t * 64:(t + 1) * 64, 2, :], W["ssm_log_dt"].rearrange("o (c t) -> t o c", t=2)[t].partition_broadcast(64), allow_slow_non_contiguous=True)
        braw = sb("braw", [128, 2, 8, 16], F32); craw = sb("craw", [128, 2, 8, 16], F32)
        for t in range(2):
            cdma(braw[t * 64:(t + 1) * 64, 0, :, :], W["ssm_b_re"].rearrange("(c t) s h -> t s c h", t=2)[t])
            cdma(braw[t * 64:(t + 1) * 64, 1, :, :], W["ssm_b_im"].rearrange("(c t) s h -> t s c h", t=2)[t])
            for cc in range(8):
                cdma(craw[t * 64:(t + 1) * 64, 0, cc, :], W["ssm_c_re"].rearrange("(c t) o s -> t c s o", t=2)[t, cc], allow_slow_non_contiguous=True)
                cdma(craw[t * 64:(t + 1) * 64, 1, cc, :], W["ssm_c_im"].rearrange("(c t) o s -> t c s o", t=2)[t, cc], allow_slow_non_contiguous=True)
        cdma(dcol[:, :], W["ssm_d"].rearrange("(m p) o -> p (m o)", p=128), allow_slow_non_contiguous=True)

        def cop(E, fn):
            return c.op(E, fn, reads=[cst_b], writes=[cst_b])

        cop(POOL, lambda: G.memset(ident_f[:], 1.0))
        cop(POOL, lambda: G.affine_select(out=ident_f[:], in_=ident_f[:], pattern=[[-1, 128]], compare_op=ALU.is_equal,
                                          fill=0.0, base=0, channel_multiplier=1))
        cop(DVE, lambda: V.tensor_copy(ident[:], ident_f[:]))
        for i in range(4):
            cop(DVE, lambda i=i: V.tensor_scalar(out=I8big[:, i, :], in0=ident_f[:], scalar1=NEG, scalar2=None, op0=ALU.mult))
        cop(POOL, lambda: G.memset(ident_f[:], 1.0))
        cop(POOL, lambda: G.affine_select(out=ident_f[:], in_=ident_f[:], pattern=[[1, 128]], compare_op=ALU.is_equal,
                                          fill=0.0, base=-127, channel_multiplier=1))
        cop(DVE, lambda: V.tensor_copy(antiJ[:], ident_f[:]))
        cop(DVE, lambda: V.memset(ones[:], 1.0))
        cop(DVE, lambda: V.memset(epsc[:], EPS))
        cop(DVE, lambda: V.tensor_scalar(out=hg[:, 0:1], in0=hg[:, 0:1], scalar1=0.125, scalar2=None, op0=ALU.mult))
        cop(DVE, lambda: V.tensor_scalar(out=hg[:, 2:3], in0=hg[:, 2:3], scalar1=0.125, scalar2=None, op0=ALU.mult))
        b0 = 0
        c.op(PE, lambda: T.matmul(banks[b0][0:8, 0:384], rb_sb[:, :], oh_sb[:, :], start=True, stop=True),
             reads=[cst_b], writes=[bank_b[b0]])
        c.op(DVE, lambda: V.tensor_scalar(out=fsb[:], in0=banks[b0][0:8, 0:384], scalar1=rb15[:, 0:1], scalar2=None,
                                          op0=ALU.subtract), reads=[bank_b[b0], cst_b], writes=[cst_b])
        fd_b = Buf("fd")
        c.dma(SP, fd_d[:, :], fsb[:], sem_const, reads=[cst_b], writes=[fd_b])
        for di, delta in enumerate((0, 128)):
            src = bass.AP(tensor=fd_d.tensor, offset=128 - delta, ap=[[1, 128], [384, 8], [1, 128]])
            c.dma(SP, hank[:, di, :, :], src, sem_const, reads=[fd_b], writes=[cst_b])
        cop(DVE, lambda: V.tensor_copy(hankb[:], hank[:]))
        for di in range(2):
            for h in range(8):
                bi = nb()
                c.op(PE, lambda di=di, h=h, bi=bi: T.matmul(banks[bi][:, 0:128], hankb[:, di, h, :], antiJ[:], start=True, stop=True),
                     reads=[cst_b], writes=[bank_b[bi]])
                c.op(DVE, lambda di=di, h=h, bi=bi: V.tensor_copy(biasT[:, di, h, :], banks[bi][:, 0:128]),
                     reads=[bank_b[bi], cst_b], writes=[cst_b])

        LR, LI, LDT, DT, MAG, TH, COS, SIN, FR, FI, T1, T2 = range(12)
        sc = lambda i: ssmc[:, i, :]
        cop(ACT, lambda: A.activation(out=sc(DT), in_=sc(LDT), func=AF.Exp))
        cop(DVE, lambda: V.tensor_tensor(out=sc(T1), in0=sc(LR), in1=sc(DT), op=ALU.mult))
        cop(ACT, lambda: A.activation(out=sc(MAG), in_=sc(T1), func=AF.Exp))
        cop(DVE, lambda: V.tensor_tensor(out=sc(TH), in0=sc(LI), in1=sc(DT), op=ALU.mult))
        MAGIC = 12582912.0
        TWO_PI = 2.0 * math.pi

        def sin_of(out_ap, in_ap, tmp_ap, shift):
            cop(DVE, lambda: V.tensor_scalar(out=tmp_ap, in0=in_ap, scalar1=shift, scalar2=1.0 / TWO_PI, op0=ALU.add, op1=ALU.mult))
            cop(DVE, lambda: V.tensor_scalar(out=tmp_ap, in0=tmp_ap, scalar1=MAGIC, scalar2=None, op0=ALU.add))
            cop(DVE, lambda: V.tensor_scalar(out=tmp_ap, in0=tmp_ap, scalar1=-MAGIC, scalar2=-TWO_PI, op0=ALU.add, op1=ALU.mult))
            cop(DVE, lambda: V.scalar_tensor_tensor(out=tmp_ap, in0=in_ap, scalar=shift, in1=tmp_ap, op0=ALU.add, op1=ALU.add))
            cop(DVE, lambda: V.tensor_scalar(out=tmp_ap, in0=tmp_ap, scalar1=math.pi, scalar2=-math.pi, op0=ALU.min, op1=ALU.max))
            cop(ACT, lambda: A.activation(out=out_ap, in_=tmp_ap, func=AF.Sin))

        sin_of(sc(SIN), sc(TH), sc(T1), 0.0)
        sin_of(sc(COS), sc(TH), sc(T1), math.pi / 2)
        arr = sb("arr", [128, 6, 8], F32)
        a_ = lambda i: arr[:, i, :]
        cop(DVE, lambda: V.tensor_tensor(out=a_(0), in0=sc(MAG), in1=sc(COS), op=ALU.mult))
        cop(DVE, lambda: V.tensor_scalar(out=a_(0), in0=a_(0), scalar1=-1.0, scalar2=None, op0=ALU.add))
        cop(DVE, lambda: V.tensor_tensor(out=a_(1), in0=sc(MAG), in1=sc(SIN), op=ALU.mult))
        cop(DVE, lambda: V.tensor_tensor(out=a_(2), in0=sc(LR), in1=sc(LR), op=ALU.mult))
        cop(DVE, lambda: V.tensor_tensor(out=a_(3), in0=sc(LI), in1=sc(LI), op=ALU.mult))
        cop(DVE, lambda: V.tensor_tensor(out=a_(2), in0=a_(2), in1=a_(3), op=ALU.add))
        cop(DVE, lambda: V.reciprocal(out=a_(5), in_=a_(2)))
        cop(DVE, lambda: V.tensor_tensor(out=a_(3), in0=a_(0), in1=sc(LR), op=ALU.mult))
        cop(DVE, lambda: V.tensor_tensor(out=a_(4), in0=a_(1), in1=sc(LI), op=ALU.mult))
        cop(DVE, lambda: V.tensor_tensor(out=a_(3), in0=a_(3), in1=a_(4), op=ALU.add))
        cop(DVE, lambda: V.tensor_tensor(out=sc(FR), in0=a_(3), in1=a_(5), op=ALU.mult))
        cop(DVE, lambda: V.tensor_tensor(out=a_(3), in0=a_(1), in1=sc(LR), op=ALU.mult))
        cop(DVE, lambda: V.tensor_tensor(out=a_(4), in0=a_(0), in1=sc(LI), op=ALU.mult))
        cop(DVE, lambda: V.tensor_tensor(out=a_(3), in0=a_(3), in1=a_(4), op=ALU.subtract))
        cop(DVE, lambda: V.tensor_tensor(out=sc(FI), in0=a_(3), in1=a_(5), op=ALU.mult))
        bbar = sb("bbar", [128, 2, 8, 16], F32); btmp = sb("btmp", [128, 8, 16], F32)
        frb = ssmc[:, FR, :].unsqueeze(2).to_broadcast([128, 8, 16])
        fib = ssmc[:, FI, :].unsqueeze(2).to_broadcast([128, 8, 16])
        cop(DVE, lambda: V.tensor_tensor(out=bbar[:, 0, :, :], in0=braw[:, 0, :, :], in1=frb, op=ALU.mult))
        cop(DVE, lambda: V.tensor_tensor(out=btmp[:], in0=braw[:, 1, :, :], in1=fib, op=ALU.mult))
        cop(DVE, lambda: V.tensor_tensor(out=bbar[:, 0, :, :], in0=bbar[:, 0, :, :], in1=btmp[:], op=ALU.subtract))
        cop(DVE, lambda: V.tensor_tensor(out=bbar[:, 1, :, :], in0=braw[:, 1, :, :], in1=frb, op=ALU.mult))
        cop(DVE, lambda: V.tensor_tensor(out=btmp[:], in0=braw[:, 0, :, :], in1=fib, op=ALU.mult))
        cop(DVE, lambda: V.tensor_tensor(out=bbar[:, 1, :, :], in0=bbar[:, 1, :, :], in1=btmp[:], op=ALU.add))
        xpad = sb("xpad", [128, 128], BF16)
        for ri in range(2):
            for cc in range(8):
                cop(DVE, lambda: V.memset(xpad[:], 0.0))
                for t in range(2):
                    col = 32 * (cc % 4) + 16 * t
                    cop(DVE, lambda ri=ri, cc=cc, t=t, col=col: V.tensor_copy(xpad[t * 64:(t + 1) * 64, col:col + 16],
                                                                               bbar[t * 64:(t + 1) * 64, ri, cc, :]))
                c.op(PE, lambda: T.transpose(psT[:, 0:128], xpad[:], ident[:]), reads=[cst_b], writes=[psT_b])
                c.op(DVE, lambda ri=ri, cc=cc: V.tensor_copy(BpT[:, ri, cc, :], psT[:, 0:128]), reads=[psT_b, cst_b], writes=[cst_b])
        cop(DVE, lambda: V.memset(Cp[:], 0.0))
        for ri in range(2):
            for cc in range(8):
                for t in range(2):
                    col = 16 * ((2 * cc + t) % 8)
                    cop(DVE, lambda ri=ri, cc=cc, t=t, col=col: V.tensor_scalar(
                        out=Cp[t * 64:(t + 1) * 64, ri, cc, col:col + 16], in0=craw[t * 64:(t + 1) * 64, ri, cc, :],
                        scalar1=(1.0 if ri == 0 else -1.0), scalar2=None, op0=ALU.mult))
        tau_i = sb("tau_i", [128, 128], I32); tau = sb("tau", [128, 128], F32); ang = sb("ang", [128, 8, 128], F32)
        atmp = sb("atmp", [128, 8, 128], F32)
        cop(POOL, lambda: G.iota(tau_i[:], pattern=[[1, 128]], base=0, channel_multiplier=0))
        cop(DVE, lambda: V.tensor_copy(tau[:], tau_i[:]))
        for cc in range(8):
            cop(DVE, lambda cc=cc: V.tensor_scalar(out=ang[:, cc, :], in0=tau[:], scalar1=ssmc[:, TH, cc:cc + 1], scalar2=None, op0=ALU.mult))
        sin_of(sinT[:], ang[:], atmp[:], 0.0)
        sin_of(cosT[:], ang[:], atmp[:], math.pi / 2)
        NSTG = 4
        with ExitStack() as es2:
            stg32 = [es2.enter_context(nc.sbuf_tensor("stg32_%d" % i, [128, SLOT], F32)) for i in range(NSTG)]
            stg16 = [es2.enter_context(nc.sbuf_tensor("stg16_%d" % i, [128, SLOT], BF16)) for i in range(NSTG)]
            b32 = [Buf("s32_%d" % i) for i in range(NSTG)]
            b16 = [Buf("s16_%d" % i) for i in range(NSTG)]
            s32sem = [c.sem("s32_%d" % i) for i in range(NSTG)]
            for i in range(NSTG):
                c.op(POOL, lambda i=i: G.memset(stg32[i][:], 0.0), writes=[b32[i]])
            for k, (key, srcs) in enumerate(pieces):
                j = k % NSTG
                n = 0
                for (p0, p1, off, shp, src) in srcs:
                    sz = shp[0] * shp[1]
                    dst = stg32[j][p0:p1, off:off + sz].rearrange("p (a b) -> p a b", a=shp[0])
                    c.dma(SP, dst, src, s32sem[j], writes=[b32[j]])
                    n = max(n, off + sz)
                E = (ACT, DVE)[k % 2]
                if E is ACT:
                    c.op(ACT, lambda j=j, n=n: A.copy(out=stg16[j][:, 0:n], in_=stg32[j][:, 0:n]),
                         reads=[b32[j]], writes=[b16[j]])
                elif E is DVE:
                    c.op(DVE, lambda j=j, n=n: V.tensor_copy(stg16[j][:, 0:n], stg32[j][:, 0:n]),
                         reads=[b32[j]], writes=[b16[j]])
                else:
                    c.op(POOL, lambda j=j, n=n: G.tensor_copy(stg16[j][:, 0:n], stg32[j][:, 0:n]),
                         reads=[b32[j]], writes=[b16[j]])
                c.dma(POOL, wscr[k, :, 0:n], stg16[j][:, 0:n], sem_pro, reads=[b16[j]], writes=[wscr_b])
            c.barrier(b32 + b16)

        c.barrier([cst_b])
        es3.close()
        sb = sb_main
        ring = [sb("ring%d" % i, [128, SLOT], BF16) for i in range(3)]
        ring_b = [Buf("ring%d" % i) for i in range(3)]
        ring_s = [c.sem("ring%d" % i) for i in range(3)]
        ring_ctr = [0]

        def piece(key):
            k = pidx[key]
            j = ring_ctr[0] % 3
            ring_ctr[0] += 1
            n = max(off + shp[0] * shp[1] for (_, _, off, shp, _) in pieces[k][1])
            c.dma(SP, ring[j][:, 0:n], wscr[k, :, 0:n], ring_s[j], reads=[wscr_b], writes=[ring_b[j]])
            return ring[j], ring_b[j]

        xt = sb("xt", [128, 4, D], F32); xt_b = [Buf("xt%d" % i) for i in range(4)]
        hT = sb("hT", [128, KC, 512], BF16); hT_b = Buf("hT")
        ss = sb("ss", [128, 8], F32); ss_b = Buf("ss")
        arena = sb("arena", [128, max(4 * S, FC * 256)], F32)
        actT = arena[:, 0:FC * 256].bitcast(BF16).rearrange("p (f n) -> p f n", f=FC); actT_b = Buf("actT")
        kT = sb("kT", [64, S], BF16); kT_b = Buf("kT")
        kiT = sb("kiT", [64, S], BF16); kiT_b = Buf("kiT")
        Vaug = sb("Vaug", [128, S // 128, 128], BF16); Vaug_b = Buf("Vaug")
        qaT = sb("qaT", [64, 8, 512], BF16); qaT_b = Buf("qaT")
        qiT = sb("qiT", [64, 4, 512], BF16); qiT_b = Buf("qiT")
        qmT = sb("qmT", [64, 4, 512], BF16); qmT_b = Buf("qmT")
        uT = sb("uT", [128, 2, 512], BF16); uT_b = Buf("uT")
        wq = sb("wq", [128, 4, 4], F32); wq_b = Buf("wq")
        Isc = [arena[:, i * S:(i + 1) * S] for i in range(4)]; Isc_b = [Buf("Isc%d" % i) for i in range(4)]
        MB = [sb("MB%d" % i, [128, S], BF16) for i in range(2)]; MB_b = [Buf("MB0"), Buf("MB1")]
        junkA = sb("junkA", [128, S], mybir.dt.float8e4); junkA_b = Buf("junkA")
        ET = [sb("ET%d" % i, [128, 512], BF16) for i in range(3)]; ET_b = [Buf("ET%d" % i) for i in range(3)]
        rec = sb("rec", [128, 512], F32); rec_b = Buf("rec")
        OaT = sb("OaT", [64, 8, 512], BF16); OaT_b = Buf("OaT")
        OmT = sb("OmT", [64, 4, 512], BF16); OmT_b = Buf("OmT")
        ysT = sb("ysT", [128, 2, 512], BF16); ysT_b = Buf("ysT")
        mgT = sb("mgT", [128, 8, 512], BF16); mgT_b = Buf("mgT")
        junk = mgT[:].rearrange("p f n -> p (f n)"); junk_b = mgT_b
        mkT = sb("mkT", [64, 4, 256], BF16); mkT_b = Buf("mkT")
        Vm = sb("Vm", [128, 2, 4, 128], BF16); Vm_b = Buf("Vm")
        memT = hT[:, :, 0:256]; memT_b = hT_b
        blo = sb("blo", [128, 4], F32); bw = sb("bw", [128, 4], F32); bmid = sb("bmid", [128, 4], F32)
        bcnt = sb("bcnt", [128, 4], F32); bpw = sb("bpw", [128, 4], F32); bmx = sb("bmx", [128, 4], F32)
        blo_b, bw_b, bmid_b, bcnt_b, bpw_b, bmx_b = (Buf(n) for n in ("blo", "bw", "bmid", "bcnt", "bpw", "bmx"))
        bthr = sb("bthr", [128, 4], F32); bthr_b = Buf("bthr")
        bcnt_c = [Buf("bcnt%d" % i) for i in range(4)]
        s_init = sb("s_init", [128, 2, 8], F32); s_init_b = Buf("s_init")
        stmp = [sb("stmp%d" % i, [128, 512], F32) for i in range(7)]; stmp_b = [Buf("stmp%d" % i) for i in range(7)]
        xrb = sb("xrb", [128, 2, 2, 512], BF16); xrb_b = [Buf("xrb0"), Buf("xrb1")]
        ssm_small = sb("ssm_small", [128, 4, 4], F32); ssm_small_b = Buf("ssm_small")
        yf = sb("yf", [128, 128], F32); yf_b = Buf("yf")
        sg = [stmp[3], stmp[4]]; sg_b = [stmp_b[3], stmp_b[4]]
        rl = [stmp[3], stmp[4]]; rl_b = [stmp_b[3], stmp_b[4]]
        hrs = stmp[0][0:64, :]; hrs_b = stmp_b[0]
        hsq = stmp[1][0:64, 0:256].bitcast(BF16); hsq_b = stmp_b[1]
        xs = stmp[2][:, :].bitcast(BF16); xs_b = stmp_b[2]
        c.op(DVE, lambda: V.memset(Vaug[:], 1.0), writes=[Vaug_b])
        c.op(DVE, lambda: V.memset(Vm[:], 1.0), writes=[Vm_b])

        def rms_to_hT(x_ap_fn, gi, dst, dst_b, ntile, src_b):
            for t in range(ntile):
                xa = x_ap_fn(t)
                c.op(ACT, lambda xa=xa, t=t: A.activation(out=junk[:, 0:D], in_=xa, func=AF.Square, accum_out=ss[:, t:t + 1]),
                     reads=[src_b[t]], writes=[junk_b, ss_b])
                c.op(ACT, lambda t=t: A.activation(out=ss[:, 4 + t:5 + t], in_=ss[:, t:t + 1], func=AF.Ln, bias=epsc[:, 0:1], scale=1.0 / D),
                     reads=[ss_b], writes=[ss_b])
                c.op(ACT, lambda t=t: A.activation(out=ss[:, 4 + t:5 + t], in_=ss[:, 4 + t:5 + t], func=AF.Exp, scale=-0.5), reads=[ss_b], writes=[ss_b])
                c.op(DVE, lambda xa=xa, t=t: V.tensor_scalar(out=xs[:], in0=xa, scalar1=ss[:, 4 + t:5 + t], scalar2=None, op0=ALU.mult),
                     reads=[src_b[t], ss_b], writes=[xs_b])

                def tr():
                    ins = None
                    for kc in range(KC):
                        ins = T.transpose(psT[:, kc * 128:(kc + 1) * 128], xs[:, kc * 128:(kc + 1) * 128], ident[:])
                    return ins
                c.op(PE, tr, reads=[xs_b], writes=[psT_b])
                c.op(DVE, lambda t=t: V.tensor_tensor(out=dst[:, :, t * 128:(t + 1) * 128],
                                                      in0=psT[:].rearrange("p (c n) -> p c n", c=KC),
                                                      in1=gcol[:, gi, :].unsqueeze(2).to_broadcast([128, KC, 128]), op=ALU.mult),
                     reads=[psT_b], writes=[dst_b])

        def head_norm_a(bi, N):
            c.op(ACT, lambda: A.activation(out=hsq[:, 0:N], in_=banks[bi][0:64, 0:N], func=AF.Square), reads=[bank_b[bi]], writes=[hsq_b])

        def head_norm_b(bi, N, gidx, out_ap, out_b):
            b2 = nb()
            mm_group(banks[b2][0:64, 0:N], [(ones[0:64, 0:64], hsq[:, 0:N])], reads=[hsq_b], writes=[bank_b[b2]])
            c.op(ACT, lambda: A.activation(out=hrs[:, 0:N], in_=banks[b2][0:64, 0:N], func=AF.Ln, bias=epsc[0:64, 0:1], scale=1.0 / 64),
                 reads=[bank_b[b2]], writes=[hrs_b])
            c.op(ACT, lambda: A.activation(out=hrs[:, 0:N], in_=hrs[:, 0:N], func=AF.Exp, scale=-0.5), reads=[hrs_b], writes=[hrs_b])
            c.op(DVE, lambda: V.scalar_tensor_tensor(out=out_ap, in0=banks[bi][0:64, 0:N], scalar=hg[:, gidx:gidx + 1], in1=hrs[:, 0:N],
                                                     op0=ALU.mult, op1=ALU.mult), reads=[bank_b[bi], hrs_b], writes=[out_b])

        def head_pipeline(tasks):
            pend = None
            for (grp, N, gidx, out_ap, out_b) in tasks:
                bi = grp()
                if pend is not None:
                    head_norm_b(*pend)
                head_norm_a(bi, N)
                pend = (bi, N, gidx, out_ap, out_b)
            if pend is not None:
                head_norm_b(*pend)

        def ffn(tag, gi):
            c.barrier(Isc_b)
            rms_to_hT(lambda t: xt[:, t, :], gi, hT, hT_b, 4, xt_b)
            for i in range(11):
                wt, wb = piece((tag, "in", i))
                w4 = wt[:, 0:4096].rearrange("p (gu kc f) -> p gu kc f", gu=2, kc=8)
                for j in range(2):
                    f = 2 * i + j
                    bg, bu = nb(), nb()
                    mm_group(banks[bg][:, :], [(w4[:, 0, kc, j * 128:(j + 1) * 128], hT[:, kc, :]) for kc in range(KC)],
                             reads=[wb, hT_b], writes=[bank_b[bg]])
                    mm_group(banks[bu][:, :], [(w4[:, 1, kc, j * 128:(j + 1) * 128], hT[:, kc, :]) for kc in range(KC)],
                             reads=[wb, hT_b], writes=[bank_b[bu]])
                    si = f % 2
                    c.op(ACT, lambda bg=bg, si=si: A.activation(out=sg[si][:], in_=banks[bg][:, :], func=AF.Silu),
                         reads=[bank_b[bg]], writes=[sg_b[si]])
                    c.op(DVE, lambda bu=bu, si=si, f=f: V.tensor_tensor(out=actT[:, f, :], in0=sg[si][:], in1=banks[bu][:, :], op=ALU.mult),
                         reads=[sg_b[si], bank_b[bu]], writes=[actT_b])
            for hh in range(2):
                acc = [pin() for _ in range(4)]
                for pp in range(2):
                    wt, wb = piece((tag, "out", hh, pp))
                    w3 = wt[:, 0:5632].rearrange("p (f d) -> p f d", f=11)
                    for t in range(4):
                        mm_group(banks[acc[t]][:, :], [(actT[:, 11 * pp + fl, t * 128:(t + 1) * 128], w3[:, fl, :]) for fl in range(11)],
                                 reads=[wb, actT_b], writes=[bank_b[acc[t]]], first=(pp == 0), last=(pp == 1))
                for t in range(4):
                    xsl = xt[:, t, hh * 512:(hh + 1) * 512]
                    c.op(DVE, lambda t=t, xsl=xsl: V.scalar_tensor_tensor(out=xsl, in0=banks[acc[t]][:, :], scalar=0.5, in1=xsl,
                                                                          op0=ALU.mult, op1=ALU.add),
                         reads=[bank_b[acc[t]], xt_b[t]], writes=[xt_b[t]])
                    unpin(acc[t])

        for b in range(NB):
            for mc in range(2):
                c.dma(SP, xt[:, mc, :], mem_d[b * 256 + mc * 128: b * 256 + (mc + 1) * 128, :], sem_x, writes=[xt_b[mc]])
            rms_to_hT(lambda t: xt[:, t, :], 3, memT, memT_b, 2, xt_b)
            wt, wb = piece(("kv",))
            wk3 = wt[:, 0:4096].rearrange("p (kc d) -> p kc d", kc=8)
            def mk_grp(h):
                def g():
                    bi = nb()
                    mm_group(banks[bi][0:64, 0:256], [(wk3[:, kc, h * 64:(h + 1) * 64], memT[:, kc, :]) for kc in range(KC)],
                             reads=[wb, memT_b], writes=[bank_b[bi]])
                    return bi
                return g
            head_pipeline([(mk_grp(h), 256, 3, mkT[:, h, :], mkT_b) for h in range(4)])
            for mc in range(2):
                bi = nb()
                mm_group(banks[bi][:, 0:256], [(memT[:, kc, mc * 128:(mc + 1) * 128], wk3[:, kc, 256:512]) for kc in range(KC)],
                         reads=[wb, memT_b], writes=[bank_b[bi]])
                c.op(DVE, lambda mc=mc, bi=bi: V.tensor_copy(Vm[:, mc, :, 0:64], banks[bi][:, 0:256].rearrange("p (h d) -> p h d", h=4)),
                     reads=[bank_b[bi]], writes=[Vm_b])
            c.op(DVE, lambda: V.memset(s_init[:], 0.0), writes=[s_init_b])

            for st in range(NT):
                row0 = b * S + st * 512
                for t in range(4):
                    c.dma(SP, xt[:, t, :], x_d[row0 + t * 128:row0 + (t + 1) * 128, :], sem_x, writes=[xt_b[t]])
                ffn("f1", 0)
                if "x1" in dbg_d and b == 0:
                    for t in range(4):
                        dump("x1", xt[:, t, :], xt_b[t], dbg_d["x1"][st * 512 + t * 128:st * 512 + (t + 1) * 128, :])
                rms_to_hT(lambda t: xt[:, t, :], 1, hT, hT_b, 4, xt_b)
                def emit_mask(t):
                    L = Ls[t]
                    m = t % 2
                    c.op(DVE, lambda: V.tensor_scalar(out=MB[m][:, 0:L], in0=Isc[t][:, 0:L], scalar1=blo[:, t:t + 1], scalar2=1.0,
                                                      op0=ALU.is_gt, op1=ALU.subtract), reads=[Isc_b[t], blo_b], writes=[MB_b[m]])

                def attention(t):
                    j = st * 4 + t
                    m = t % 2
                    O = [pin(), pin()]
                    steps = [(kc, g) for kc in range(j + 1) for g in range(2)]
                    LOOK = 2
                    for i in range(len(steps) + LOOK):
                        if i < len(steps):
                            kc, g = steps[i]
                            bi = nb()
                            prs = [(kT[:, kc * 128:(kc + 1) * 128], qaT[:, 4 * g:4 * g + 4, t * 128:(t + 1) * 128]),
                                   (MB[m][:, kc * 128:(kc + 1) * 128], I8big[:, :, :])]
                            if kc >= j - 1:
                                prs.append((ident[:], biasT[:, j - kc, 4 * g:4 * g + 4, :]))
                            mm_group(banks[bi][:, :], prs, reads=[kT_b, qaT_b, MB_b[m]], writes=[bank_b[bi]])
                            e = i % 3
                            c.op(ACT, lambda bi=bi, e=e: A.activation(out=ET[e][:], in_=banks[bi][:, :], func=AF.Exp), reads=[bank_b[bi]], writes=[ET_b[e]])
                        if i >= LOOK:
                            kc, g = steps[i - LOOK]
                            e = (i - LOOK) % 3
                            mm_group(banks[O[g]][:, :], [(Vaug[:, kc, :], ET[e][:])], reads=[Vaug_b, ET_b[e]], writes=[bank_b[O[g]]],
                                     first=(kc == 0), last=(kc == j))
                    for g in range(2):
                        c.op(ACT, lambda g=g: A.activation(out=rec[64:128, :], in_=banks[O[g]][64:128, :], func=AF.Ln), reads=[bank_b[O[g]]], writes=[rec_b])
                        c.op(ACT, lambda: A.activation(out=rec[64:128, :], in_=rec[64:128, :], func=AF.Exp, scale=-1.0), reads=[rec_b], writes=[rec_b])
                        c.op(DVE, lambda g=g: V.tensor_tensor(out=OaT[:, 4 * g:4 * g + 4, t * 128:(t + 1) * 128],
                                                             in0=banks[O[g]][0:64, :].rearrange("p (h q) -> p h q", h=4),
                                                             in1=rec[64:128, :].rearrange("p (h q) -> p h q", h=4), op=ALU.mult),
                             reads=[bank_b[O[g]], rec_b], writes=[OaT_b])
                        unpin(O[g])

                ssm_banks = {}

                def ssm_B(sub):
                    tk = slice(sub * 128, (sub + 1) * 128)
                    for hh in range(2):
                        pr_, pi_ = pin(), pin()
                        ssm_banks[(sub, hh)] = (pr_, pi_)
                        for ri, bi in ((0, pr_), (1, pi_)):
                            def fn(ri=ri, bi=bi, hh=hh):
                                ins = None
                                for cl in range(4):
                                    ins = T.matmul(banks[bi][:, cl * 128:(cl + 1) * 128], BpT[:, ri, 4 * hh + cl, :], uT[:, hh, tk], start=True, stop=True)
                                return ins
                            c.op(PE, fn, reads=[uT_b], writes=[bank_b[bi]])

                def ssm_dve(sub):
                    s1, s3, s4 = stmp[2], stmp[3], stmp[4]
                    z1, z3, z4 = stmp_b[2], stmp_b[3], stmp_b[4]
                    Wb = [(stmp[0], stmp[1], stmp_b[0], stmp_b[1]), (stmp[5], stmp[6], stmp_b[5], stmp_b[6])]
                    for hh in range(2):
                        pr_, pi_ = ssm_banks[(sub, hh)]
                        cs = cosT[:, 4 * hh:4 * hh + 4, :].rearrange("p c n -> p (c n)")
                        sn = sinT[:, 4 * hh:4 * hh + 4, :].rearrange("p c n -> p (c n)")
                        s0, s2, z0, z2 = Wb[hh]
                        PR, PI = banks[pr_][:, :], banks[pi_][:, :]
                        c.op(DVE, lambda: V.tensor_tensor(out=s0[:], in0=PR, in1=cs, op=ALU.mult), reads=[bank_b[pr_]], writes=[z0])
                        c.op(DVE, lambda: V.tensor_tensor(out=s1[:], in0=PI, in1=sn, op=ALU.mult), reads=[bank_b[pi_]], writes=[z1])
                        c.op(DVE, lambda: V.tensor_tensor(out=s0[:], in0=s0[:], in1=s1[:], op=ALU.add), reads=[z0, z1], writes=[z0])
                        c.op(DVE, lambda: V.tensor_tensor(out=s2[:], in0=PI, in1=cs, op=ALU.mult), reads=[bank_b[pi_]], writes=[z2])
                        c.op(DVE, lambda: V.tensor_tensor(out=s1[:], in0=PR, in1=sn, op=ALU.mult), reads=[bank_b[pr_], z1], writes=[z1])
                        c.op(DVE, lambda: V.tensor_tensor(out=s2[:], in0=s2[:], in1=s1[:], op=ALU.subtract), reads=[z2, z1], writes=[z2])
                        unpin(pr_); unpin(pi_)
                    for hh in range(2):
                        cs = cosT[:, 4 * hh:4 * hh + 4, :].rearrange("p c n -> p (c n)")
                        sn = sinT[:, 4 * hh:4 * hh + 4, :].rearrange("p c n -> p (c n)")
                        s0, s2, z0, z2 = Wb[hh]
                        for cl in range(4):
                            cc = 4 * hh + cl
                            sl = slice(cl * 128, (cl + 1) * 128)
                            mgb = ssmc[:, MAG, cc:cc + 1].to_broadcast([128, 128])
                            c.op(DVE, lambda cc=cc, sl=sl, mgb=mgb: V.tensor_tensor_scan(out=s3[:, sl], data0=mgb, data1=s0[:, sl], initial=s_init[:, 0, cc:cc + 1],
                                                                                         op0=ALU.mult, op1=ALU.add), reads=[z0, s_init_b], writes=[z3])
                            c.op(DVE, lambda cc=cc, sl=sl, mgb=mgb: V.tensor_tensor_scan(out=s4[:, sl], data0=mgb, data1=s2[:, sl], initial=s_init[:, 1, cc:cc + 1],
                                                                                         op0=ALU.mult, op1=ALU.add), reads=[z2, s_init_b], writes=[z4])
                        q = ssm_small
                        lastc = lambda ap: ap.rearrange("p (c n) -> p c n", c=4)[:, :, 127]
                        c.op(DVE, lambda: V.tensor_tensor(out=s0[:], in0=s3[:], in1=cs, op=ALU.mult), reads=[z3, z0], writes=[z0])
                        c.op(DVE, lambda: V.tensor_tensor(out=s1[:], in0=s4[:], in1=sn, op=ALU.mult), reads=[z4, z1], writes=[z1])
                        c.op(DVE, lambda hh=hh: V.tensor_tensor(out=xrb[:, hh, 0, :], in0=s0[:], in1=s1[:], op=ALU.subtract), reads=[z0, z1], writes=[xrb_b[hh]])
                        c.op(DVE, lambda: V.tensor_tensor(out=q[:, 0, :], in0=lastc(s0[:]), in1=lastc(s1[:]), op=ALU.subtract), reads=[z0, z1], writes=[ssm_small_b])
                        c.op(DVE, lambda: V.tensor_tensor(out=s2[:], in0=s3[:], in1=sn, op=ALU.mult), reads=[z3, z2], writes=[z2])
                        c.op(DVE, lambda: V.tensor_tensor(out=s1[:], in0=s4[:], in1=cs, op=ALU.mult), reads=[z4, z1], writes=[z1])
                        c.op(DVE, lambda hh=hh: V.tensor_tensor(out=xrb[:, hh, 1, :], in0=s2[:], in1=s1[:], op=ALU.add), reads=[z2, z1], writes=[xrb_b[hh]])
                        c.op(DVE, lambda: V.tensor_tensor(out=q[:, 2, :], in0=lastc(s2[:]), in1=lastc(s1[:]), op=ALU.add), reads=[z2, z1], writes=[ssm_small_b])
                        cth = ssmc[:, COS, 4 * hh:4 * hh + 4]; sth = ssmc[:, SIN, 4 * hh:4 * hh + 4]
                        c.op(DVE, lambda: V.tensor_tensor(out=q[:, 1, :], in0=q[:, 2, :], in1=sth, op=ALU.mult), reads=[ssm_small_b], writes=[ssm_small_b])
                        c.op(DVE, lambda: V.tensor_tensor(out=q[:, 3, :], in0=q[:, 2, :], in1=cth, op=ALU.mult), reads=[ssm_small_b], writes=[ssm_small_b])
                        c.op(DVE, lambda: V.tensor_tensor(out=q[:, 2, :], in0=q[:, 0, :], in1=sth, op=ALU.mult), reads=[ssm_small_b], writes=[ssm_small_b])
                        c.op(DVE, lambda: V.tensor_tensor(out=q[:, 0, :], in0=q[:, 0, :], in1=cth, op=ALU.mult), reads=[ssm_small_b], writes=[ssm_small_b])
                        c.op(DVE, lambda hh=hh: V.tensor_tensor(out=s_init[:, 0, 4 * hh:4 * hh + 4], in0=q[:, 0, :], in1=q[:, 1, :], op=ALU.subtract),
                             reads=[ssm_small_b, s_init_b], writes=[s_init_b])
                        c.op(DVE, lambda hh=hh: V.tensor_tensor(out=s_init[:, 1, 4 * hh:4 * hh + 4], in0=q[:, 2, :], in1=q[:, 3, :], op=ALU.add),
                             reads=[ssm_small_b, s_init_b], writes=[s_init_b])

                def ssm_C(sub):
                    tk = slice(sub * 128, (sub + 1) * 128)
                    for hh in range(2):
                        by = nb()
                        prs = []
                        for cl in range(4):
                            prs.append((Cp[:, 0, 4 * hh + cl, :], xrb[:, hh, 0, cl * 128:(cl + 1) * 128]))
                            prs.append((Cp[:, 1, 4 * hh + cl, :], xrb[:, hh, 1, cl * 128:(cl + 1) * 128]))
                        mm_group(banks[by][:, 0:128], prs, reads=[xrb_b[hh]], writes=[bank_b[by]])
                        c.op(DVE, lambda by=by, hh=hh: V.scalar_tensor_tensor(out=yf[:], in0=uT[:, hh, tk], scalar=dcol[:, hh:hh + 1], in1=banks[by][:, 0:128],
                                                                              op0=ALU.mult, op1=ALU.add), reads=[uT_b, bank_b[by]], writes=[yf_b])
                        c.op(ACT, lambda hh=hh: A.activation(out=ysT[:, hh, tk], in_=yf[:], func=AF.Gelu), reads=[yf_b], writes=[ysT_b])

                def mem_attention():
                    steps = [(h, mc) for h in range(4) for mc in range(2)]
                    om = {}
                    LOOK = 2
                    for i in range(len(steps) + LOOK):
                        if i < len(steps):
                            h, mc = steps[i]
                            bi = nb()
                            mm_group(banks[bi][:, :], [(mkT[:, h, mc * 128:(mc + 1) * 128], qmT[:, h, :])], reads=[mkT_b, qmT_b], writes=[bank_b[bi]])
                            e = i % 3
                            c.op(ACT, lambda bi=bi, e=e: A.activation(out=ET[e][:], in_=banks[bi][:, :], func=AF.Exp), reads=[bank_b[bi]], writes=[ET_b[e]])
                        if i >= LOOK:
                            h, mc = steps[i - LOOK]
                            e = (i - LOOK) % 3
                            if mc == 0:
                                om[h] = pin()
                            mm_group(banks[om[h]][:, :], [(Vm[:, mc, h, :], ET[e][:])], reads=[Vm_b, ET_b[e]], writes=[bank_b[om[h]]], first=(mc == 0), last=(mc == 1))
                            if mc == 1:
                                o_ = om[h]
                                c.op(ACT, lambda o_=o_: A.activation(out=rec[64:128, :], in_=banks[o_][64:128, :], func=AF.Ln), reads=[bank_b[o_]], writes=[rec_b])
                                c.op(ACT, lambda: A.activation(out=rec[64:128, :], in_=rec[64:128, :], func=AF.Exp, scale=-1.0), reads=[rec_b], writes=[rec_b])
                                c.op(DVE, lambda o_=o_, h=h: V.tensor_tensor(out=OmT[:, h, :], in0=banks[o_][0:64, :], in1=rec[64:128, :], op=ALU.mult),
                                     reads=[bank_b[o_], rec_b], writes=[OmT_b])
                                unpin(o_)

                def hd_grp(w3, wb, c0):
                    def g():
                        bi = nb()
                        mm_group(banks[bi][0:64, :], [(w3[:, kc, c0:c0 + 64], hT[:, kc, :]) for kc in range(KC)],
                                 reads=[wb, hT_b], writes=[bank_b[bi]])
                        return bi
                    return g
                wt, wb = piece(("p2",))
                w3 = wt[:, 0:8 * 452].rearrange("p (kc d) -> p kc d", kc=8)
                head_pipeline([(hd_grp(w3, wb, 0), 512, 1, kT[:, st * 512:(st + 1) * 512], kT_b)])
                bi = nb()
                mm_group(banks[bi][0:64, :], [(w3[:, kc, 384:448], hT[:, kc, :]) for kc in range(KC)], reads=[wb, hT_b], writes=[bank_b[bi]])
                c.op(ACT, lambda bi=bi: A.copy(out=kiT[:, st * 512:(st + 1) * 512], in_=banks[bi][0:64, :]), reads=[bank_b[bi]], writes=[kiT_b])
                for h in range(4):
                    bi = nb()
                    mm_group(banks[bi][0:64, :], [(w3[:, kc, 128 + h * 64:192 + h * 64], hT[:, kc, :]) for kc in range(KC)],
                             reads=[wb, hT_b], writes=[bank_b[bi]])
                    c.op(ACT, lambda bi=bi, h=h: A.copy(out=qiT[:, h, :], in_=banks[bi][0:64, :]), reads=[bank_b[bi]], writes=[qiT_b])
                for t in range(4):
                    bi = nb()
                    mm_group(banks[bi][:, 0:64], [(hT[:, kc, t * 128:(t + 1) * 128], w3[:, kc, 64:128]) for kc in range(KC)],
                             reads=[wb, hT_b], writes=[bank_b[bi]])
                    mm_group(banks[bi][:, 64:68], [(hT[:, kc, t * 128:(t + 1) * 128], w3[:, kc, 448:452]) for kc in range(KC)],
                             reads=[wb, hT_b], writes=[bank_b[bi]])
                    c.op(DVE, lambda bi=bi, t=t: V.tensor_copy(Vaug[:, st * 4 + t, 0:64], banks[bi][:, 0:64]), reads=[bank_b[bi]], writes=[Vaug_b])
                    c.op(DVE, lambda bi=bi, t=t: V.tensor_scalar(out=wq[:, t, :], in0=banks[bi][:, 64:68], scalar1=0.0625, scalar2=None, op0=ALU.mult),
                         reads=[bank_b[bi]], writes=[wq_b])
                wt1, wb1 = piece(("p1",))
                w31 = wt1[:, 0:4096].rearrange("p (kc d) -> p kc d", kc=8)
                wt3, wb3 = piece(("p3",))
                w33 = wt3[:, 0:4096].rearrange("p (kc d) -> p kc d", kc=8)
                hp_pend = [None]

                def hp_push(task):
                    grp, N, gidx, out_ap, out_b = task
                    bi = grp()
                    if hp_pend[0] is not None:
                        head_norm_b(*hp_pend[0])
                    head_norm_a(bi, N)
                    hp_pend[0] = (bi, N, gidx, out_ap, out_b)

                def hp_flush():
                    if hp_pend[0] is not None:
                        head_norm_b(*hp_pend[0])
                        hp_pend[0] = None

                def u_grp(m):
                    bi = nb()
                    mm_group(banks[bi][:, :], [(w33[:, kc, m * 128:(m + 1) * 128], hT[:, kc, :]) for kc in range(KC)],
                             reads=[wb3, hT_b], writes=[bank_b[bi]])
                    c.op(ACT, lambda: A.copy(out=uT[:, m, :], in_=banks[bi][:, :]), reads=[bank_b[bi]], writes=[uT_b])

                fillers = []
                for h in range(8):
                    fillers.append(lambda h=h: hp_push((hd_grp(w31, wb1, h * 64), 512, 0, qaT[:, h, :], qaT_b)))
                for m in range(2):
                    fillers.append(lambda m=m: u_grp(m))
                for h in range(4):
                    fillers.append(lambda h=h: hp_push((hd_grp(w33, wb3, 256 + h * 64), 512, 2, qmT[:, h, :], qmT_b)))
                fillers.append(hp_flush)
                fillers.append(lambda: mem_attention())

                c.barrier([actT_b])
                for t in range(4):
                    j = st * 4 + t
                    L = (j + 1) * 128
                    for kb in range((L + 511) // 512):
                        k0 = kb * 512
                        kn = min(512, L - k0)
                        for h in range(4):
                            bi = nb()
                            mm_group(banks[bi][:, 0:kn], [(qiT[:, h, t * 128:(t + 1) * 128], kiT[:, k0:k0 + kn])],
                                     reads=[qiT_b, kiT_b], writes=[bank_b[bi]])
                            if h == 0:
                                c.op(DVE, lambda bi=bi, t=t, k0=k0, kn=kn: V.tensor_scalar(
                                    out=Isc[t][:, k0:k0 + kn], in0=banks[bi][:, 0:kn], scalar1=0.0, scalar2=wq[:, t, 0:1], op0=ALU.max, op1=ALU.mult),
                                    reads=[bank_b[bi], wq_b], writes=[Isc_b[t]])
                            else:
                                ri = h % 2
                                c.op(ACT, lambda bi=bi, ri=ri, kn=kn: A.activation(out=rl[ri][:, 0:kn], in_=banks[bi][:, 0:kn], func=AF.Relu),
                                     reads=[bank_b[bi]], writes=[rl_b[ri]])
                                c.op(DVE, lambda ri=ri, t=t, h=h, k0=k0, kn=kn: V.scalar_tensor_tensor(
                                    out=Isc[t][:, k0:k0 + kn], in0=rl[ri][:, 0:kn], scalar=wq[:, t, h:h + 1], in1=Isc[t][:, k0:k0 + kn],
                                    op0=ALU.mult, op1=ALU.add), reads=[rl_b[ri], wq_b, Isc_b[t]], writes=[Isc_b[t]])
                    c.op(DVE, lambda t=t, L=L: V.tensor_reduce(out=bmx[:, t:t + 1], in_=Isc[t][:, 0:L], axis=mybir.AxisListType.X, op=ALU.max),
                         reads=[Isc_b[t]], writes=[bmx_b])
                    c.op(DVE, lambda t=t, L=L: V.tensor_reduce(out=blo[:, t:t + 1], in_=Isc[t][:, 0:L], axis=mybir.AxisListType.X, op=ALU.min),
                         reads=[Isc_b[t]], writes=[blo_b])
                    c.op(DVE, lambda t=t, L=L: V.memset(Isc[t][0:64, L - 64:L], -BIG), reads=[Isc_b[t]], writes=[Isc_b[t]])
                need = [((st * 4 + t + 1) * 128 > TOPK) for t in range(4)]
                Ls = [(st * 4 + t + 1) * 128 for t in range(4)]
                on_act = [False, True, True, False]
                if any(need):
                    for t in range(4):
                        thr = float(Ls[t] - 2 * TOPK) if on_act[t] else float(Ls[t] - TOPK)
                        c.op(DVE, lambda t=t, thr=thr: V.memset(bthr[:, t:t + 1], thr), writes=[bthr_b])
                    c.op(DVE, lambda: V.tensor_tensor(out=bw[:], in0=bmx[:], in1=blo[:], op=ALU.subtract), reads=[bmx_b, blo_b], writes=[bw_b])
                    c.op(DVE, lambda: V.tensor_scalar(out=bpw[:], in0=bw[:], scalar1=1e-3, scalar2=1e-6, op0=ALU.mult, op1=ALU.add),
                         reads=[bw_b], writes=[bpw_b])
                    c.op(DVE, lambda: V.tensor_tensor(out=blo[:], in0=blo[:], in1=bpw[:], op=ALU.subtract), reads=[bpw_b, blo_b], writes=[blo_b])
                    c.op(DVE, lambda: V.tensor_tensor(out=bw[:], in0=bmx[:], in1=blo[:], op=ALU.subtract), reads=[bmx_b, blo_b], writes=[bw_b])
                    for t in range(4):
                        if not need[t]:
                            c.op(DVE, lambda t=t: V.memset(bcnt[:, t:t + 1], 0.0), writes=[bcnt_c[t]])
                    for it in range(NITER):
                        hw_ = 0.5 ** (it + 1)
                        c.op(DVE, lambda hw_=hw_: V.scalar_tensor_tensor(out=bmid[:], in0=bw[:], scalar=hw_, in1=blo[:], op0=ALU.mult, op1=ALU.add),
                             reads=[blo_b, bw_b], writes=[bmid_b])
                        for t in (1, 2, 3, 0):
                            if not need[t]:
                                continue
                            L = Ls[t]
                            if on_act[t]:
                                c.op(ACT, lambda t=t, L=L: A.activation(out=junkA[:, 0:L], in_=Isc[t][:, 0:L], func=AF.Sign, bias=bmid[:, t:t + 1], scale=-1.0, saturate=False,
                                                                        accum_out=bcnt[:, t:t + 1]),
                                     reads=[Isc_b[t], bmid_b], writes=[junkA_b, bcnt_c[t]])
                            else:
                                c.op(DVE, lambda t=t, L=L: V.tensor_scalar(out=junk[:, 0:L], in0=Isc[t][:, 0:L], scalar1=bmid[:, t:t + 1], scalar2=0.0,
                                                                            op0=ALU.is_le, op1=ALU.add, accum_out=bcnt[:, t:t + 1]),
                                     reads=[Isc_b[t], bmid_b], writes=[junk_b, bcnt_c[t]])
                        c.op(DVE, lambda: V.tensor_tensor(out=bpw[:], in0=bcnt[:], in1=bthr[:], op=ALU.is_le), reads=bcnt_c + [bthr_b], writes=[bpw_b])
                        c.op(DVE, lambda: V.tensor_tensor(out=bpw[:], in0=bpw[:], in1=bw[:], op=ALU.mult), reads=[bpw_b, bw_b], writes=[bpw_b])
                        c.op(DVE, lambda hw_=hw_: V.scalar_tensor_tensor(out=blo[:], in0=bpw[:], scalar=hw_, in1=blo[:], op0=ALU.mult, op1=ALU.add),
                             reads=[blo_b, bpw_b], writes=[blo_b])
                        if fillers:
                            fillers.pop(0)()
                while fillers:
                    fillers.pop(0)()
                for t in range(4):
                    if not need[t]:
                        c.op(DVE, lambda t=t: V.memset(blo[:, t:t + 1], -1.0e29), reads=[blo_b], writes=[blo_b])
                if "isc" in dbg_d and b == 0 and st == NT - 1:
                    dump("isc", Isc[3][:, 0:S], Isc_b[3], dbg_d["isc"][:, :])
                    dump("lo", blo[:, :], blo_b, dbg_d["lo"][:, :])

                emit_mask(0)
                emit_mask(1)
                attention(0)
                ssm_B(0); ssm_dve(0)
                emit_mask(2)
                attention(1)
                ssm_C(0); ssm_B(1); ssm_dve(1)
                emit_mask(3)
                attention(2)
                ssm_C(1); ssm_B(2); ssm_dve(2)
                attention(3)
                ssm_C(2); ssm_B(3); ssm_dve(3)
                ssm_C(3)

                for f in range(8):
                    wt, wb = piece(("mg", f))
                    wg = wt[:, 0:3072].rearrange("p (br kc d) -> p br kc d", br=3, kc=8)
                    woa = wt[0:64, 3072:4096].rearrange("p (h d) -> p h d", h=8)
                    wgl = wt[:, 4096:4608].rearrange("p (ab kc d) -> p ab kc d", ab=2, kc=2)
                    wom = wt[0:64, 4608:5120].rearrange("p (h d) -> p h d", h=4)
                    gb = [nb() for _ in range(3)]
                    for br in range(3):
                        mm_group(banks[gb[br]][:, :], [(wg[:, br, kc, :], hT[:, kc, :]) for kc in range(KC)], reads=[wb, hT_b], writes=[bank_b[gb[br]]])
                    ba = nb()
                    mm_group(banks[ba][:, :], [(woa[:, h, :], OaT[:, h, :]) for h in range(8)], reads=[wb, OaT_b], writes=[bank_b[ba]])
                    bga, bgb = nb(), nb()
                    mm_group(banks[bga][:, :], [(wgl[:, 0, kc, :], ysT[:, kc, :]) for kc in range(2)], reads=[wb, ysT_b], writes=[bank_b[bga]])
                    mm_group(banks[bgb][:, :], [(wgl[:, 1, kc, :], ysT[:, kc, :]) for kc in range(2)], reads=[wb, ysT_b], writes=[bank_b[bgb]])
                    bm = nb()
                    mm_group(banks[bm][:, :], [(wom[:, h, :], OmT[:, h, :]) for h in range(4)], reads=[wb, OmT_b], writes=[bank_b[bm]])
                    m0, m1, m2 = stmp[0], stmp[1], stmp[2]
                    y0, y1, y2 = stmp_b[0], stmp_b[1], stmp_b[2]
                    c.op(ACT, lambda: A.activation(out=m0[:], in_=banks[gb[0]][:, :], func=AF.Sigmoid), reads=[bank_b[gb[0]]], writes=[y0])
                    c.op(DVE, lambda: V.tensor_tensor(out=m0[:], in0=m0[:], in1=banks[ba][:, :], op=ALU.mult), reads=[y0, bank_b[ba]], writes=[y0])
                    c.op(ACT, lambda: A.activation(out=m1[:], in_=banks[bgb][:, :], func=AF.Sigmoid), reads=[bank_b[bgb]], writes=[y1])
                    c.op(DVE, lambda: V.tensor_tensor(out=m1[:], in0=m1[:], in1=banks[bga][:, :], op=ALU.mult), reads=[y1, bank_b[bga]], writes=[y1])
                    c.op(ACT, lambda: A.activation(out=m2[:], in_=banks[gb[1]][:, :], func=AF.Sigmoid), reads=[bank_b[gb[1]]], writes=[y2])
                    c.op(DVE, lambda: V.tensor_tensor(out=m1[:], in0=m1[:], in1=m2[:], op=ALU.mult), reads=[y1, y2], writes=[y1])
                    c.op(DVE, lambda: V.tensor_tensor(out=m0[:], in0=m0[:], in1=m1[:], op=ALU.add), reads=[y0, y1], writes=[y0])
                    c.op(ACT, lambda: A.activation(out=m2[:], in_=banks[gb[2]][:, :], func=AF.Sigmoid), reads=[bank_b[gb[2]]], writes=[y2])
                    c.op(DVE, lambda: V.tensor_tensor(out=m2[:], in0=m2[:], in1=banks[bm][:, :], op=ALU.mult), reads=[y2, bank_b[bm]], writes=[y2])
                    c.op(DVE, lambda f=f: V.tensor_tensor(out=mgT[:, f, :], in0=m0[:], in1=m2[:], op=ALU.add), reads=[y0, y2], writes=[mgT_b])
                for hh in range(2):
                    wt, wb = piece(("wo", hh))
                    w3 = wt[:, 0:4096].rearrange("p (kc d) -> p kc d", kc=8)
                    for t in range(4):
                        bi = nb()
                        mm_group(banks[bi][:, :], [(mgT[:, kc, t * 128:(t + 1) * 128], w3[:, kc, :]) for kc in range(KC)],
                                 reads=[wb, mgT_b], writes=[bank_b[bi]])
                        xsl = xt[:, t, hh * 512:(hh + 1) * 512]
                        c.op(DVE, lambda bi=bi, xsl=xsl: V.tensor_tensor(out=xsl, in0=xsl, in1=banks[bi][:, :], op=ALU.add),
                             reads=[bank_b[bi], xt_b[t]], writes=[xt_b[t]])
                if "x2" in dbg_d and b == 0:
                    for t in range(4):
                        dump("x2", xt[:, t, :], xt_b[t], dbg_d["x2"][st * 512 + t * 128:st * 512 + (t + 1) * 128, :])
                ffn("f2", 2)
                for t in range(4):
                    c.op(ACT, lambda t=t: A.activation(out=junk[:, 0:D], in_=xt[:, t, :], func=AF.Square, accum_out=ss[:, t:t + 1]),
                         reads=[xt_b[t]], writes=[junk_b, ss_b])
                    c.op(ACT, lambda t=t: A.activation(out=ss[:, 4 + t:5 + t], in_=ss[:, t:t + 1], func=AF.Ln, bias=epsc[:, 0:1], scale=1.0 / D),
                         reads=[ss_b], writes=[ss_b])
                    c.op(ACT, lambda t=t: A.activation(out=ss[:, 4 + t:5 + t], in_=ss[:, 4 + t:5 + t], func=AF.Exp, scale=-0.5), reads=[ss_b], writes=[ss_b])
                    c.op(DVE, lambda t=t: V.scalar_tensor_tensor(out=xt[:, t, :], in0=xt[:, t, :], scalar=ss[:, 4 + t:5 + t], in1=gfin[:],
                                                                 op0=ALU.mult, op1=ALU.mult), reads=[xt_b[t], ss_b], writes=[xt_b[t]])
                    c.dma(POOL, out_d[row0 + t * 128:row0 + (t + 1) * 128, :], xt[:, t, :], sem_out, reads=[xt_b[t]])
        G.wait_ge(sem_out.h, sem_out.n)
        if sem_misc.n:
            G.wait_ge(sem_misc.h, sem_misc.n)
    return nc


_CACHE = {}


def _run(inputs, NB, S, dbg=()):
    key = (NB, S, str(dbg))
    if key not in _CACHE:
        _CACHE[key] = build(NB, S, dbg)
    nc = _CACHE[key]
    x = np.ascontiguousarray(inputs["x"], dtype=np.float32)
    mem = np.ascontiguousarray(inputs["mem"], dtype=np.float32)
    ncore = 8
    wmap = {n: np.ascontiguousarray(np.asarray(inputs[n], dtype=np.float32).reshape(WSHAPES[n])) for n in WNAMES}
    wmap["onehot"] = _onehot_table()
    in_maps = []
    for i in range(ncore):
        m = dict(wmap)
        m["x"] = x[i * NB:(i + 1) * NB].reshape(NB * S, D)
        m["mem"] = mem[i * NB:(i + 1) * NB].reshape(NB * 256, D)
        in_maps.append(m)
    res = run_bass_kernel_spmd(nc, in_maps, core_ids=list(range(ncore)))
    return res


def kernel(**inputs):
    B, S = inputs["x"].shape[0], inputs["x"].shape[1]
    NB = B // 8
    res = _run(inputs, NB, S)
    out = np.concatenate([r["out"].reshape(NB, S, D) for r in res.results], axis=0)
    return out.astype(np.float32)
```

```python
import math
from contextlib import ExitStack
import numpy as np
import concourse.bass as bass
import concourse.mybir as mybir
from concourse.bass_utils import run_bass_kernel_spmd

F32 = mybir.dt.float32
BF16 = mybir.dt.bfloat16
I32 = mybir.dt.int32
AF = mybir.ActivationFunctionType
ALU = mybir.AluOpType

D = 1024
KC = 8
FFN = 2816
FC = 22
WIN = 4548
EPS = 1e-6
NEG = 30000.0
BIG = 1.0e30
SLOT = 5632
NITER = 20


class Tok:
    __slots__ = ("sem", "val")

    def __init__(self, sem, val):
        self.sem = sem
        self.val = val


class Sem:
    def __init__(self, h):
        self.h = h
        self.n = 0


class Buf:
    def __init__(self, name):
        self.name = name
        self.w = None
        self.r = {}


class Eng:
    def __init__(self, eng, sem):
        self.eng = eng
        self.sem = sem
        self.waited = {}

    def wait(self, tok):
        if tok is None:
            return
        k = id(tok.sem)
        if self.waited.get(k, 0) >= tok.val:
            return
        self.eng.wait_ge(tok.sem.h, tok.val)
        self.waited[k] = tok.val


class Ctx:
    def __init__(self, nc, es):
        self.nc = nc
        self.es = es
        self.PE = Eng(nc.tensor, self.sem("pe"))
        self.ACT = Eng(nc.scalar, self.sem("act"))
        self.DVE = Eng(nc.vector, self.sem("dve"))
        self.POOL = Eng(nc.gpsimd, self.sem("pool"))
        self.SP = Eng(nc.sync, self.sem("sp"))

    def sem(self, name):
        return Sem(self.es.enter_context(self.nc.semaphore(name)))

    def _deps(self, E, reads, writes):
        for b in reads:
            E.wait(b.w)
        pe = E is self.PE
        for b in writes:
            if b.w is not None and not (pe and b.w.sem is E.sem):
                E.wait(b.w)
            for t in b.r.values():
                if not (pe and t.sem is E.sem):
                    E.wait(t)

    def _commit(self, tok, reads, writes):
        for b in reads:
            b.r[id(tok.sem)] = tok
        for b in writes:
            b.w = tok
            b.r = {}

    def op(self, E, fn, reads=(), writes=()):
        self._deps(E, reads, writes)
        ins = fn()
        E.sem.n += 1
        ins.then_inc(E.sem.h, 1)
        tok = Tok(E.sem, E.sem.n)
        self._commit(tok, reads, writes)
        return tok

    def dma(self, Q, out, in_, sem, reads=(), writes=(), **kw):
        self._deps(Q, reads, writes)
        ins = Q.eng.dma_start(out=out, in_=in_, **kw)
        sem.n += 16
        ins.then_inc(sem.h, 16)
        tok = Tok(sem, sem.n)
        self._commit(tok, reads, writes)
        return tok

    def barrier(self, bufs):
        for E in (self.PE, self.ACT, self.DVE, self.POOL, self.SP):
            for b in bufs:
                E.wait(b.w)
                for t in b.r.values():
                    E.wait(t)


def _t5_bucket_np(rel):
    half = 16
    max_exact = 8
    n = np.abs(rel)
    nf = np.maximum(n, 1).astype(np.float32)
    large = max_exact + (np.log(nf / np.float32(max_exact)) / np.float32(math.log(128 / 8))
                         * np.float32(half - max_exact)).astype(np.int32)
    large = np.minimum(large, half - 1)
    return np.where(rel > 0, half, 0) + np.where(n < max_exact, n, large)


def _onehot_table():
    rel = np.arange(-255, 129, dtype=np.int32)
    bk = _t5_bucket_np(rel)
    oh = np.zeros((32, 384), np.float32)
    oh[bk, np.arange(384)] = 1.0
    return oh


WNAMES = ["ffn1_norm", "ffn1_w_in", "ffn1_w_out", "mix_norm", "w_in", "a_q_gain", "a_k_gain", "rel_bias",
          "w_o_a", "ssm_lambda_re", "ssm_lambda_im", "ssm_log_dt", "ssm_b_re", "ssm_b_im", "ssm_c_re",
          "ssm_c_im", "ssm_d", "w_glu", "mem_norm", "w_mem_kv", "m_q_gain", "m_k_gain", "w_o_m", "w_out",
          "ffn2_norm", "ffn2_w_in", "ffn2_w_out", "final_norm"]
WSHAPES = {"ffn1_norm": [1, D], "ffn1_w_in": [D, 2 * FFN], "ffn1_w_out": [FFN, D], "mix_norm": [1, D],
           "w_in": [D, WIN], "a_q_gain": [64, 1], "a_k_gain": [64, 1], "rel_bias": [32, 8],
           "w_o_a": [512, D], "ssm_lambda_re": [16, 64], "ssm_lambda_im": [16, 64], "ssm_log_dt": [1, 16],
           "ssm_b_re": [16, 64, 16], "ssm_b_im": [16, 64, 16], "ssm_c_re": [16, 16, 64],
           "ssm_c_im": [16, 16, 64], "ssm_d": [256, 1], "w_glu": [256, 2 * D], "mem_norm": [1, D],
           "w_mem_kv": [D, 512], "m_q_gain": [64, 1], "m_k_gain": [64, 1], "w_o_m": [256, D],
           "w_out": [D, D], "ffn2_norm": [1, D], "ffn2_w_in": [D, 2 * FFN], "ffn2_w_out": [FFN, D],
           "final_norm": [1, D]}


def build(NB, S, dbg=()):
    NT = S // 512
    TOPK = min(256, S // 4)
    nc = bass.Bass("TRN2", target_bir_lowering=False)
    x_d = nc.dram_tensor("x", [NB * S, D], F32, kind="ExternalInput").ap()
    mem_d = nc.dram_tensor("mem", [NB * 256, D], F32, kind="ExternalInput").ap()
    W = {n: nc.dram_tensor(n, WSHAPES[n], F32, kind="ExternalInput").ap() for n in WNAMES}
    oh_d = nc.dram_tensor("onehot", [32, 384], F32, kind="ExternalInput").ap()
    out_d = nc.dram_tensor("out", [NB * S, D], F32, kind="ExternalOutput").ap()
    dbg_d = {n: nc.dram_tensor("dbg_" + n, shp, F32, kind="ExternalOutput").ap() for n, shp in dbg}

    pieces = []

    def ffn_pieces(tag, win, wout):
        wi4 = win.rearrange("(kc p) (gu f) -> p kc gu f", p=128, gu=2)
        for i in range(11):
            srcs = [(0, 128, gu * 2048, [8, 256], wi4[:, :, gu, 256 * i:256 * i + 256]) for gu in range(2)]
            pieces.append(((tag, "in", i), srcs))
        wo3 = wout.rearrange("(f p) d -> p f d", p=128)
        for hh in range(2):
            for pp in range(2):
                pieces.append(((tag, "out", hh, pp),
                               [(0, 128, 0, [11, 512], wo3[:, 11 * pp:11 * pp + 11, 512 * hh:512 * hh + 512])]))

    win3 = W["w_in"].rearrange("(kc p) c -> p kc c", p=128)
    ffn_pieces("f1", W["ffn1_w_in"], W["ffn1_w_out"])
    pieces.append((("p1",), [(0, 128, 0, [8, 512], win3[:, :, 0:512])]))
    pieces.append((("p2",), [(0, 128, 0, [8, 452], win3[:, :, 512:964])]))
    pieces.append((("p3",), [(0, 128, 0, [8, 512], win3[:, :, 964:1476])]))
    woa3 = W["w_o_a"].rearrange("(h p) d -> p h d", p=64)
    wglu3 = W["w_glu"].rearrange("(kc p) d -> p kc d", p=128)
    wom3 = W["w_o_m"].rearrange("(h p) d -> p h d", p=64)
    for f in range(8):
        srcs = []
        for br in range(3):
            c0 = 1476 + br * 1024 + f * 128
            srcs.append((0, 128, br * 1024, [8, 128], win3[:, :, c0:c0 + 128]))
        srcs.append((0, 64, 3072, [8, 128], woa3[:, :, f * 128:f * 128 + 128]))
        for ab in range(2):
            srcs.append((0, 128, 4096 + ab * 256, [2, 128], wglu3[:, :, ab * 1024 + f * 128:ab * 1024 + f * 128 + 128]))
        srcs.append((0, 64, 4608, [4, 128], wom3[:, :, f * 128:f * 128 + 128]))
        pieces.append((("mg", f), srcs))
    wout3 = W["w_out"].rearrange("(kc p) d -> p kc d", p=128)
    for hh in range(2):
        pieces.append((("wo", hh), [(0, 128, 0, [8, 512], wout3[:, :, 512 * hh:512 * hh + 512])]))
    ffn_pieces("f2", W["ffn2_w_in"], W["ffn2_w_out"])
    wkv3 = W["w_mem_kv"].rearrange("(kc p) d -> p kc d", p=128)
    pieces.append((("kv",), [(0, 128, 0, [8, 512], wkv3[:, :, :])]))
    pidx = {k: i for i, (k, _) in enumerate(pieces)}
    NP = len(pieces)
    wscr = nc.dram_tensor("wscr", [NP, 128, SLOT], BF16, kind="Internal").ap()
    fd_d = nc.dram_tensor("fdscr", [8, 384], F32, kind="Internal").ap()

    with ExitStack() as es:
        c = Ctx(nc, es)
        PE, ACT, DVE, POOL, SP = c.PE, c.ACT, c.DVE, c.POOL, c.SP
        V, A, G, T = nc.vector, nc.scalar, nc.gpsimd, nc.tensor

        def sb(name, shape, dt):
            return es.enter_context(nc.sbuf_tensor(name, shape, dt))

        wscr_b = Buf("wscr")
        sem_pro = c.sem("pro")
        sem_const = c.sem("const")
        sem_out = c.sem("outst")
        sem_x = c.sem("xld")
        sem_misc = c.sem("misc")

        ident_f = sb("ident_f", [128, 128], F32)
        ident = sb("ident", [128, 128], BF16)
        antiJ = sb("antiJ", [128, 128], BF16)
        I8big = sb("I8big", [128, 4, 128], BF16)
        ones = sb("ones", [128, 128], BF16)
        ones2 = sb("ones2", [128, 128], BF16)
        epsc = sb("epsc", [128, 1], F32)
        gcol = sb("gcol", [128, 4, KC], F32)
        gfin = sb("gfin", [128, D], F32)
        hg = sb("hg", [128, 4], F32)
        biasT = sb("biasT", [128, 2, 8, 128], BF16)
        cst_b = Buf("const")
        cosT = sb("cosT", [128, 8, 128], F32); sinT = sb("sinT", [128, 8, 128], F32)
        BpT = sb("BpT", [128, 2, 8, 128], BF16)
        Cp = sb("Cp", [128, 2, 8, 128], BF16)
        ssmc = sb("ssmc", [128, 12, 8], F32)
        dcol = sb("dcol", [128, 2], F32)

        banks = [es.enter_context(nc.psum_tensor("bank%d" % i, [128, 512], F32)) for i in range(7)]
        bank_b = [Buf("bank%d" % i) for i in range(7)]
        psT = es.enter_context(nc.psum_tensor("psT", [128, 1024], BF16)); psT_b = Buf("psT")
        pinned = set()
        rr = [0]

        def nb():
            while True:
                i = rr[0] % 7
                rr[0] += 1
                if i not in pinned:
                    return i

        def pin():
            i = nb()
            pinned.add(i)
            return i

        def unpin(i):
            pinned.discard(i)

        def mm_group(out_ap, pairs, reads, writes, first=True, last=True):
            def fn():
                ins = None
                n = len(pairs)
                for i, (l, r) in enumerate(pairs):
                    ins = T.matmul(out_ap, l, r, start=(first and i == 0), stop=(last and i == n - 1))
                return ins
            return c.op(PE, fn, reads=reads, writes=writes)

        def dump(name, ap_sb, buf, dst):
            if name in dbg_d:
                c.dma(POOL, dst, ap_sb, sem_misc, reads=[buf])

        def cdma(out, in_, **kw):
            c.dma(SP, out, in_, sem_const, writes=[cst_b], **kw)

        for gi, nm in enumerate(["ffn1_norm", "mix_norm", "ffn2_norm", "mem_norm"]):
            cdma(gcol[:, gi, :], W[nm].rearrange("o (c p) -> p (o c)", p=128), allow_slow_non_contiguous=True)
        cdma(gfin[:], W["final_norm"][0:1, :].partition_broadcast(128))
        for gi, nm in enumerate(["a_q_gain", "a_k_gain", "m_q_gain", "m_k_gain"]):
            cdma(hg[0:64, gi:gi + 1], W[nm][:, :])
            cdma(hg[64:128, gi:gi + 1], W[nm][:, :])
        es3 = ExitStack()
        sb_main = sb

        def sb(name, shape, dt):
            return es3.enter_context(nc.sbuf_tensor(name, shape, dt))
        rb_sb = sb("rb_sb", [32, 8], F32); oh_sb = sb("oh_sb", [32, 384], F32); rb15 = sb("rb15", [8, 1], F32)
        fsb = sb("fsb", [8, 384], F32); hank = sb("hank", [128, 2, 8, 128], F32); hankb = sb("hankb", [128, 2, 8, 128], BF16)
        cdma(rb_sb[:], W["rel_bias"][:, :])
        cdma(oh_sb[:], oh_d[:, :])
        cdma(rb15[:], W["rel_bias"][15:16, :].rearrange("o h -> h o"), allow_slow_non_contiguous=True)
        for t in range(2):
            cdma(ssmc[t * 64:(t + 1) * 64, 0, :], W["ssm_lambda_re"].rearrange("(c t) s -> t s c", t=2)[t], allow_slow_non_contiguous=True)
            cdma(ssmc[t * 64:(t + 1) * 64, 1, :], W["ssm_lambda_im"].rearrange("(c t) s -> t s c", t=2)[t], allow_slow_non_contiguous=True)
            cdma(ssmc[t * 64:(t + 1) * 64, 2, :], W["ssm_log_dt"].rearrange("o (c t) -> t o c", t=2)[t].partition_broadcast(64), allow_slow_non_contiguous=True)
        braw = sb("braw", [128, 2, 8, 16], F32); craw = sb("craw", [128, 2, 8, 16], F32)
        for t in range(2):
            cdma(braw[t * 64:(t + 1) * 64, 0, :, :], W["ssm_b_re"].rearrange("(c t) s h -> t s c h", t=2)[t])
            cdma(braw[t * 64:(t + 1) * 64, 1, :, :], W["ssm_b_im"].rearrange("(c t) s h -> t s c h", t=2)[t])
            for cc in range(8):
                cdma(craw[t * 64:(t + 1) * 64, 0, cc, :], W["ssm_c_re"].rearrange("(c t) o s -> t c s o", t=2)[t, cc], allow_slow_non_contiguous=True)
                cdma(craw[t * 64:(t + 1) * 64, 1, cc, :], W["ssm_c_im"].rearrange("(c t) o s -> t c s o", t=2)[t, cc], allow_slow_non_contiguous=True)
        cdma(dcol[:, :], W["ssm_d"].rearrange("(m p) o -> p (m o)", p=128), allow_slow_non_contiguous=True)

        def cop(E, fn):
            return c.op(E, fn, reads=[cst_b], writes=[cst_b])

        cop(POOL, lambda: G.memset(ident_f[:], 1.0))
        cop(POOL, lambda: G.affine_select(out=ident_f[:], in_=ident_f[:], pattern=[[-1, 128]], compare_op=ALU.is_equal,
                                          fill=0.0, base=0, channel_multiplier=1))
        cop(DVE, lambda: V.tensor_copy(ident[:], ident_f[:]))
        for i in range(4):
            cop(DVE, lambda i=i: V.tensor_scalar(out=I8big[:, i, :], in0=ident_f[:], scalar1=NEG, scalar2=None, op0=ALU.mult))
        cop(POOL, lambda: G.memset(ident_f[:], 1.0))
        cop(POOL, lambda: G.affine_select(out=ident_f[:], in_=ident_f[:], pattern=[[1, 128]], compare_op=ALU.is_equal,
                                          fill=0.0, base=-127, channel_multiplier=1))
        cop(DVE, lambda: V.tensor_copy(antiJ[:], ident_f[:]))
        cop(DVE, lambda: V.memset(ones[:], 1.0))
        cop(DVE, lambda: V.memset(ones2[:], 0.0))
        cop(DVE, lambda: V.memset(ones2[0:64, 0:64], 1.0))
        cop(DVE, lambda: V.memset(ones2[64:128, 64:128], 1.0))
        cop(DVE, lambda: V.memset(epsc[:], EPS))
        cop(DVE, lambda: V.tensor_scalar(out=hg[:, 0:1], in0=hg[:, 0:1], scalar1=0.125, scalar2=None, op0=ALU.mult))
        cop(DVE, lambda: V.tensor_scalar(out=hg[:, 2:3], in0=hg[:, 2:3], scalar1=0.125, scalar2=None, op0=ALU.mult))
        b0 = 0
        c.op(PE, lambda: T.matmul(banks[b0][0:8, 0:384], rb_sb[:, :], oh_sb[:, :], start=True, stop=True),
             reads=[cst_b], writes=[bank_b[b0]])
        c.op(DVE, lambda: V.tensor_scalar(out=fsb[:], in0=banks[b0][0:8, 0:384], scalar1=rb15[:, 0:1], scalar2=None,
                                          op0=ALU.subtract), reads=[bank_b[b0], cst_b], writes=[cst_b])
        fd_b = Buf("fd")
        c.dma(SP, fd_d[:, :], fsb[:], sem_const, reads=[cst_b], writes=[fd_b])
        for di, delta in enumerate((0, 128)):
            src = bass.AP(tensor=fd_d.tensor, offset=128 - delta, ap=[[1, 128], [384, 8], [1, 128]])
            c.dma(SP, hank[:, di, :, :], src, sem_const, reads=[fd_b], writes=[cst_b])
        cop(DVE, lambda: V.tensor_copy(hankb[:], hank[:]))
        for di in range(2):
            for h in range(8):
                bi = nb()
                c.op(PE, lambda di=di, h=h, bi=bi: T.matmul(banks[bi][:, 0:128], hankb[:, di, h, :], antiJ[:], start=True, stop=True),
                     reads=[cst_b], writes=[bank_b[bi]])
                c.op(DVE, lambda di=di, h=h, bi=bi: V.tensor_copy(biasT[:, di, h, :], banks[bi][:, 0:128]),
                     reads=[bank_b[bi], cst_b], writes=[cst_b])

        LR, LI, LDT, DT, MAG, TH, COS, SIN, FR, FI, T1, T2 = range(12)
        sc = lambda i: ssmc[:, i, :]
        cop(ACT, lambda: A.activation(out=sc(DT), in_=sc(LDT), func=AF.Exp))
        cop(DVE, lambda: V.tensor_tensor(out=sc(T1), in0=sc(LR), in1=sc(DT), op=ALU.mult))
        cop(ACT, lambda: A.activation(out=sc(MAG), in_=sc(T1), func=AF.Exp))
        cop(DVE, lambda: V.tensor_tensor(out=sc(TH), in0=sc(LI), in1=sc(DT), op=ALU.mult))
        MAGIC = 12582912.0
        TWO_PI = 2.0 * math.pi

        def sin_of(out_ap, in_ap, tmp_ap, shift):
            cop(DVE, lambda: V.tensor_scalar(out=tmp_ap, in0=in_ap, scalar1=shift, scalar2=1.0 / TWO_PI, op0=ALU.add, op1=ALU.mult))
            cop(DVE, lambda: V.tensor_scalar(out=tmp_ap, in0=tmp_ap, scalar1=MAGIC, scalar2=None, op0=ALU.add))
            cop(DVE, lambda: V.tensor_scalar(out=tmp_ap, in0=tmp_ap, scalar1=-MAGIC, scalar2=-TWO_PI, op0=ALU.add, op1=ALU.mult))
            cop(DVE, lambda: V.scalar_tensor_tensor(out=tmp_ap, in0=in_ap, scalar=shift, in1=tmp_ap, op0=ALU.add, op1=ALU.add))
            cop(DVE, lambda: V.tensor_scalar(out=tmp_ap, in0=tmp_ap, scalar1=math.pi, scalar2=-math.pi, op0=ALU.min, op1=ALU.max))
            cop(ACT, lambda: A.activation(out=out_ap, in_=tmp_ap, func=AF.Sin))

        sin_of(sc(SIN), sc(TH), sc(T1), 0.0)
        sin_of(sc(COS), sc(TH), sc(T1), math.pi / 2)
        arr = sb("arr", [128, 6, 8], F32)
        a_ = lambda i: arr[:, i, :]
        cop(DVE, lambda: V.tensor_tensor(out=a_(0), in0=sc(MAG), in1=sc(COS), op=ALU.mult))
        cop(DVE, lambda: V.tensor_scalar(out=a_(0), in0=a_(0), scalar1=-1.0, scalar2=None, op0=ALU.add))
        cop(DVE, lambda: V.tensor_tensor(out=a_(1), in0=sc(MAG), in1=sc(SIN), op=ALU.mult))
        cop(DVE, lambda: V.tensor_tensor(out=a_(2), in0=sc(LR), in1=sc(LR), op=ALU.mult))
        cop(DVE, lambda: V.tensor_tensor(out=a_(3), in0=sc(LI), in1=sc(LI), op=ALU.mult))
        cop(DVE, lambda: V.tensor_tensor(out=a_(2), in0=a_(2), in1=a_(3), op=ALU.add))
        cop(DVE, lambda: V.reciprocal(out=a_(5), in_=a_(2)))
        cop(DVE, lambda: V.tensor_tensor(out=a_(3), in0=a_(0), in1=sc(LR), op=ALU.mult))
        cop(DVE, lambda: V.tensor_tensor(out=a_(4), in0=a_(1), in1=sc(LI), op=ALU.mult))
        cop(DVE, lambda: V.tensor_tensor(out=a_(3), in0=a_(3), in1=a_(4), op=ALU.add))
        cop(DVE, lambda: V.tensor_tensor(out=sc(FR), in0=a_(3), in1=a_(5), op=ALU.mult))
        cop(DVE, lambda: V.tensor_tensor(out=a_(3), in0=a_(1), in1=sc(LR), op=ALU.mult))
        cop(DVE, lambda: V.tensor_tensor(out=a_(4), in0=a_(0), in1=sc(LI), op=ALU.mult))
        cop(DVE, lambda: V.tensor_tensor(out=a_(3), in0=a_(3), in1=a_(4), op=ALU.subtract))
        cop(DVE, lambda: V.tensor_tensor(out=sc(FI), in0=a_(3), in1=a_(5), op=ALU.mult))
        bbar = sb("bbar", [128, 2, 8, 16], F32); btmp = sb("btmp", [128, 8, 16], F32)
        frb = ssmc[:, FR, :].unsqueeze(2).to_broadcast([128, 8, 16])
        fib = ssmc[:, FI, :].unsqueeze(2).to_broadcast([128, 8, 16])
        cop(DVE, lambda: V.tensor_tensor(out=bbar[:, 0, :, :], in0=braw[:, 0, :, :], in1=frb, op=ALU.mult))
        cop(DVE, lambda: V.tensor_tensor(out=btmp[:], in0=braw[:, 1, :, :], in1=fib, op=ALU.mult))
        cop(DVE, lambda: V.tensor_tensor(out=bbar[:, 0, :, :], in0=bbar[:, 0, :, :], in1=btmp[:], op=ALU.subtract))
        cop(DVE, lambda: V.tensor_tensor(out=bbar[:, 1, :, :], in0=braw[:, 1, :, :], in1=frb, op=ALU.mult))
        cop(DVE, lambda: V.tensor_tensor(out=btmp[:], in0=braw[:, 0, :, :], in1=fib, op=ALU.mult))
        cop(DVE, lambda: V.tensor_tensor(out=bbar[:, 1, :, :], in0=bbar[:, 1, :, :], in1=btmp[:], op=ALU.add))
        xpad = sb("xpad", [128, 128], BF16)
        for ri in range(2):
            for cc in range(8):
                cop(DVE, lambda: V.memset(xpad[:], 0.0))
                for t in range(2):
                    col = 32 * (cc % 4) + 16 * t
                    cop(DVE, lambda ri=ri, cc=cc, t=t, col=col: V.tensor_copy(xpad[t * 64:(t + 1) * 64, col:col + 16],
                                                                               bbar[t * 64:(t + 1) * 64, ri, cc, :]))
                c.op(PE, lambda: T.transpose(psT[:, 0:128], xpad[:], ident[:]), reads=[cst_b], writes=[psT_b])
                c.op(DVE, lambda ri=ri, cc=cc: V.tensor_copy(BpT[:, ri, cc, :], psT[:, 0:128]), reads=[psT_b, cst_b], writes=[cst_b])
        cop(DVE, lambda: V.memset(Cp[:], 0.0))
        for ri in range(2):
            for cc in range(8):
                for t in range(2):
                    col = 16 * ((2 * cc + t) % 8)
                    cop(DVE, lambda ri=ri, cc=cc, t=t, col=col: V.tensor_scalar(
                        out=Cp[t * 64:(t + 1) * 64, ri, cc, col:col + 16], in0=craw[t * 64:(t + 1) * 64, ri, cc, :],
                        scalar1=(1.0 if ri == 0 else -1.0), scalar2=None, op0=ALU.mult))
        tau_i = sb("tau_i", [128, 128], I32); tau = sb("tau", [128, 128], F32); ang = sb("ang", [128, 8, 128], F32)
        atmp = sb("atmp", [128, 8, 128], F32)
        cop(POOL, lambda: G.iota(tau_i[:], pattern=[[1, 128]], base=0, channel_multiplier=0))
        cop(DVE, lambda: V.tensor_copy(tau[:], tau_i[:]))
        for cc in range(8):
            cop(DVE, lambda cc=cc: V.tensor_scalar(out=ang[:, cc, :], in0=tau[:], scalar1=ssmc[:, TH, cc:cc + 1], scalar2=None, op0=ALU.mult))
        sin_of(sinT[:], ang[:], atmp[:], 0.0)
        sin_of(cosT[:], ang[:], atmp[:], math.pi / 2)
        NSTG = 4
        with ExitStack() as es2:
            stg32 = [es2.enter_context(nc.sbuf_tensor("stg32_%d" % i, [128, SLOT], F32)) for i in range(NSTG)]
            stg16 = [es2.enter_context(nc.sbuf_tensor("stg16_%d" % i, [128, SLOT], BF16)) for i in range(NSTG)]
            b32 = [Buf("s32_%d" % i) for i in range(NSTG)]
            b16 = [Buf("s16_%d" % i) for i in range(NSTG)]
            s32sem = [c.sem("s32_%d" % i) for i in range(NSTG)]
            for i in range(NSTG):
                c.op(POOL, lambda i=i: G.memset(stg32[i][:], 0.0), writes=[b32[i]])
            for k, (key, srcs) in enumerate(pieces):
                j = k % NSTG
                n = 0
                for (p0, p1, off, shp, src) in srcs:
                    sz = shp[0] * shp[1]
                    dst = stg32[j][p0:p1, off:off + sz].rearrange("p (a b) -> p a b", a=shp[0])
                    c.dma(SP, dst, src, s32sem[j], writes=[b32[j]])
                    n = max(n, off + sz)
                E = (ACT, DVE)[k % 2]
                if E is ACT:
                    c.op(ACT, lambda j=j, n=n: A.copy(out=stg16[j][:, 0:n], in_=stg32[j][:, 0:n]),
                         reads=[b32[j]], writes=[b16[j]])
                elif E is DVE:
                    c.op(DVE, lambda j=j, n=n: V.tensor_copy(stg16[j][:, 0:n], stg32[j][:, 0:n]),
                         reads=[b32[j]], writes=[b16[j]])
                else:
                    c.op(POOL, lambda j=j, n=n: G.tensor_copy(stg16[j][:, 0:n], stg32[j][:, 0:n]),
                         reads=[b32[j]], writes=[b16[j]])
                c.dma(POOL, wscr[k, :, 0:n], stg16[j][:, 0:n], sem_pro, reads=[b16[j]], writes=[wscr_b])
            c.barrier(b32 + b16)

        c.barrier([cst_b])
        es3.close()
        sb = sb_main
        ring = [sb("ring%d" % i, [128, SLOT], BF16) for i in range(3)]
        ring_b = [Buf("ring%d" % i) for i in range(3)]
        ring_s = [c.sem("ring%d" % i) for i in range(3)]
        ring_ctr = [0]

        def piece(key):
            k = pidx[key]
            j = ring_ctr[0] % 3
            ring_ctr[0] += 1
            n = max(off + shp[0] * shp[1] for (_, _, off, shp, _) in pieces[k][1])
            c.dma(SP, ring[j][:, 0:n], wscr[k, :, 0:n], ring_s[j], reads=[wscr_b], writes=[ring_b[j]])
            return ring[j], ring_b[j]

        xt = sb("xt", [128, 4, D], F32); xt_b = [Buf("xt%d" % i) for i in range(4)]
        hT = sb("hT", [128, KC, 512], BF16); hT_b = Buf("hT")
        ss = sb("ss", [128, 8], F32); ss_b = Buf("ss")
        arena = sb("arena", [128, max(4 * S, FC * 256)], F32)
        actT = arena[:, 0:FC * 256].bitcast(BF16).rearrange("p (f n) -> p f n", f=FC); actT_b = Buf("actT")
        kT = sb("kT", [128, S], BF16); kT_b = Buf("kT")
        kiT = sb("kiT", [64, S], BF16); kiT_b = Buf("kiT")
        Vaug = sb("Vaug", [128, S // 128, 128], BF16); Vaug_b = Buf("Vaug")
        qaT = sb("qaT", [128, 4, 512], BF16); qaT_b = Buf("qaT")
        qiT = sb("qiT", [64, 4, 512], BF16); qiT_b = Buf("qiT")
        qmT = sb("qmT", [64, 4, 512], BF16); qmT_b = Buf("qmT")
        uT = sb("uT", [128, 2, 512], BF16); uT_b = Buf("uT")
        wq = sb("wq", [128, 4, 4], F32); wq_b = Buf("wq")
        Isc = [arena[:, i * S:(i + 1) * S] for i in range(4)]; Isc_b = [Buf("Isc%d" % i) for i in range(4)]
        MB = [sb("MB%d" % i, [128, S], BF16) for i in range(2)]; MB_b = [Buf("MB0"), Buf("MB1")]
        junkA = sb("junkA", [128, S], mybir.dt.float8e4); junkA_b = Buf("junkA")
        ET = [sb("ET%d" % i, [128, 512], BF16) for i in range(4)]; ET_b = [Buf("ET%d" % i) for i in range(4)]
        rec = sb("rec", [128, 512], F32); rec_b = Buf("rec")
        OaT = sb("OaT", [64, 8, 512], BF16); OaT_b = Buf("OaT")
        OmT = sb("OmT", [64, 4, 512], BF16); OmT_b = Buf("OmT")
        ysT = sb("ysT", [128, 2, 512], BF16); ysT_b = Buf("ysT")
        mgT = sb("mgT", [128, 8, 512], BF16); mgT_b = Buf("mgT")
        junk = mgT[:].rearrange("p f n -> p (f n)"); junk_b = mgT_b
        mkT = sb("mkT", [64, 4, 256], BF16); mkT_b = Buf("mkT")
        Vm = sb("Vm", [128, 2, 4, 128], BF16); Vm_b = Buf("Vm")
        memT = hT[:, :, 0:256]; memT_b = hT_b
        blo = sb("blo", [128, 4], F32); bw = sb("bw", [128, 4], F32); bmid = sb("bmid", [128, 4], F32)
        bcnt = sb("bcnt", [128, 4], F32); bpw = sb("bpw", [128, 4], F32); bmx = sb("bmx", [128, 4], F32)
        blo_b, bw_b, bmid_b, bcnt_b, bpw_b, bmx_b = (Buf(n) for n in ("blo", "bw", "bmid", "bcnt", "bpw", "bmx"))
        bthr = sb("bthr", [128, 4], F32); bthr_b = Buf("bthr")
        bcnt_c = [Buf("bcnt%d" % i) for i in range(4)]
        s_init = sb("s_init", [128, 2, 8], F32); s_init_b = Buf("s_init")
        stmp = [sb("stmp%d" % i, [128, 512], F32) for i in range(7)]; stmp_b = [Buf("stmp%d" % i) for i in range(7)]
        xrb = sb("xrb", [128, 2, 2, 512], BF16); xrb_b = [Buf("xrb0"), Buf("xrb1")]
        ssm_small = sb("ssm_small", [128, 4, 4], F32); ssm_small_b = Buf("ssm_small")
        yf = sb("yf", [128, 128], F32); yf_b = Buf("yf")
        sg = [stmp[3], stmp[4]]; sg_b = [stmp_b[3], stmp_b[4]]
        rl = [stmp[3], stmp[4]]; rl_b = [stmp_b[3], stmp_b[4]]
        hrs = stmp[0][:, :]; hrs_b = stmp_b[0]
        hsq = stmp[1][:, 0:256].bitcast(BF16); hsq_b = stmp_b[1]
        xs = stmp[2][:, :].bitcast(BF16); xs_b = stmp_b[2]
        c.op(DVE, lambda: V.memset(Vaug[:], 1.0), writes=[Vaug_b])
        c.op(DVE, lambda: V.memset(Vm[:], 1.0), writes=[Vm_b])

        def rms_to_hT(x_ap_fn, gi, dst, dst_b, ntile, src_b):
            for t in range(ntile):
                xa = x_ap_fn(t)
                c.op(ACT, lambda xa=xa, t=t: A.activation(out=junk[:, 0:D], in_=xa, func=AF.Square, accum_out=ss[:, t:t + 1]),
                     reads=[src_b[t]], writes=[junk_b, ss_b])
                c.op(ACT, lambda t=t: A.activation(out=ss[:, 4 + t:5 + t], in_=ss[:, t:t + 1], func=AF.Ln, bias=epsc[:, 0:1], scale=1.0 / D),
                     reads=[ss_b], writes=[ss_b])
                c.op(ACT, lambda t=t: A.activation(out=ss[:, 4 + t:5 + t], in_=ss[:, 4 + t:5 + t], func=AF.Exp, scale=-0.5), reads=[ss_b], writes=[ss_b])
                c.op(DVE, lambda xa=xa, t=t: V.tensor_scalar(out=xs[:], in0=xa, scalar1=ss[:, 4 + t:5 + t], scalar2=None, op0=ALU.mult),
                     reads=[src_b[t], ss_b], writes=[xs_b])

                def tr():
                    ins = None
                    for kc in range(KC):
                        ins = T.transpose(psT[:, kc * 128:(kc + 1) * 128], xs[:, kc * 128:(kc + 1) * 128], ident[:])
                    return ins
                c.op(PE, tr, reads=[xs_b], writes=[psT_b])
                c.op(DVE, lambda t=t: V.tensor_tensor(out=dst[:, :, t * 128:(t + 1) * 128],
                                                      in0=psT[:].rearrange("p (c n) -> p c n", c=KC),
                                                      in1=gcol[:, gi, :].unsqueeze(2).to_broadcast([128, KC, 128]), op=ALU.mult),
                     reads=[psT_b], writes=[dst_b])

        def head_norm_a(bi, N, P):
            c.op(ACT, lambda: A.activation(out=hsq[0:P, 0:N], in_=banks[bi][0:P, 0:N], func=AF.Square), reads=[bank_b[bi]], writes=[hsq_b])

        def head_norm_b(bi, N, gidx, out_ap, out_b, P):
            b2 = nb()
            on = ones[0:64, 0:64] if P == 64 else ones2[:, :]
            mm_group(banks[b2][0:P, 0:N], [(on, hsq[0:P, 0:N])], reads=[hsq_b], writes=[bank_b[b2]])
            c.op(ACT, lambda: A.activation(out=hrs[0:P, 0:N], in_=banks[b2][0:P, 0:N], func=AF.Ln, bias=epsc[0:P, 0:1], scale=1.0 / 64),
                 reads=[bank_b[b2]], writes=[hrs_b])
            c.op(ACT, lambda: A.activation(out=hrs[0:P, 0:N], in_=hrs[0:P, 0:N], func=AF.Exp, scale=-0.5), reads=[hrs_b], writes=[hrs_b])
            c.op(DVE, lambda: V.scalar_tensor_tensor(out=out_ap, in0=banks[bi][0:P, 0:N], scalar=hg[0:P, gidx:gidx + 1], in1=hrs[0:P, 0:N],
                                                     op0=ALU.mult, op1=ALU.mult), reads=[bank_b[bi], hrs_b], writes=[out_b])

        def head_pipeline(tasks):
            pend = None
            for (grp, N, gidx, out_ap, out_b, P) in tasks:
                bi = grp()
                if pend is not None:
                    head_norm_b(*pend)
                head_norm_a(bi, N, P)
                pend = (bi, N, gidx, out_ap, out_b, P)
            if pend is not None:
                head_norm_b(*pend)

        def ffn(tag, gi):
            c.barrier(Isc_b)
            rms_to_hT(lambda t: xt[:, t, :], gi, hT, hT_b, 4, xt_b)
            for i in range(11):
                wt, wb = piece((tag, "in", i))
                w4 = wt[:, 0:4096].rearrange("p (gu kc f) -> p gu kc f", gu=2, kc=8)
                for j in range(2):
                    f = 2 * i + j
                    bg, bu = nb(), nb()
                    mm_group(banks[bg][:, :], [(w4[:, 0, kc, j * 128:(j + 1) * 128], hT[:, kc, :]) for kc in range(KC)],
                             reads=[wb, hT_b], writes=[bank_b[bg]])
                    mm_group(banks[bu][:, :], [(w4[:, 1, kc, j * 128:(j + 1) * 128], hT[:, kc, :]) for kc in range(KC)],
                             reads=[wb, hT_b], writes=[bank_b[bu]])
                    si = f % 2
                    c.op(ACT, lambda bg=bg, si=si: A.activation(out=sg[si][:], in_=banks[bg][:, :], func=AF.Silu),
                         reads=[bank_b[bg]], writes=[sg_b[si]])
                    c.op(DVE, lambda bu=bu, si=si, f=f: V.tensor_tensor(out=actT[:, f, :], in0=sg[si][:], in1=banks[bu][:, :], op=ALU.mult),
                         reads=[sg_b[si], bank_b[bu]], writes=[actT_b])
            for hh in range(2):
                acc = [pin() for _ in range(4)]
                for pp in range(2):
                    wt, wb = piece((tag, "out", hh, pp))
                    w3 = wt[:, 0:5632].rearrange("p (f d) -> p f d", f=11)
                    for t in range(4):
                        mm_group(banks[acc[t]][:, :], [(actT[:, 11 * pp + fl, t * 128:(t + 1) * 128], w3[:, fl, :]) for fl in range(11)],
                                 reads=[wb, actT_b], writes=[bank_b[acc[t]]], first=(pp == 0), last=(pp == 1))
                for t in range(4):
                    xsl = xt[:, t, hh * 512:(hh + 1) * 512]
                    c.op(DVE, lambda t=t, xsl=xsl: V.scalar_tensor_tensor(out=xsl, in0=banks[acc[t]][:, :], scalar=0.5, in1=xsl,
                                                                          op0=ALU.mult, op1=ALU.add),
                         reads=[bank_b[acc[t]], xt_b[t]], writes=[xt_b[t]])
                    unpin(acc[t])

        for b in range(NB):
            for mc in range(2):
                c.dma(SP, xt[:, mc, :], mem_d[b * 256 + mc * 128: b * 256 + (mc + 1) * 128, :], sem_x, writes=[xt_b[mc]])
            rms_to_hT(lambda t: xt[:, t, :], 3, memT, memT_b, 2, xt_b)
            wt, wb = piece(("kv",))
            wk3 = wt[:, 0:4096].rearrange("p (kc d) -> p kc d", kc=8)
            def mk_grp(h):
                def g():
                    bi = nb()
                    mm_group(banks[bi][0:64, 0:256], [(wk3[:, kc, h * 64:(h + 1) * 64], memT[:, kc, :]) for kc in range(KC)],
                             reads=[wb, memT_b], writes=[bank_b[bi]])
                    return bi
                return g
            head_pipeline([(mk_grp(h), 256, 3, mkT[:, h, :], mkT_b, 64) for h in range(4)])
            for mc in range(2):
                bi = nb()
                mm_group(banks[bi][:, 0:256], [(memT[:, kc, mc * 128:(mc + 1) * 128], wk3[:, kc, 256:512]) for kc in range(KC)],
                         reads=[wb, memT_b], writes=[bank_b[bi]])
                c.op(DVE, lambda mc=mc, bi=bi: V.tensor_copy(Vm[:, mc, :, 0:64], banks[bi][:, 0:256].rearrange("p (h d) -> p h d", h=4)),
                     reads=[bank_b[bi]], writes=[Vm_b])
            c.op(DVE, lambda: V.memset(s_init[:], 0.0), writes=[s_init_b])

            for st in range(NT):
                row0 = b * S + st * 512
                for t in range(4):
                    c.dma(SP, xt[:, t, :], x_d[row0 + t * 128:row0 + (t + 1) * 128, :], sem_x, writes=[xt_b[t]])
                ffn("f1", 0)
                if "x1" in dbg_d and b == 0:
                    for t in range(4):
                        dump("x1", xt[:, t, :], xt_b[t], dbg_d["x1"][st * 512 + t * 128:st * 512 + (t + 1) * 128, :])
                rms_to_hT(lambda t: xt[:, t, :], 1, hT, hT_b, 4, xt_b)
                def emit_mask(t):
                    L = Ls[t]
                    m = t % 2
                    c.op(DVE, lambda: V.tensor_scalar(out=MB[m][:, 0:L], in0=Isc[t][:, 0:L], scalar1=blo[:, t:t + 1], scalar2=1.0,
                                                      op0=ALU.is_gt, op1=ALU.subtract), reads=[Isc_b[t], blo_b], writes=[MB_b[m]])

                def attention(t):
                    j = st * 4 + t
                    m = t % 2
                    O = [pin(), pin()]
                    LOOK = 1
                    sb_ = {}
                    for i in range(j + 1 + LOOK):
                        if i <= j:
                            kc = i
                            bA, bB = nb(), nb()
                            sb_[kc] = (bA, bB)
                            ks = slice(kc * 128, (kc + 1) * 128)
                            tq = slice(t * 128, (t + 1) * 128)
                            nb_ = (kc >= j - 1)

                            def fn(bA=bA, bB=bB, ks=ks, tq=tq, nb_=nb_, kc=kc):
                                T.matmul(banks[bA][:, :], kT[0:64, ks], qaT[0:64, :, tq], start=True, stop=False)
                                T.matmul(banks[bB][:, :], kT[64:128, ks], qaT[64:128, :, tq], start=True, stop=False)
                                T.matmul(banks[bA][:, :], MB[m][:, ks], I8big[:, :, :], start=False, stop=not nb_)
                                ins = T.matmul(banks[bB][:, :], MB[m][:, ks], I8big[:, :, :], start=False, stop=not nb_)
                                if nb_:
                                    T.matmul(banks[bA][:, :], ident[:], biasT[:, j - kc, 0:4, :], start=False, stop=True)
                                    ins = T.matmul(banks[bB][:, :], ident[:], biasT[:, j - kc, 4:8, :], start=False, stop=True)
                                return ins
                            c.op(PE, fn, reads=[kT_b, qaT_b, MB_b[m]], writes=[bank_b[bA], bank_b[bB]])
                            for g, bi in ((0, bA), (1, bB)):
                                e = (2 * i + g) % 4
                                c.op(ACT, lambda bi=bi, e=e: A.activation(out=ET[e][:], in_=banks[bi][:, :], func=AF.Exp), reads=[bank_b[bi]], writes=[ET_b[e]])
                        if i >= LOOK:
                            kc = i - LOOK
                            for g in range(2):
                                e = (2 * kc + g) % 4
                                mm_group(banks[O[g]][:, :], [(Vaug[:, kc, :], ET[e][:])], reads=[Vaug_b, ET_b[e]], writes=[bank_b[O[g]]],
                                         first=(kc == 0), last=(kc == j))
                    for g in range(2):
                        c.op(ACT, lambda g=g: A.activation(out=rec[64:128, :], in_=banks[O[g]][64:128, :], func=AF.Ln), reads=[bank_b[O[g]]], writes=[rec_b])
                        c.op(ACT, lambda: A.activation(out=rec[64:128, :], in_=rec[64:128, :], func=AF.Exp, scale=-1.0), reads=[rec_b], writes=[rec_b])
                        c.op(DVE, lambda g=g: V.tensor_tensor(out=OaT[:, 4 * g:4 * g + 4, t * 128:(t + 1) * 128],
                                                             in0=banks[O[g]][0:64, :].rearrange("p (h q) -> p h q", h=4),
                                                             in1=rec[64:128, :].rearrange("p (h q) -> p h q", h=4), op=ALU.mult),
                             reads=[bank_b[O[g]], rec_b], writes=[OaT_b])
                        unpin(O[g])

                ssm_banks = {}

                def ssm_B(sub):
                    tk = slice(sub * 128, (sub + 1) * 128)
                    for hh in range(2):
                        pr_, pi_ = pin(), pin()
                        ssm_banks[(sub, hh)] = (pr_, pi_)
                        for ri, bi in ((0, pr_), (1, pi_)):
                            def fn(ri=ri, bi=bi, hh=hh):
                                ins = None
                                for cl in range(4):
                                    ins = T.matmul(banks[bi][:, cl * 128:(cl + 1) * 128], BpT[:, ri, 4 * hh + cl, :], uT[:, hh, tk], start=True, stop=True)
                                return ins
                            c.op(PE, fn, reads=[uT_b], writes=[bank_b[bi]])

                def ssm_dve(sub):
                    s1, s3, s4 = stmp[2], stmp[3], stmp[4]
                    z1, z3, z4 = stmp_b[2], stmp_b[3], stmp_b[4]
                    Wb = [(stmp[0], stmp[1], stmp_b[0], stmp_b[1]), (stmp[5], stmp[6], stmp_b[5], stmp_b[6])]
                    for hh in range(2):
                        pr_, pi_ = ssm_banks[(sub, hh)]
                        cs = cosT[:, 4 * hh:4 * hh + 4, :].rearrange("p c n -> p (c n)")
                        sn = sinT[:, 4 * hh:4 * hh + 4, :].rearrange("p c n -> p (c n)")
                        s0, s2, z0, z2 = Wb[hh]
                        PR, PI = banks[pr_][:, :], banks[pi_][:, :]
                        c.op(DVE, lambda: V.tensor_tensor(out=s0[:], in0=PR, in1=cs, op=ALU.mult), reads=[bank_b[pr_]], writes=[z0])
                        c.op(DVE, lambda: V.tensor_tensor(out=s1[:], in0=PI, in1=sn, op=ALU.mult), reads=[bank_b[pi_]], writes=[z1])
                        c.op(DVE, lambda: V.tensor_tensor(out=s0[:], in0=s0[:], in1=s1[:], op=ALU.add), reads=[z0, z1], writes=[z0])
                        c.op(DVE, lambda: V.tensor_tensor(out=s2[:], in0=PI, in1=cs, op=ALU.mult), reads=[bank_b[pi_]], writes=[z2])
                        c.op(DVE, lambda: V.tensor_tensor(out=s1[:], in0=PR, in1=sn, op=ALU.mult), reads=[bank_b[pr_], z1], writes=[z1])
                        c.op(DVE, lambda: V.tensor_tensor(out=s2[:], in0=s2[:], in1=s1[:], op=ALU.subtract), reads=[z2, z1], writes=[z2])
                        unpin(pr_); unpin(pi_)
                    for hh in range(2):
                        cs = cosT[:, 4 * hh:4 * hh + 4, :].rearrange("p c n -> p (c n)")
                        sn = sinT[:, 4 * hh:4 * hh + 4, :].rearrange("p c n -> p (c n)")
                        s0, s2, z0, z2 = Wb[hh]
                        for cl in range(4):
                            cc = 4 * hh + cl
                            sl = slice(cl * 128, (cl + 1) * 128)
                            mgb = ssmc[:, MAG, cc:cc + 1].to_broadcast([128, 128])
                            c.op(DVE, lambda cc=cc, sl=sl, mgb=mgb: V.tensor_tensor_scan(out=s3[:, sl], data0=mgb, data1=s0[:, sl], initial=s_init[:, 0, cc:cc + 1],
                                                                                         op0=ALU.mult, op1=ALU.add), reads=[z0, s_init_b], writes=[z3])
                            c.op(DVE, lambda cc=cc, sl=sl, mgb=mgb: V.tensor_tensor_scan(out=s4[:, sl], data0=mgb, data1=s2[:, sl], initial=s_init[:, 1, cc:cc + 1],
                                                                                         op0=ALU.mult, op1=ALU.add), reads=[z2, s_init_b], writes=[z4])
                        q = ssm_small
                        lastc = lambda ap: ap.rearrange("p (c n) -> p c n", c=4)[:, :, 127]
                        c.op(DVE, lambda: V.tensor_tensor(out=s0[:], in0=s3[:], in1=cs, op=ALU.mult), reads=[z3, z0], writes=[z0])
                        c.op(DVE, lambda: V.tensor_tensor(out=s1[:], in0=s4[:], in1=sn, op=ALU.mult), reads=[z4, z1], writes=[z1])
                        c.op(DVE, lambda hh=hh: V.tensor_tensor(out=xrb[:, hh, 0, :], in0=s0[:], in1=s1[:], op=ALU.subtract), reads=[z0, z1], writes=[xrb_b[hh]])
                        c.op(DVE, lambda: V.tensor_tensor(out=q[:, 0, :], in0=lastc(s0[:]), in1=lastc(s1[:]), op=ALU.subtract), reads=[z0, z1], writes=[ssm_small_b])
                        c.op(DVE, lambda: V.tensor_tensor(out=s2[:], in0=s3[:], in1=sn, op=ALU.mult), reads=[z3, z2], writes=[z2])
                        c.op(DVE, lambda: V.tensor_tensor(out=s1[:], in0=s4[:], in1=cs, op=ALU.mult), reads=[z4, z1], writes=[z1])
                        c.op(DVE, lambda hh=hh: V.tensor_tensor(out=xrb[:, hh, 1, :], in0=s2[:], in1=s1[:], op=ALU.add), reads=[z2, z1], writes=[xrb_b[hh]])
                        c.op(DVE, lambda: V.tensor_tensor(out=q[:, 2, :], in0=lastc(s2[:]), in1=lastc(s1[:]), op=ALU.add), reads=[z2, z1], writes=[ssm_small_b])
                        cth = ssmc[:, COS, 4 * hh:4 * hh + 4]; sth = ssmc[:, SIN, 4 * hh:4 * hh + 4]
                        c.op(DVE, lambda: V.tensor_tensor(out=q[:, 1, :], in0=q[:, 2, :], in1=sth, op=ALU.mult), reads=[ssm_small_b], writes=[ssm_small_b])
                        c.op(DVE, lambda: V.tensor_tensor(out=q[:, 3, :], in0=q[:, 2, :], in1=cth, op=ALU.mult), reads=[ssm_small_b], writes=[ssm_small_b])
                        c.op(DVE, lambda: V.tensor_tensor(out=q[:, 2, :], in0=q[:, 0, :], in1=sth, op=ALU.mult), reads=[ssm_small_b], writes=[ssm_small_b])
                        c.op(DVE, lambda: V.tensor_tensor(out=q[:, 0, :], in0=q[:, 0, :], in1=cth, op=ALU.mult), reads=[ssm_small_b], writes=[ssm_small_b])
                        c.op(DVE, lambda hh=hh: V.tensor_tensor(out=s_init[:, 0, 4 * hh:4 * hh + 4], in0=q[:, 0, :], in1=q[:, 1, :], op=ALU.subtract),
                             reads=[ssm_small_b, s_init_b], writes=[s_init_b])
                        c.op(DVE, lambda hh=hh: V.tensor_tensor(out=s_init[:, 1, 4 * hh:4 * hh + 4], in0=q[:, 2, :], in1=q[:, 3, :], op=ALU.add),
                             reads=[ssm_small_b, s_init_b], writes=[s_init_b])

                def ssm_C(sub):
                    tk = slice(sub * 128, (sub + 1) * 128)
                    for hh in range(2):
                        by = nb()
                        prs = []
                        for cl in range(4):
                            prs.append((Cp[:, 0, 4 * hh + cl, :], xrb[:, hh, 0, cl * 128:(cl + 1) * 128]))
                            prs.append((Cp[:, 1, 4 * hh + cl, :], xrb[:, hh, 1, cl * 128:(cl + 1) * 128]))
                        mm_group(banks[by][:, 0:128], prs, reads=[xrb_b[hh]], writes=[bank_b[by]])
                        c.op(DVE, lambda by=by, hh=hh: V.scalar_tensor_tensor(out=yf[:], in0=uT[:, hh, tk], scalar=dcol[:, hh:hh + 1], in1=banks[by][:, 0:128],
                                                                              op0=ALU.mult, op1=ALU.add), reads=[uT_b, bank_b[by]], writes=[yf_b])
                        c.op(ACT, lambda hh=hh: A.activation(out=ysT[:, hh, tk], in_=yf[:], func=AF.Gelu), reads=[yf_b], writes=[ysT_b])

                def mem_attention():
                    steps = [(h, mc) for h in range(4) for mc in range(2)]
                    om = {}
                    LOOK = 2
                    for i in range(len(steps) + LOOK):
                        if i < len(steps):
                            h, mc = steps[i]
                            bi = nb()
                            mm_group(banks[bi][:, :], [(mkT[:, h, mc * 128:(mc + 1) * 128], qmT[:, h, :])], reads=[mkT_b, qmT_b], writes=[bank_b[bi]])
                            e = i % 3
                            c.op(ACT, lambda bi=bi, e=e: A.activation(out=ET[e][:], in_=banks[bi][:, :], func=AF.Exp), reads=[bank_b[bi]], writes=[ET_b[e]])
                        if i >= LOOK:
                            h, mc = steps[i - LOOK]
                            e = (i - LOOK) % 3
                            if mc == 0:
                                om[h] = pin()
                            mm_group(banks[om[h]][:, :], [(Vm[:, mc, h, :], ET[e][:])], reads=[Vm_b, ET_b[e]], writes=[bank_b[om[h]]], first=(mc == 0), last=(mc == 1))
                            if mc == 1:
                                o_ = om[h]
                                c.op(ACT, lambda o_=o_: A.activation(out=rec[64:128, :], in_=banks[o_][64:128, :], func=AF.Ln), reads=[bank_b[o_]], writes=[rec_b])
                                c.op(ACT, lambda: A.activation(out=rec[64:128, :], in_=rec[64:128, :], func=AF.Exp, scale=-1.0), reads=[rec_b], writes=[rec_b])
                                c.op(DVE, lambda o_=o_, h=h: V.tensor_tensor(out=OmT[:, h, :], in0=banks[o_][0:64, :], in1=rec[64:128, :], op=ALU.mult),
                                     reads=[bank_b[o_], rec_b], writes=[OmT_b])
                                unpin(o_)

                def hd_grp(w3, wb, c0):
                    def g():
                        bi = nb()
                        mm_group(banks[bi][0:64, :], [(w3[:, kc, c0:c0 + 64], hT[:, kc, :]) for kc in range(KC)],
                                 reads=[wb, hT_b], writes=[bank_b[bi]])
                        return bi
                    return g
                wt, wb = piece(("p2",))
                w3 = wt[:, 0:8 * 452].rearrange("p (kc d) -> p kc d", kc=8)
                def k_grp():
                    bi = nb()
                    for half in range(2):
                        mm_group(banks[bi][64 * half:64 * half + 64, :], [(w3[:, kc, 0:64], hT[:, kc, :]) for kc in range(KC)],
                                 reads=[wb, hT_b], writes=[bank_b[bi]])
                    return bi
                head_pipeline([(k_grp, 512, 1, kT[:, st * 512:(st + 1) * 512], kT_b, 128)])
                bi = nb()
                mm_group(banks[bi][0:64, :], [(w3[:, kc, 384:448], hT[:, kc, :]) for kc in range(KC)], reads=[wb, hT_b], writes=[bank_b[bi]])
                c.op(ACT, lambda bi=bi: A.copy(out=kiT[:, st * 512:(st + 1) * 512], in_=banks[bi][0:64, :]), reads=[bank_b[bi]], writes=[kiT_b])
                for h in range(4):
                    bi = nb()
                    mm_group(banks[bi][0:64, :], [(w3[:, kc, 128 + h * 64:192 + h * 64], hT[:, kc, :]) for kc in range(KC)],
                             reads=[wb, hT_b], writes=[bank_b[bi]])
                    c.op(ACT, lambda bi=bi, h=h: A.copy(out=qiT[:, h, :], in_=banks[bi][0:64, :]), reads=[bank_b[bi]], writes=[qiT_b])
                for t in range(4):
                    bi = nb()
                    mm_group(banks[bi][:, 0:64], [(hT[:, kc, t * 128:(t + 1) * 128], w3[:, kc, 64:128]) for kc in range(KC)],
                             reads=[wb, hT_b], writes=[bank_b[bi]])
                    mm_group(banks[bi][:, 64:68], [(hT[:, kc, t * 128:(t + 1) * 128], w3[:, kc, 448:452]) for kc in range(KC)],
                             reads=[wb, hT_b], writes=[bank_b[bi]])
                    c.op(DVE, lambda bi=bi, t=t: V.tensor_copy(Vaug[:, st * 4 + t, 0:64], banks[bi][:, 0:64]), reads=[bank_b[bi]], writes=[Vaug_b])
                    c.op(DVE, lambda bi=bi, t=t: V.tensor_scalar(out=wq[:, t, :], in0=banks[bi][:, 64:68], scalar1=0.0625, scalar2=None, op0=ALU.mult),
                         reads=[bank_b[bi]], writes=[wq_b])
                wt1, wb1 = piece(("p1",))
                w31 = wt1[:, 0:4096].rearrange("p (kc d) -> p kc d", kc=8)
                wt3, wb3 = piece(("p3",))
                w33 = wt3[:, 0:4096].rearrange("p (kc d) -> p kc d", kc=8)
                hp_pend = [None]

                def hp_push(task):
                    grp, N, gidx, out_ap, out_b, P = task
                    bi = grp()
                    if hp_pend[0] is not None:
                        head_norm_b(*hp_pend[0])
                    head_norm_a(bi, N, P)
                    hp_pend[0] = (bi, N, gidx, out_ap, out_b, P)

                def hp_flush():
                    if hp_pend[0] is not None:
                        head_norm_b(*hp_pend[0])
                        hp_pend[0] = None

                def u_grp(m):
                    bi = nb()
                    mm_group(banks[bi][:, :], [(w33[:, kc, m * 128:(m + 1) * 128], hT[:, kc, :]) for kc in range(KC)],
                             reads=[wb3, hT_b], writes=[bank_b[bi]])
                    c.op(ACT, lambda: A.copy(out=uT[:, m, :], in_=banks[bi][:, :]), reads=[bank_b[bi]], writes=[uT_b])

                fillers = []
                def qa_grp(p):
                    def g():
                        bi = nb()
                        for half in range(2):
                            c0 = (p + 4 * half) * 64
                            mm_group(banks[bi][64 * half:64 * half + 64, :], [(w31[:, kc, c0:c0 + 64], hT[:, kc, :]) for kc in range(KC)],
                                     reads=[wb1, hT_b], writes=[bank_b[bi]])
                        return bi
                    return g
                for p in range(4):
                    fillers.append(lambda p=p: hp_push((qa_grp(p), 512, 0, qaT[:, p, :], qaT_b, 128)))
                for m in range(2):
                    fillers.append(lambda m=m: u_grp(m))
                for h in range(4):
                    fillers.append(lambda h=h: hp_push((hd_grp(w33, wb3, 256 + h * 64), 512, 2, qmT[:, h, :], qmT_b, 64)))
                fillers.append(hp_flush)
                fillers.append(lambda: mem_attention())

                c.barrier([actT_b])
                for t in range(4):
                    j = st * 4 + t
                    L = (j + 1) * 128
                    for kb in range((L + 511) // 512):
                        k0 = kb * 512
                        kn = min(512, L - k0)
                        for h in range(4):
                            bi = nb()
                            mm_group(banks[bi][:, 0:kn], [(qiT[:, h, t * 128:(t + 1) * 128], kiT[:, k0:k0 + kn])],
                                     reads=[qiT_b, kiT_b], writes=[bank_b[bi]])
                            if h == 0:
                                c.op(DVE, lambda bi=bi, t=t, k0=k0, kn=kn: V.tensor_scalar(
                                    out=Isc[t][:, k0:k0 + kn], in0=banks[bi][:, 0:kn], scalar1=0.0, scalar2=wq[:, t, 0:1], op0=ALU.max, op1=ALU.mult),
                                    reads=[bank_b[bi], wq_b], writes=[Isc_b[t]])
                            else:
                                ri = h % 2
                                c.op(ACT, lambda bi=bi, ri=ri, kn=kn: A.activation(out=rl[ri][:, 0:kn], in_=banks[bi][:, 0:kn], func=AF.Relu),
                                     reads=[bank_b[bi]], writes=[rl_b[ri]])
                                c.op(DVE, lambda ri=ri, t=t, h=h, k0=k0, kn=kn: V.scalar_tensor_tensor(
                                    out=Isc[t][:, k0:k0 + kn], in0=rl[ri][:, 0:kn], scalar=wq[:, t, h:h + 1], in1=Isc[t][:, k0:k0 + kn],
                                    op0=ALU.mult, op1=ALU.add), reads=[rl_b[ri], wq_b, Isc_b[t]], writes=[Isc_b[t]])
                    c.op(DVE, lambda t=t, L=L: V.tensor_reduce(out=bmx[:, t:t + 1], in_=Isc[t][:, 0:L], axis=mybir.AxisListType.X, op=ALU.max),
                         reads=[Isc_b[t]], writes=[bmx_b])
                    c.op(DVE, lambda t=t, L=L: V.tensor_reduce(out=blo[:, t:t + 1], in_=Isc[t][:, 0:L], axis=mybir.AxisListType.X, op=ALU.min),
                         reads=[Isc_b[t]], writes=[blo_b])
                    c.op(DVE, lambda t=t, L=L: V.memset(Isc[t][0:64, L - 64:L], -BIG), reads=[Isc_b[t]], writes=[Isc_b[t]])
                need = [((st * 4 + t + 1) * 128 > TOPK) for t in range(4)]
                Ls = [(st * 4 + t + 1) * 128 for t in range(4)]
                on_act = [False, True, True, False]
                if any(need):
                    for t in range(4):
                        thr = float(Ls[t] - 2 * TOPK) if on_act[t] else float(Ls[t] - TOPK)
                        c.op(DVE, lambda t=t, thr=thr: V.memset(bthr[:, t:t + 1], thr), writes=[bthr_b])
                    c.op(DVE, lambda: V.tensor_tensor(out=bw[:], in0=bmx[:], in1=blo[:], op=ALU.subtract), reads=[bmx_b, blo_b], writes=[bw_b])
                    c.op(DVE, lambda: V.tensor_scalar(out=bpw[:], in0=bw[:], scalar1=1e-3, scalar2=1e-6, op0=ALU.mult, op1=ALU.add),
                         reads=[bw_b], writes=[bpw_b])
                    c.op(DVE, lambda: V.tensor_tensor(out=blo[:], in0=blo[:], in1=bpw[:], op=ALU.subtract), reads=[bpw_b, blo_b], writes=[blo_b])
                    c.op(DVE, lambda: V.tensor_tensor(out=bw[:], in0=bmx[:], in1=blo[:], op=ALU.subtract), reads=[bmx_b, blo_b], writes=[bw_b])
                    for t in range(4):
                        if not need[t]:
                            c.op(DVE, lambda t=t: V.memset(bcnt[:, t:t + 1], 0.0), writes=[bcnt_c[t]])
                    for it in range(NITER):
                        hw_ = 0.5 ** (it + 1)
                        c.op(DVE, lambda hw_=hw_: V.scalar_tensor_tensor(out=bmid[:], in0=bw[:], scalar=hw_, in1=blo[:], op0=ALU.mult, op1=ALU.add),
                             reads=[blo_b, bw_b], writes=[bmid_b])
                        for t in (1, 2, 3, 0):
                            if not need[t]:
                                continue
                            L = Ls[t]
                            if on_act[t]:
                                c.op(ACT, lambda t=t, L=L: A.activation(out=junkA[:, 0:L], in_=Isc[t][:, 0:L], func=AF.Sign, bias=bmid[:, t:t + 1], scale=-1.0, saturate=False,
                                                                        accum_out=bcnt[:, t:t + 1]),
                                     reads=[Isc_b[t], bmid_b], writes=[junkA_b, bcnt_c[t]])
                            else:
                                c.op(DVE, lambda t=t, L=L: V.tensor_scalar(out=junk[:, 0:L], in0=Isc[t][:, 0:L], scalar1=bmid[:, t:t + 1], scalar2=0.0,
                                                                            op0=ALU.is_le, op1=ALU.add, accum_out=bcnt[:, t:t + 1]),
                                     reads=[Isc_b[t], bmid_b], writes=[junk_b, bcnt_c[t]])
                        c.op(DVE, lambda: V.tensor_tensor(out=bpw[:], in0=bcnt[:], in1=bthr[:], op=ALU.is_le), reads=bcnt_c + [bthr_b], writes=[bpw_b])
                        c.op(DVE, lambda: V.tensor_tensor(out=bpw[:], in0=bpw[:], in1=bw[:], op=ALU.mult), reads=[bpw_b, bw_b], writes=[bpw_b])
                        c.op(DVE, lambda hw_=hw_: V.scalar_tensor_tensor(out=blo[:], in0=bpw[:], scalar=hw_, in1=blo[:], op0=ALU.mult, op1=ALU.add),
                             reads=[blo_b, bpw_b], writes=[blo_b])
                        if fillers:
                            fillers.pop(0)()
                while fillers:
                    fillers.pop(0)()
                for t in range(4):
                    if not need[t]:
                        c.op(DVE, lambda t=t: V.memset(blo[:, t:t + 1], -1.0e29), reads=[blo_b], writes=[blo_b])
                if "isc" in dbg_d and b == 0 and st == NT - 1:
                    dump("isc", Isc[3][:, 0:S], Isc_b[3], dbg_d["isc"][:, :])
                    dump("lo", blo[:, :], blo_b, dbg_d["lo"][:, :])

                emit_mask(0)
                emit_mask(1)
                attention(0)
                ssm_B(0); ssm_dve(0)
                emit_mask(2)
                attention(1)
                ssm_C(0); ssm_B(1); ssm_dve(1)
                emit_mask(3)
                attention(2)
                ssm_C(1); ssm_B(2); ssm_dve(2)
                attention(3)
                ssm_C(2); ssm_B(3); ssm_dve(3)
                ssm_C(3)

                for f in range(8):
                    wt, wb = piece(("mg", f))
                    wg = wt[:, 0:3072].rearrange("p (br kc d) -> p br kc d", br=3, kc=8)
                    woa = wt[0:64, 3072:4096].rearrange("p (h d) -> p h d", h=8)
                    wgl = wt[:, 4096:4608].rearrange("p (ab kc d) -> p ab kc d", ab=2, kc=2)
                    wom = wt[0:64, 4608:5120].rearrange("p (h d) -> p h d", h=4)
                    gb = [nb() for _ in range(3)]
                    for br in range(3):
                        mm_group(banks[gb[br]][:, :], [(wg[:, br, kc, :], hT[:, kc, :]) for kc in range(KC)], reads=[wb, hT_b], writes=[bank_b[gb[br]]])
                    ba = nb()
                    mm_group(banks[ba][:, :], [(woa[:, h, :], OaT[:, h, :]) for h in range(8)], reads=[wb, OaT_b], writes=[bank_b[ba]])
                    bga, bgb = nb(), nb()
                    mm_group(banks[bga][:, :], [(wgl[:, 0, kc, :], ysT[:, kc, :]) for kc in range(2)], reads=[wb, ysT_b], writes=[bank_b[bga]])
                    mm_group(banks[bgb][:, :], [(wgl[:, 1, kc, :], ysT[:, kc, :]) for kc in range(2)], reads=[wb, ysT_b], writes=[bank_b[bgb]])
                    bm = nb()
                    mm_group(banks[bm][:, :], [(wom[:, h, :], OmT[:, h, :]) for h in range(4)], reads=[wb, OmT_b], writes=[bank_b[bm]])
                    m0, m1, m2 = stmp[0], stmp[1], stmp[2]
                    y0, y1, y2 = stmp_b[0], stmp_b[1], stmp_b[2]
                    c.op(ACT, lambda: A.activation(out=m0[:], in_=banks[gb[0]][:, :], func=AF.Sigmoid), reads=[bank_b[gb[0]]], writes=[y0])
                    c.op(DVE, lambda: V.tensor_tensor(out=m0[:], in0=m0[:], in1=banks[ba][:, :], op=ALU.mult), reads=[y0, bank_b[ba]], writes=[y0])
                    c.op(ACT, lambda: A.activation(out=m1[:], in_=banks[bgb][:, :], func=AF.Sigmoid), reads=[bank_b[bgb]], writes=[y1])
                    c.op(DVE, lambda: V.tensor_tensor(out=m1[:], in0=m1[:], in1=banks[bga][:, :], op=ALU.mult), reads=[y1, bank_b[bga]], writes=[y1])
                    c.op(ACT, lambda: A.activation(out=m2[:], in_=banks[gb[1]][:, :], func=AF.Sigmoid), reads=[bank_b[gb[1]]], writes=[y2])
                    c.op(DVE, lambda: V.tensor_tensor(out=m1[:], in0=m1[:], in1=m2[:], op=ALU.mult), reads=[y1, y2], writes=[y1])
                    c.op(DVE, lambda: V.tensor_tensor(out=m0[:], in0=m0[:], in1=m1[:], op=ALU.add), reads=[y0, y1], writes=[y0])
                    c.op(ACT, lambda: A.activation(out=m2[:], in_=banks[gb[2]][:, :], func=AF.Sigmoid), reads=[bank_b[gb[2]]], writes=[y2])
                    c.op(DVE, lambda: V.tensor_tensor(out=m2[:], in0=m2[:], in1=banks[bm][:, :], op=ALU.mult), reads=[y2, bank_b[bm]], writes=[y2])
                    c.op(DVE, lambda f=f: V.tensor_tensor(out=mgT[:, f, :], in0=m0[:], in1=m2[:], op=ALU.add), reads=[y0, y2], writes=[mgT_b])
                for hh in range(2):
                    wt, wb = piece(("wo", hh))
                    w3 = wt[:, 0:4096].rearrange("p (kc d) -> p kc d", kc=8)
                    for t in range(4):
                        bi = nb()
                        mm_group(banks[bi][:, :], [(mgT[:, kc, t * 128:(t + 1) * 128], w3[:, kc, :]) for kc in range(KC)],
                                 reads=[wb, mgT_b], writes=[bank_b[bi]])
                        xsl = xt[:, t, hh * 512:(hh + 1) * 512]
                        c.op(DVE, lambda bi=bi, xsl=xsl: V.tensor_tensor(out=xsl, in0=xsl, in1=banks[bi][:, :], op=ALU.add),
                             reads=[bank_b[bi], xt_b[t]], writes=[xt_b[t]])
                if "x2" in dbg_d and b == 0:
                    for t in range(4):
                        dump("x2", xt[:, t, :], xt_b[t], dbg_d["x2"][st * 512 + t * 128:st * 512 + (t + 1) * 128, :])
                ffn("f2", 2)
                for t in range(4):
                    c.op(ACT, lambda t=t: A.activation(out=junk[:, 0:D], in_=xt[:, t, :], func=AF.Square, accum_out=ss[:, t:t + 1]),
                         reads=[xt_b[t]], writes=[junk_b, ss_b])
                    c.op(ACT, lambda t=t: A.activation(out=ss[:, 4 + t:5 + t], in_=ss[:, t:t + 1], func=AF.Ln, bias=epsc[:, 0:1], scale=1.0 / D),
                         reads=[ss_b], writes=[ss_b])
                    c.op(ACT, lambda t=t: A.activation(out=ss[:, 4 + t:5 + t], in_=ss[:, 4 + t:5 + t], func=AF.Exp, scale=-0.5), reads=[ss_b], writes=[ss_b])
                    c.op(DVE, lambda t=t: V.scalar_tensor_tensor(out=xt[:, t, :], in0=xt[:, t, :], scalar=ss[:, 4 + t:5 + t], in1=gfin[:],
                                                                 op0=ALU.mult, op1=ALU.mult), reads=[xt_b[t], ss_b], writes=[xt_b[t]])
                    c.dma(POOL, out_d[row0 + t * 128:row0 + (t + 1) * 128, :], xt[:, t, :], sem_out, reads=[xt_b[t]])
        G.wait_ge(sem_out.h, sem_out.n)
        if sem_misc.n:
            G.wait_ge(sem_misc.h, sem_misc.n)
    return nc


_CACHE = {}


def _run(inputs, NB, S, dbg=()):
    key = (NB, S, str(dbg))
    if key not in _CACHE:
        _CACHE[key] = build(NB, S, dbg)
    nc = _CACHE[key]
    x = np.ascontiguousarray(inputs["x"], dtype=np.float32)
    mem = np.ascontiguousarray(inputs["mem"], dtype=np.float32)
    ncore = 8
    wmap = {n: np.ascontiguousarray(np.asarray(inputs[n], dtype=np.float32).reshape(WSHAPES[n])) for n in WNAMES}
    wmap["onehot"] = _onehot_table()
    in_maps = []
    for i in range(ncore):
        m = dict(wmap)
        m["x"] = x[i * NB:(i + 1) * NB].reshape(NB * S, D)
        m["mem"] = mem[i * NB:(i + 1) * NB].reshape(NB * 256, D)
        in_maps.append(m)
    res = run_bass_kernel_spmd(nc, in_maps, core_ids=list(range(ncore)))
    return res


def kernel(**inputs):
    B, S = inputs["x"].shape[0], inputs["x"].shape[1]
    NB = B // 8
    res = _run(inputs, NB, S)
    out = np.concatenate([r["out"].reshape(NB, S, D) for r in res.results], axis=0)
    return out.astype(np.float32)
```
